# Optimizing a Trainium2 kernel written in Bass

```python
import math
import jax
import jax.numpy as jnp
from jax import lax
import numpy as np

D_MODEL = 1024
BATCH = 8
SEQ = 8192
DEPTH = 2

N_MIXERS = 4
N_HEADS = 4
HEAD_DIM = 64
MIX_WIDTH = N_HEADS * HEAD_DIM
D_FF = 2816
PLE_DIM = 256
NORM_EPS = 1e-6
NEG_BIG = -1e30
POS_BIG = 1e30
GATE_FLOOR = 1e-20

NSA_CMP_BLOCK = 32
NSA_CMP_STRIDE = 16
NSA_SEL_BLOCK = 64
NSA_TOP_N = 16
NSA_WINDOW = 512
NSA_Q_CHUNK = 64
NSA_CMP_HIDDEN = 128
NSA_WIDTHS = (MIX_WIDTH, HEAD_DIM, HEAD_DIM, HEAD_DIM, HEAD_DIM, HEAD_DIM, HEAD_DIM, 3 * N_HEADS)

HGRN_CHUNK = 32
HGRN_WIDTHS = (MIX_WIDTH, MIX_WIDTH, MIX_WIDTH, MIX_WIDTH)

RET_CHUNK = 64
RET_ROPE_BASE = 10000.0
RET_GN_EPS = 1e-5
RET_WIDTHS = (MIX_WIDTH, MIX_WIDTH, MIX_WIDTH, MIX_WIDTH)

RWKV_W_RANK = 64
RWKV_A_RANK = 64
RWKV_G_RANK = 128
RWKV_GN_EPS = 64e-5
RWKV_WIDTHS = (MIX_WIDTH, MIX_WIDTH, MIX_WIDTH, RWKV_W_RANK, RWKV_A_RANK, RWKV_G_RANK)
RWKV_IN = sum(RWKV_WIDTHS)

GROUP_WIDTHS = (sum(NSA_WIDTHS), sum(HGRN_WIDTHS), sum(RET_WIDTHS), RWKV_IN)
D_IN = sum(GROUP_WIDTHS)

kernel_name = 'hybrid_nsa_hgrn2_retnet_rwkv7_block'


def _split(a, widths):
    out = []
    off = 0
    for w in widths:
        out.append(a[..., off:off + w])
        off += w
    return out


def _rmsnorm(x, g):
    xf = x.astype(jnp.float32)
    y = xf * lax.rsqrt(jnp.mean(xf * xf, axis=-1, keepdims=True) + NORM_EPS)
    return (y * g.astype(jnp.float32)).astype(x.dtype)


def _head_rmsnorm(o, g):
    b, s, h, d = o.shape
    y = o * lax.rsqrt(jnp.mean(o * o, axis=-1, keepdims=True) + NORM_EPS)
    return y.reshape(b, s, h * d) * g.astype(jnp.float32)


def _head_layernorm(o, g, beta, eps):
    b, s, h, d = o.shape
    mu = jnp.mean(o, axis=-1, keepdims=True)
    var = jnp.mean(jnp.square(o - mu), axis=-1, keepdims=True)
    y = ((o - mu) * lax.rsqrt(var + eps)).reshape(b, s, h * d)
    return y * g.astype(jnp.float32) + beta.astype(jnp.float32)


def _nsa_mixer(u, pos_k, pos_v, cmp_k1, cmp_k2, cmp_v1, cmp_v2):
    u = u.astype(jnp.float32)
    bsz, seq, _ = u.shape
    q, k_c, v_c, k_s, v_s, k_w, v_w, g_logit = _split(u, NSA_WIDTHS)
    q = q.reshape(bsz, seq, N_HEADS, HEAD_DIM) * (HEAD_DIM ** -0.5)
    gates = jax.nn.sigmoid(g_logit).reshape(bsz, seq, 3, N_HEADS)

    n_cmp = (seq - NSA_CMP_BLOCK) // NSA_CMP_STRIDE + 1
    cmp_start = jnp.arange(n_cmp) * NSA_CMP_STRIDE
    blk_idx = cmp_start[:, None] + jnp.arange(NSA_CMP_BLOCK)[None, :]

    def compress(kv, pos, w1, w2):
        blk = kv[:, blk_idx] + pos.astype(jnp.float32)
        hid = jax.nn.silu(blk.reshape(bsz, n_cmp, NSA_CMP_BLOCK * HEAD_DIM) @ w1)
        return hid @ w2

    k_cmp = compress(k_c, pos_k, cmp_k1, cmp_k2)
    v_cmp = compress(v_c, pos_v, cmp_v1, cmp_v2)
    cmp_end = cmp_start + NSA_CMP_BLOCK - 1

    n_sel = seq // NSA_SEL_BLOCK
    n_top = min(NSA_TOP_N, n_sel)
    sel_start = jnp.arange(n_sel) * NSA_SEL_BLOCK
    overlap = ((cmp_start[:, None] < sel_start[None, :] + NSA_SEL_BLOCK)
               & (cmp_start[:, None] + NSA_CMP_BLOCK > sel_start[None, :])).astype(jnp.float32)
    k_blocks = k_s.reshape(bsz, n_sel, NSA_SEL_BLOCK, HEAD_DIM)
    v_blocks = v_s.reshape(bsz, n_sel, NSA_SEL_BLOCK, HEAD_DIM)
    blk_ids = jnp.arange(n_sel)
    gather = jax.vmap(lambda blocks, ix: blocks[ix])

    k_win = jnp.pad(k_w, ((0, 0), (NSA_WINDOW, 0), (0, 0)))
    v_win = jnp.pad(v_w, ((0, 0), (NSA_WINDOW, 0), (0, 0)))

    def query_chunk(c):
        t0 = c * NSA_Q_CHUNK
        qc = lax.dynamic_slice_in_dim(q, t0, NSA_Q_CHUNK, axis=1)
        gc = lax.dynamic_slice_in_dim(gates, t0, NSA_Q_CHUNK, axis=1)
        t = t0 + jnp.arange(NSA_Q_CHUNK)
        m_cmp = cmp_end[None, :] <= t[:, None]
        s_cmp = jnp.einsum('bqhd,bnd->bhqn', qc, k_cmp)
        p_cmp = jax.nn.softmax(jnp.where(m_cmp, s_cmp, NEG_BIG), axis=-1) * m_cmp
        o_cmp = jnp.einsum('bhqn,bnd->bqhd', p_cmp, v_cmp)
        importance = jnp.einsum('bhqn,ns->bqs', p_cmp, overlap)
        cur = t // NSA_SEL_BLOCK
        allowed = blk_ids[None, :] <= cur[:, None]
        forced = ((blk_ids[None, :] == 0) | (blk_ids[None, :] == cur[:, None])
                  | (blk_ids[None, :] == cur[:, None] - 1))
        score = jnp.where(forced, POS_BIG, jnp.where(allowed, importance, NEG_BIG))
        _, sel = lax.top_k(score, n_top)
        k_sel = gather(k_blocks, sel)
        v_sel = gather(v_blocks, sel)
        key_pos = sel[..., None] * NSA_SEL_BLOCK + jnp.arange(NSA_SEL_BLOCK)
        m_sel = (key_pos <= t[None, :, None, None])[:, :, None]
        s_sel = jnp.where(m_sel, jnp.einsum('bqhd,bqnkd->bqhnk', qc, k_sel), NEG_BIG)
        p_sel = jax.nn.softmax(s_sel.reshape(bsz, NSA_Q_CHUNK, N_HEADS, n_top * NSA_SEL_BLOCK), axis=-1)
        p_sel = p_sel.reshape(bsz, NSA_Q_CHUNK, N_HEADS, n_top, NSA_SEL_BLOCK)
        o_sel = jnp.einsum('bqhnk,bqnkd->bqhd', p_sel, v_sel)
        kw = lax.dynamic_slice_in_dim(k_win, t0, NSA_Q_CHUNK + NSA_WINDOW, axis=1)
        vw = lax.dynamic_slice_in_dim(v_win, t0, NSA_Q_CHUNK + NSA_WINDOW, axis=1)
        s_pos = t0 - NSA_WINDOW + jnp.arange(NSA_Q_CHUNK + NSA_WINDOW)
        dist = t[:, None] - s_pos[None, :]
        m_win = (dist >= 0) & (dist < NSA_WINDOW) & (s_pos[None, :] >= 0)
        s_win = jnp.einsum('bqhd,bkd->bhqk', qc, kw)
        p_win = jax.nn.softmax(jnp.where(m_win, s_win, NEG_BIG), axis=-1)
        o_win = jnp.einsum('bhqk,bkd->bqhd', p_win, vw)
        return (gc[:, :, 0, :, None] * o_cmp + gc[:, :, 1, :, None] * o_sel
                + gc[:, :, 2, :, None] * o_win)

    out = lax.map(query_chunk, jnp.arange(seq // NSA_Q_CHUNK))
    return out.transpose(1, 0, 2, 3, 4).reshape(bsz, seq, MIX_WIDTH)


def _chunk_gated_linear(q, k, v, log_f, chunk):
    bsz, seq, nh, dk = q.shape
    dv = v.shape[-1]
    n = seq // chunk

    def to_chunks(a):
        return a.reshape(bsz, n, chunk, nh, a.shape[-1]).transpose(1, 0, 3, 2, 4)

    causal = jnp.tril(jnp.ones((chunk, chunk), bool))[:, :, None]

    def step(state, inp):
        qc, kc, vc, gc = inp
        b = jnp.cumsum(gc, axis=2)
        o_inter = jnp.einsum('bhtd,bhde->bhte', qc * jnp.exp(b), state)
        diff = b[:, :, :, None, :] - b[:, :, None, :, :]
        decay = jnp.where(causal, jnp.exp(jnp.where(causal, diff, 0.0)), 0.0)
        attn = jnp.einsum('bhtd,bhsd,bhtsd->bhts', qc, kc, decay)
        o = o_inter + jnp.einsum('bhts,bhse->bhte', attn, vc)
        b_last = b[:, :, -1:, :]
        state = (jnp.exp(b_last[:, :, 0, :])[..., None] * state
                 + jnp.einsum('bhsd,bhse->bhde', kc * jnp.exp(b_last - b), vc))
        return state, o

    init = jnp.zeros((bsz, nh, dk, dv), jnp.float32)
    _, o = lax.scan(step, init, (to_chunks(q), to_chunks(k), to_chunks(v), to_chunks(log_f)))
    return o.transpose(1, 0, 3, 2, 4).reshape(bsz, seq, nh, dv)


def _hgrn2_mixer(u, lower_bound, norm_g):
    u = u.astype(jnp.float32)
    bsz, seq, _ = u.shape
    q, f_logit, i_in, o_gate = _split(u, HGRN_WIDTHS)
    lb = lower_bound.astype(jnp.float32)
    f = lb + (1.0 - lb) * jax.nn.sigmoid(f_logit)
    log_f = jnp.log(jnp.maximum(f, GATE_FLOOR))
    k = 1.0 - f
    q = jax.nn.silu(q)

    def heads(a):
        return a.reshape(bsz, seq, N_HEADS, HEAD_DIM)

    o = _chunk_gated_linear(heads(q), heads(k), heads(i_in), heads(log_f), HGRN_CHUNK)
    return _head_rmsnorm(o, norm_g) * jax.nn.silu(o_gate)


def _rope(a, cos, sin):
    half = a.shape[-1] // 2
    a1, a2 = a[..., :half], a[..., half:]
    return jnp.concatenate([a1 * cos - a2 * sin, a1 * sin + a2 * cos], axis=-1)


def _retention_mixer(u, norm_g, norm_b):
    u = u.astype(jnp.float32)
    bsz, seq, _ = u.shape
    q, k, v, g = _split(u, RET_WIDTHS)
    q = q.reshape(bsz, seq, N_HEADS, HEAD_DIM)
    k = k.reshape(bsz, seq, N_HEADS, HEAD_DIM)
    v = v.reshape(bsz, seq, N_HEADS, HEAD_DIM)
    pos = jnp.arange(seq, dtype=jnp.float32)
    inv_freq = RET_ROPE_BASE ** (-jnp.arange(0, HEAD_DIM, 2, dtype=jnp.float32) / HEAD_DIM)
    ang = pos[:, None] * inv_freq[None, :]
    cos, sin = jnp.cos(ang)[:, None, :], jnp.sin(ang)[:, None, :]
    q = _rope(q, cos, sin)
    k = _rope(k, cos, sin) * (HEAD_DIM ** -0.5)
    log_gamma = jnp.log(1.0 - jnp.exp2(-5.0 - jnp.arange(N_HEADS, dtype=jnp.float32)))

    n = seq // RET_CHUNK
    qc = q.reshape(bsz, n, RET_CHUNK, N_HEADS, HEAD_DIM)
    kc = k.reshape(bsz, n, RET_CHUNK, N_HEADS, HEAD_DIM)
    vc = v.reshape(bsz, n, RET_CHUNK, N_HEADS, HEAD_DIM)
    i = jnp.arange(RET_CHUNK, dtype=jnp.float32)
    dpos = i[:, None] - i[None, :]
    decay_mask = jnp.where(dpos >= 0, jnp.exp(jnp.maximum(dpos, 0.0)[None] * log_gamma[:, None, None]), 0.0)
    s = jnp.einsum('bnthd,bnshd->bnhts', qc, kc) * decay_mask
    o_intra = jnp.einsum('bnhts,bnshe->bnthe', s, vc)

    k_decay = jnp.exp((RET_CHUNK - 1.0 - i)[:, None] * log_gamma[None, :])
    kv = jnp.einsum('bnshd,sh,bnshe->nbhde', kc, k_decay, vc)
    chunk_decay = jnp.exp(RET_CHUNK * log_gamma)

    def carry_state(state, kv_c):
        return chunk_decay[None, :, None, None] * state + kv_c, state

    _, states = lax.scan(carry_state, jnp.zeros((bsz, N_HEADS, HEAD_DIM, HEAD_DIM), jnp.float32), kv)
    q_decay = jnp.exp((i + 1.0)[:, None] * log_gamma[None, :])
    o_inter = jnp.einsum('bnthd,th,nbhde->bnthe', qc, q_decay, states)
    o = (o_intra + o_inter).reshape(bsz, seq, N_HEADS, HEAD_DIM)
    return _head_layernorm(o, norm_g, norm_b, RET_GN_EPS) * jax.nn.silu(g)


def _rwkv7_mixer(u, mu, w0, w_up, a0, a_up, g_up, k_k, k_a, r_k, norm_g, norm_b):
    u = u.astype(jnp.float32)
    bsz, seq, _ = u.shape
    u_prev = jnp.pad(u[:, :-1], ((0, 0), (1, 0), (0, 0)))
    u = u + mu * (u_prev - u)
    r, k, v, w_lo, a_lo, g_lo = _split(u, RWKV_WIDTHS)
    decay = jnp.exp(-math.exp(-0.5) * jax.nn.sigmoid(w0 + jnp.tanh(w_lo) @ w_up))
    a = jax.nn.sigmoid(a0 + a_lo @ a_up)
    g = jax.nn.sigmoid(g_lo) @ g_up

    def heads(t):
        return t.reshape(bsz, seq, N_HEADS, HEAD_DIM)

    kk = heads(k * k_k)
    kk = kk * lax.rsqrt(jnp.maximum(jnp.sum(kk * kk, axis=-1, keepdims=True), 1e-24))
    k = k * (1.0 + (a - 1.0) * k_a)
    r_h, k_h, v_h, w_h, a_h = heads(r), heads(k), heads(v), heads(decay), heads(a)

    def step(state, inp):
        r_t, w_t, k_t, v_t, kk_t, a_t = inp
        sa = jnp.einsum('bhvk,bhk->bhv', state, -kk_t)
        state = (state * w_t[:, :, None, :] + sa[..., None] * (kk_t * a_t)[:, :, None, :]
                 + v_t[..., None] * k_t[:, :, None, :])
        return state, jnp.einsum('bhvk,bhk->bhv', state, r_t)

    def time_major(t):
        return t.transpose(1, 0, 2, 3)

    init = jnp.zeros((bsz, N_HEADS, HEAD_DIM, HEAD_DIM), jnp.float32)
    _, y = lax.scan(step, init, (time_major(r_h), time_major(w_h), time_major(k_h),
                                 time_major(v_h), time_major(kk), time_major(a_h)))
    y = _head_layernorm(time_major(y), norm_g, norm_b, RWKV_GN_EPS)
    bonus = jnp.sum(r_h * k_h * r_k.reshape(N_HEADS, HEAD_DIM), axis=-1, keepdims=True) * v_h
    return (y + bonus.reshape(bsz, seq, MIX_WIDTH)) * g


def setup_inputs(seed: int = 0) -> dict:
    key = jax.random.key(seed)
    keys = iter(jax.random.split(key, 48))

    def normal(shape, scale):
        return scale * jax.random.normal(next(keys), shape, jnp.float32)

    def gain(shape):
        return 1.0 + normal(shape, 0.02)

    L = DEPTH
    cmp_in = NSA_CMP_BLOCK * HEAD_DIM
    return {
        'x': normal((BATCH, SEQ, D_MODEL), 1.0),
        'p': normal((DEPTH, BATCH, SEQ, PLE_DIM), 1.0),
        'norm_mix': gain((L, D_MODEL)),
        'w_in': normal((L, D_MODEL, D_IN), D_MODEL ** -0.5),
        'nsa_pos_k': normal((L, NSA_CMP_BLOCK, HEAD_DIM), 0.02),
        'nsa_pos_v': normal((L, NSA_CMP_BLOCK, HEAD_DIM), 0.02),
        'nsa_cmp_k1': normal((L, cmp_in, NSA_CMP_HIDDEN), cmp_in ** -0.5),
        'nsa_cmp_k2': normal((L, NSA_CMP_HIDDEN, HEAD_DIM), NSA_CMP_HIDDEN ** -0.5),
        'nsa_cmp_v1': normal((L, cmp_in, NSA_CMP_HIDDEN), cmp_in ** -0.5),
        'nsa_cmp_v2': normal((L, NSA_CMP_HIDDEN, HEAD_DIM), NSA_CMP_HIDDEN ** -0.5),
        'hgrn_lb_logits': normal((L, MIX_WIDTH), 0.5),
        'hgrn_norm': gain((L, MIX_WIDTH)),
        'ret_norm_g': gain((L, MIX_WIDTH)),
        'ret_norm_b': normal((L, MIX_WIDTH), 0.02),
        'rwkv_mu': jax.random.uniform(next(keys), (L, RWKV_IN), jnp.float32, minval=0.2, maxval=0.8),
        'rwkv_w0': normal((L, MIX_WIDTH), 0.5),
        'rwkv_w_up': normal((L, RWKV_W_RANK, MIX_WIDTH), 0.5 * RWKV_W_RANK ** -0.5),
        'rwkv_a0': normal((L, MIX_WIDTH), 0.1),
        'rwkv_a_up': normal((L, RWKV_A_RANK, MIX_WIDTH), 0.5 * RWKV_A_RANK ** -0.5),
        'rwkv_g_up': normal((L, RWKV_G_RANK, MIX_WIDTH), RWKV_G_RANK ** -0.5),
        'rwkv_k_k': 0.85 + normal((L, MIX_WIDTH), 0.02),
        'rwkv_k_a': 1.0 + normal((L, MIX_WIDTH), 0.02),
        'rwkv_r_k': normal((L, MIX_WIDTH), 0.1),
        'rwkv_norm_g': gain((L, MIX_WIDTH)),
        'rwkv_norm_b': normal((L, MIX_WIDTH), 0.02),
        'w_branch': normal((L, N_MIXERS, MIX_WIDTH, D_MODEL), MIX_WIDTH ** -0.5),
        'w_gate': normal((L, N_MIXERS, D_MODEL, D_MODEL), D_MODEL ** -0.5),
        'b_gate': normal((L, N_MIXERS, D_MODEL), 0.01),
        'w_out': normal((L, D_MODEL, D_MODEL), D_MODEL ** -0.5),
        'norm_ffn': gain((L, D_MODEL)),
        'w_ffn_gate': normal((L, D_MODEL, D_FF), D_MODEL ** -0.5),
        'w_ffn_up': normal((L, D_MODEL, D_FF), D_MODEL ** -0.5),
        'w_ffn_down': normal((L, D_FF, D_MODEL), D_FF ** -0.5),
        'norm_ple': gain((L, D_MODEL)),
        'w_ple_gate': normal((L, D_MODEL, D_MODEL), D_MODEL ** -0.5),
        'w_ple_proj': normal((L, PLE_DIM, D_MODEL), PLE_DIM ** -0.5),
        'norm_final': gain((D_MODEL,)),
    }


def reference(x, p, norm_mix, w_in, nsa_pos_k, nsa_pos_v, nsa_cmp_k1, nsa_cmp_k2, nsa_cmp_v1,
              nsa_cmp_v2, hgrn_lb_logits, hgrn_norm, ret_norm_g, ret_norm_b, rwkv_mu, rwkv_w0,
              rwkv_w_up, rwkv_a0, rwkv_a_up, rwkv_g_up, rwkv_k_k, rwkv_k_a, rwkv_r_k, rwkv_norm_g,
              rwkv_norm_b, w_branch, w_gate, b_gate, w_out, norm_ffn, w_ffn_gate, w_ffn_up,
              w_ffn_down, norm_ple, w_ple_gate, w_ple_proj, norm_final):
    lb_soft = jax.nn.softmax(hgrn_lb_logits.astype(jnp.float32), axis=0)
    lower_bounds = jnp.cumsum(lb_soft, axis=0) - lb_soft[0]

    h = x
    for i in range(DEPTH):
        xn = _rmsnorm(h, norm_mix[i])
        u = xn @ w_in[i]
        u_nsa, u_hgrn, u_ret, u_rwkv = _split(u, GROUP_WIDTHS)
        branch_outs = (
            _nsa_mixer(u_nsa, nsa_pos_k[i], nsa_pos_v[i], nsa_cmp_k1[i], nsa_cmp_k2[i],
                       nsa_cmp_v1[i], nsa_cmp_v2[i]),
            _hgrn2_mixer(u_hgrn, lower_bounds[i], hgrn_norm[i]),
            _retention_mixer(u_ret, ret_norm_g[i], ret_norm_b[i]),
            _rwkv7_mixer(u_rwkv, rwkv_mu[i], rwkv_w0[i], rwkv_w_up[i], rwkv_a0[i], rwkv_a_up[i],
                         rwkv_g_up[i], rwkv_k_k[i], rwkv_k_a[i], rwkv_r_k[i], rwkv_norm_g[i],
                         rwkv_norm_b[i]),
        )
        merged = None
        for m in range(N_MIXERS):
            gate = jax.nn.sigmoid(xn @ w_gate[i, m] + b_gate[i, m])
            term = gate * (branch_outs[m] @ w_branch[i, m])
            merged = term if merged is None else merged + term
        h = h + (merged @ w_out[i]).astype(h.dtype)

        hn = _rmsnorm(h, norm_ffn[i])
        ff = (jax.nn.silu(hn @ w_ffn_gate[i]) * (hn @ w_ffn_up[i])) @ w_ffn_down[i]
        h = h + ff.astype(h.dtype)

        hp = _rmsnorm(h, norm_ple[i])
        ple = jax.nn.sigmoid(hp @ w_ple_gate[i]) * (p[i] @ w_ple_proj[i])
        h = h + ple.astype(h.dtype)
    return _rmsnorm(h, norm_final)
```

```python
import numpy as np
from contextlib import ExitStack
import concourse.bass as bass
import concourse.mybir as mybir
from concourse.bass_utils import run_bass_kernel_spmd

F32 = mybir.dt.float32
BF16 = mybir.dt.bfloat16
AF = mybir.ActivationFunctionType
ALU = mybir.AluOpType
AX = mybir.AxisListType

D = 1024
DEPTH = 2
DFF = 2816
NFC = DFF // 128
NSA_BASE, HG_BASE, RET_BASE, RW_BASE = 0, 652, 1676, 2700
EPS = 1e-6
STORE_Q = 'sp'


class _Keep:
    def __init__(self, es):
        self.es = es

    def __enter__(self):
        return self.es

    def __exit__(self, *a):
        return False


class Sched:
    EPOCH = 8000
    NDMA = 24

    def __init__(self, nc, es):
        self.nc = nc
        self.es = es
        self.E = {'pe': nc.tensor, 'dve': nc.vector, 'act': nc.scalar, 'pool': nc.gpsimd, 'sp': nc.sync}
        self.sems = []
        self.prog = {}
        for e in self.E:
            self.prog[e] = [self.new_sem(), 0]
        self.waited = {e: {} for e in self.E}
        self.lastw = {}
        self.readers = {}
        self.dma_pool = [self.new_sem() for _ in range(self.NDMA)]
        self.dma_val = {s: 0 for s in self.dma_pool}
        self.dma_next = 0
        self.n_inst = 0
        self.ns = None
        self.pe_sync = False

    def new_sem(self):
        h = self.es.enter_context(self.nc.semaphore("s%d" % len(self.sems)))
        self.sems.append(h)
        return len(self.sems) - 1

    def _wait(self, eng, deps):
        w = self.waited[eng]
        for s, v in deps.items():
            if w.get(s, 0) >= v:
                continue
            self.E[eng].wait_ge(self.sems[s], v)
            w[s] = v

    def _deps(self, reads, writes):
        deps = {}
        for k in reads:
            lw = self.lastw.get(k)
            if lw and deps.get(lw[0], 0) < lw[1]:
                deps[lw[0]] = lw[1]
        for k in writes:
            lw = self.lastw.get(k)
            if lw and deps.get(lw[0], 0) < lw[1]:
                deps[lw[0]] = lw[1]
            rd = self.readers.get(k)
            if rd:
                for s, v in rd.items():
                    if deps.get(s, 0) < v:
                        deps[s] = v
        return deps

    def _record(self, reads, writes, s, v):
        for k in writes:
            self.lastw[k] = (s, v)
            self.readers[k] = {}
        for k in reads:
            self.readers.setdefault(k, {})[s] = v

    def op(self, eng, r, w, fn):
        r = [self.nk(k) for k in r]
        w = [self.nk(k) for k in w]
        pk = [k for k in r if (isinstance(k, tuple) and k[0] == 'ps') or k == 'pb0']
        if pk:
            r = [k for k in r if k not in pk]
            w = list(w) + pk
        sync = False
        if eng == 'pes':
            eng, sync = 'pe', True
        deps = self._deps(r, w)
        pr = self.prog[eng]
        if eng == 'pe' and not sync:
            deps.pop(pr[0], None)
        self._wait(eng, deps)
        inst = fn(self.E[eng])
        pr[1] += 1
        inst.then_inc(self.sems[pr[0]], 1)
        self._record(r, w, pr[0], pr[1])
        self.n_inst += 1
        ret = (pr[0], pr[1])
        if pr[1] >= self.EPOCH:
            self.prog[eng] = [self.new_sem(), 0]
        return ret

    GLOBAL_KEYS = {'pb0', 'ident', 'identf', 'onesbd', 'gmask', 'rmask', 'rw_mask4', 'rw_lmask', 'rw_rmask'}

    def nk(self, k):
        if k == 'pb1':
            return 'pb0'
        if self.ns is None or k in self.GLOBAL_KEYS or (isinstance(k, tuple) and k[0] == 'ps'):
            return k
        return (self.ns, k)

    def run_streams(self, streams):
        live = list(streams)
        while live:
            for item in list(live):
                self.ns = item[0]
                try:
                    next(item[1])
                except StopIteration:
                    live.remove(item)
        self.ns = None

    def dma(self, eng, out, in_, r, w):
        r = [self.nk(k) for k in r]
        w = [self.nk(k) for k in w]
        s = self.dma_pool[self.dma_next % self.NDMA]
        self.dma_next += 1
        deps = self._deps(r, w)
        pv = self.dma_val[s]
        if pv and deps.get(s, 0) < pv:
            deps[s] = pv
        self._wait(eng, deps)
        self.E[eng].dma_start(out=out, in_=in_).then_inc(self.sems[s], 16)
        self.dma_val[s] = pv + 16
        self._record(r, w, s, pv + 16)
        self.n_inst += 1

    def barrier(self):
        allv = {}
        for e, (s, v) in self.prog.items():
            if v:
                allv[s] = v
        for s, v in self.dma_val.items():
            if v:
                allv[s] = v
        for e in self.E:
            d = dict(allv)
            self._wait(e, d)
        self.lastw = {}
        self.readers = {}


class MK:
    def __init__(self, S, last_layer_final=True):
        self.S = S
        self.nc = bass.Bass("TRN2", target_bir_lowering=False)
        self.inputs = {}

    def din(self, name, shape, dt=F32):
        t = self.nc.dram_tensor(name, list(shape), dt, kind="ExternalInput").ap()
        self.inputs[name] = t
        return t

    def dscr(self, name, shape, dt=F32):
        return self.nc.dram_tensor(name, list(shape), dt, kind="Internal").ap()

    def sb(self, es, name, shape, dt):
        self.uid = getattr(self, 'uid', 0) + 1
        return es.enter_context(self.nc.sbuf_tensor("%s_u%d" % (name, self.uid), list(shape), dt))


def _mm(sc, ps_key, ps_ap, pairs, rkeys):
    n = len(pairs)
    for i, (l, r) in enumerate(pairs):
        sc.op('pe', rkeys, [ps_key],
              lambda e, l=l, r=r, i=i: e.matmul(ps_ap, lhsT=l, rhs=r, start=(i == 0), stop=(i == n - 1)))


def p1_plan():
    cols = []
    groups = []

    def add(name, idx):
        groups.append((name, len(cols), len(idx)))
        cols.extend(idx)

    b = NSA_BASE
    for h in range(4):
        add('nsa_q%d' % h, list(range(b + 64 * h, b + 64 * h + 64)))
    add('nsa_kc', list(range(b + 256, b + 320)))
    add('nsa_vc', list(range(b + 320, b + 384)))
    add('nsa_ks', list(range(b + 384, b + 448)))
    add('nsa_kw', list(range(b + 512, b + 576)))
    for gi in range(12):
        add('nsa_g%d' % gi, [b + 640 + gi] * 64)
    b = HG_BASE
    for nm, off in (('hg_q', 0), ('hg_f', 256), ('hg_og', 768)):
        for c in range(2):
            add('%s%d' % (nm, c), list(range(b + off + 128 * c, b + off + 128 * c + 128)))
    b = RET_BASE

    def rot(base):
        out = []
        for h in range(4):
            for d in range(64):
                out.append(base + 64 * h + (d + 32) % 64)
        return out
    for nm, idx in (('ret_q', list(range(b, b + 256))), ('ret_qr', rot(b)),
                    ('ret_k', list(range(b + 256, b + 512))), ('ret_kr', rot(b + 256)),
                    ('ret_g', list(range(b + 768, b + 1024)))):
        for c in range(2):
            add('%s%d' % (nm, c), idx[128 * c:128 * c + 128])
    b = RW_BASE
    for c in range(8):
        add('rw%d' % c, list(range(b + 128 * c, b + 128 * c + 128)))
    nch = len(cols)
    tok = (list(range(NSA_BASE + 448, NSA_BASE + 512)) + list(range(NSA_BASE + 576, NSA_BASE + 640))
           + list(range(HG_BASE + 512, HG_BASE + 768)) + list(range(RET_BASE + 512, RET_BASE + 768)))
    cols.extend(tok)
    return np.array(cols, dtype=np.int64), groups, nch, len(tok)


P1_COLS, P1_GROUPS, P1_NCH, P1_NTOK = p1_plan()
P1_NC = len(P1_COLS)
GIDX = {g[0]: i for i, g in enumerate(P1_GROUPS)}
NG = len(P1_GROUPS)


class Dense:
    def __init__(self, mk, sc, es):
        self.mk, self.sc, self.nc = mk, sc, mk.nc
        nc = self.nc
        self.ps = [es.enter_context(nc.psum_tensor("ps%d" % i, [128, 512], F32)) for i in range(7)]
        pb = es.enter_context(nc.psum_tensor("pb0", [128, 1024], BF16))
        self.pb = [pb, pb]
        self.ps_pool = list(range(7))
        self.ps_i = 0
        self.ident = mk.sb(es, "ident", [128, 128], BF16)
        self.identf = mk.sb(es, "identf", [128, 128], F32)
        idin = mk.din("c_ident", [128, 128])
        sc.dma('sp', self.identf[:], idin[:, :], [], ['identf'])
        sc.op('dve', ['identf'], ['ident'], lambda e: e.tensor_copy(out=self.ident[:], in_=self.identf[:]))
        self.ev = 0

    def next_ps(self):
        i = self.ps_pool[self.ps_i % len(self.ps_pool)]
        self.ps_i += 1
        return i

    def evac_eng(self):
        self.ev += 1
        return 'act' if self.ev % 2 else 'dve'

    def copy(self, eng, out, in_, r, w):
        if eng == 'act':
            self.sc.op('act', r, w, lambda e: e.activation(out=out, in_=in_, func=AF.Copy))
        else:
            self.sc.op(eng, r, w, lambda e: e.tensor_copy(out=out, in_=in_))

    def load_w(self, es, name, src, K, N, stage):
        sc = self.sc
        kc = K // 128
        dst = self.mk.sb(es, name, [128, kc, N], BF16)
        engs = ['pool', 'dve', 'act']
        for c in range(kc):
            for n0 in range(0, N, 2048):
                n1 = min(N, n0 + 2048)
                si = self.ev % 2
                self.ev += 1
                st = stage[si]
                sc.dma('sp', st[:, 0:n1 - n0], src[c * 128:(c + 1) * 128, n0:n1], [], [('wst', si)])
                self.copy(engs[self.ev % 3], dst[:, c, n0:n1], st[:, 0:n1 - n0], [('wst', si)], [name])
        return dst

    def front(self, bufs, h_src, t0, nsub, gain_key):
        sc = self.sc
        hk, xnT, xn, junk, ss, gain = bufs['h'], bufs['xnT'], bufs['xn'], bufs['junk'], bufs['ss'], bufs[gain_key]
        pb = self.pb[0]
        for sub in range(nsub):
            r0 = t0 + sub * 128
            sc.dma('sp', hk[:, sub, :], h_src[r0:r0 + 128, :], [], [('h', sub)])
            self.norm_T(bufs, hk[:, sub, :], ('h', sub), gain, gain_key, sub)

    def rstd(self, h_key, h_ap, junk, ss):
        sc = self.sc
        sc.op('act', [h_key], ['junk', 'ss'], lambda e: e.activation(
            out=junk[:], in_=h_ap, func=AF.Square, scale=1.0 / 32.0, accum_out=ss[:, 0:1]))
        sc.op('dve', ['ss'], ['ss'], lambda e: e.tensor_scalar(
            out=ss[:, 1:2], in0=ss[:, 0:1], scalar1=EPS, scalar2=None, op0=ALU.add))
        sc.op('act', ['ss'], ['ss'], lambda e: e.activation(out=ss[:, 3:4], in_=ss[:, 1:2], func=AF.Ln))
        sc.op('act', ['ss'], ['ss'], lambda e: e.activation(out=ss[:, 2:3], in_=ss[:, 3:4], func=AF.Exp, scale=-0.5))

    def norm_T(self, bufs, h_ap, h_key, gain, gain_key, sub):
        sc = self.sc
        xnT, xn, junk, ss = bufs['xnT'], bufs['xn'], bufs['junk'], bufs['ss']
        pb = self.pb[0]
        self.rstd(h_key, h_ap, junk, ss)
        sc.op('dve', [h_key, 'ss', gain_key], ['xn'], lambda e: e.scalar_tensor_tensor(
            out=xn[:], in0=h_ap, scalar=ss[:, 2:3], in1=gain[:], op0=ALU.mult, op1=ALU.mult))
        for k in range(8):
            sc.op('pe', ['xn', 'ident'], ['pb0'], lambda e, k=k: e.transpose(
                out=pb[:, k * 128:(k + 1) * 128], in_=xn[:, k * 128:(k + 1) * 128], identity=self.ident[:]))
        sc.op('act', ['pb0'], ['xnT'], lambda e: e.activation(
            out=xnT[:, :, sub * 128:(sub + 1) * 128], in_=pb[:].rearrange("p (k t) -> p k t", k=8), func=AF.Copy))

    def front_bufs(self, es, nsub, gains):
        mk = self.mk
        b = {'h': mk.sb(es, "f_h", [128, nsub, D], F32), 'xnT': mk.sb(es, "f_xnT", [128, 8, nsub * 128], BF16),
             'xn': mk.sb(es, "f_xn", [128, D], BF16), 'junk': mk.sb(es, "f_junk", [128, D], BF16),
             'ss': mk.sb(es, "f_ss", [128, 4], F32)}
        for key, src in gains.items():
            b[key] = mk.sb(es, "f_" + key, [128, D], F32)
            self.sc.dma('sp', b[key][:], src, [], [key])
        return b

    def pass_p1(self, L, h_src, w1_src, gain_src, UC, UT):
        sc, mk, S = self.sc, self.mk, self.mk.S
        with ExitStack() as es:
            stage = [mk.sb(es, "wst%d" % i, [128, 2048], F32) for i in range(2)]
            W1 = self.load_w(es, "W1", w1_src, D, P1_NC, stage)
            fb = self.front_bufs(es, 4, {'gain': gain_src})
            ost = [mk.sb(es, "ost%d" % i, [128, 640], F32) for i in range(4)]
            oi = 0
            for t0 in range(0, S, 512):
                self.front(fb, h_src, t0, 4, 'gain')
                xnT = fb['xnT']
                for gi, (name, off, M) in enumerate(P1_GROUPS):
                    pi = self.next_ps()
                    ps = self.ps[pi]
                    _mm(sc, ('ps', pi), ps[0:M, :],
                        [(W1[:, k, off:off + M], xnT[:, k, :]) for k in range(8)], ['W1', 'xnT'])
                    o = oi % 4
                    oi += 1
                    self.copy(self.evac_eng(), ost[o][0:M, 0:512], ps[0:M, :], [('ps', pi)], [('ost', o)])
                    sc.dma(STORE_Q, UC[gi, 0:M, t0:t0 + 512], ost[o][0:M, 0:512], [('ost', o)], [])
                for sub in range(4):
                    o = oi % 4
                    oi += 1
                    for (c0, c1) in ((0, 512), (512, P1_NTOK)):
                        pi = self.next_ps()
                        ps = self.ps[pi]
                        _mm(sc, ('ps', pi), ps[:, 0:c1 - c0],
                            [(xnT[:, k, sub * 128:(sub + 1) * 128], W1[:, k, P1_NCH + c0:P1_NCH + c1])
                             for k in range(8)], ['W1', 'xnT'])
                        self.copy(self.evac_eng(), ost[o][:, c0:c1], ps[:, 0:c1 - c0], [('ps', pi)], [('ost', o)])
                    r0 = t0 + sub * 128
                    sc.dma(STORE_Q, UT[r0:r0 + 128, :], ost[o][:, 0:P1_NTOK], [('ost', o)], [])
            sc.barrier()

    def pass_merge(self, L, h_src, h_dst, OT, wg_src, wb_src, wo_src, bg_src, gain_src):
        sc, mk, S = self.sc, self.mk, self.mk.S
        with ExitStack() as es:
            stage = [mk.sb(es, "wst%d" % i, [128, 2048], F32) for i in range(2)]
            Wg = [self.load_w(es, "Wg%d" % m, wg_src[m], D, D, stage) for m in range(4)]
            Wb = [self.load_w(es, "Wb%d" % m, wb_src[m], 256, D, stage) for m in range(4)]
            Wo = self.load_w(es, "Wo", wo_src, D, D, stage)
            bg = mk.sb(es, "bg", [128, 32], F32)
            sc.dma('sp', bg[:], bg_src, [], ['bg'])
            sc.op('dve', ['bg'], ['bg'], lambda e: e.tensor_scalar(
                out=bg[:], in0=bg[:], scalar1=-1.0, scalar2=None, op0=ALU.mult))
            NS = 2
            TT = NS * 128
            fb = self.front_bufs(es, NS, {'gain': gain_src})
            ot = mk.sb(es, "m_ot", [128, 8, TT], BF16)
            gsb = [mk.sb(es, "m_g%d" % i, [128, TT], F32) for i in range(2)]
            acc = mk.sb(es, "m_acc", [128, TT], F32)
            tmp = mk.sb(es, "m_tmp", [128, TT], F32)
            mT = mk.sb(es, "m_mT", [128, 8, TT], BF16)
            hn = mk.sb(es, "m_hn", [128, D], F32)
            gi_ = 0
            for t0 in range(0, S, TT):
                self.front(fb, h_src, t0, NS, 'gain')
                xnT = fb['xnT']
                for m in range(4):
                    for c in range(2):
                        sc.dma('sp', ot[:, m * 2 + c, :], OT[m, c, :, t0:t0 + TT], [], ['ot'])
                for j in range(8):
                    for m in range(4):
                        pg = self.next_ps()
                        _mm(sc, ('ps', pg), self.ps[pg][:, 0:TT],
                            [(Wg[m][:, k, j * 128:(j + 1) * 128], xnT[:, k, :]) for k in range(8)],
                            ['Wg%d' % m, 'xnT'])
                        pbr = self.next_ps()
                        _mm(sc, ('ps', pbr), self.ps[pbr][:, 0:TT],
                            [(Wb[m][:, c, j * 128:(j + 1) * 128], ot[:, m * 2 + c, :]) for c in range(2)],
                            ['Wb%d' % m, 'ot'])
                        g = gi_ % 2
                        gi_ += 1
                        sc.op('act', [('ps', pg), 'bg'], [('gsb', g)], lambda e, g=g, pg=pg, m=m, j=j: e.activation(
                            out=gsb[g][:], in_=self.ps[pg][:, 0:TT], func=AF.Exp,
                            bias=bg[:, m * 8 + j:m * 8 + j + 1], scale=-1.0))
                        sc.op('pool', [('gsb', g)], [('gsb', g)], lambda e, g=g: e.tensor_scalar(
                            out=gsb[g][:], in0=gsb[g][:], scalar1=1.0, scalar2=None, op0=ALU.add))
                        sc.op('dve', [('gsb', g)], [('gsb', g)], lambda e, g=g: e.reciprocal(out=gsb[g][:], in_=gsb[g][:]))
                        if m == 0:
                            sc.op('dve', [('gsb', g), ('ps', pbr)], ['acc'], lambda e, g=g, pbr=pbr: e.tensor_tensor(
                                out=acc[:], in0=gsb[g][:], in1=self.ps[pbr][:, 0:TT], op=ALU.mult))
                        else:
                            sc.op('dve', [('gsb', g), ('ps', pbr)], ['tmp'], lambda e, g=g, pbr=pbr: e.tensor_tensor(
                                out=tmp[:], in0=gsb[g][:], in1=self.ps[pbr][:, 0:TT], op=ALU.mult))
                            if m < 3:
                                sc.op('pool', ['tmp', 'acc'], ['acc'], lambda e: e.tensor_tensor(
                                    out=acc[:], in0=acc[:], in1=tmp[:], op=ALU.add))
                            else:
                                sc.op('pool', ['tmp', 'acc'], ['mT'], lambda e, j=j: e.tensor_tensor(
                                    out=mT[:, j, :], in0=acc[:], in1=tmp[:], op=ALU.add))
                for sub in range(NS):
                    for half in range(2):
                        po = self.next_ps()
                        _mm(sc, ('ps', po), self.ps[po][:, :],
                            [(mT[:, k, sub * 128:(sub + 1) * 128], Wo[:, k, half * 512:(half + 1) * 512])
                             for k in range(8)], ['Wo', 'mT'])
                        sc.op('dve', [('ps', po), ('h', sub)], ['hn'], lambda e, po=po, sub=sub, half=half: e.tensor_tensor(
                            out=hn[:, half * 512:(half + 1) * 512], in0=fb['h'][:, sub, half * 512:(half + 1) * 512],
                            in1=self.ps[po][:, :], op=ALU.add))
                    r0 = t0 + sub * 128
                    sc.dma(STORE_Q, h_dst[r0:r0 + 128, :], hn[:], ['hn'], [])
            sc.barrier()

    def pass_ffn(self, L, h_src, h_dst, wfg_src, wfu_src, wfd_src, gain_src):
        sc, mk, S = self.sc, self.mk, self.mk.S
        with ExitStack() as es:
            stage = [mk.sb(es, "wst%d" % i, [128, 2048], F32) for i in range(2)]
            Wfg = self.load_w(es, "Wfg", wfg_src, D, DFF, stage)
            Wfu = self.load_w(es, "Wfu", wfu_src, D, DFF, stage)
            Wfd = self.load_w(es, "Wfd", wfd_src, DFF, D, stage)
            NS = 2
            TT = NS * 128
            fb = self.front_bufs(es, NS, {'gain': gain_src})
            hid = mk.sb(es, "f_hid", [128, NFC, TT], BF16)
            gsb = [mk.sb(es, "f_g%d" % i, [128, TT], F32) for i in range(2)]
            hn = mk.sb(es, "f_hn", [128, D], F32)
            gi_ = 0
            for t0 in range(0, S, TT):
                self.front(fb, h_src, t0, NS, 'gain')
                xnT = fb['xnT']
                for f in range(NFC):
                    pg = self.next_ps()
                    _mm(sc, ('ps', pg), self.ps[pg][:, 0:TT],
                        [(Wfg[:, k, f * 128:(f + 1) * 128], xnT[:, k, :]) for k in range(8)], ['Wfg', 'xnT'])
                    pu = self.next_ps()
                    _mm(sc, ('ps', pu), self.ps[pu][:, 0:TT],
                        [(Wfu[:, k, f * 128:(f + 1) * 128], xnT[:, k, :]) for k in range(8)], ['Wfu', 'xnT'])
                    g = gi_ % 2
                    gi_ += 1
                    sc.op('act', [('ps', pg)], [('gsb', g)], lambda e, g=g, pg=pg: e.activation(
                        out=gsb[g][:], in_=self.ps[pg][:, 0:TT], func=AF.Exp, scale=-1.0))
                    sc.op('pool', [('gsb', g)], [('gsb', g)], lambda e, g=g: e.tensor_scalar(
                        out=gsb[g][:], in0=gsb[g][:], scalar1=1.0, scalar2=None, op0=ALU.add))
                    sc.op('dve', [('gsb', g)], [('gsb', g)], lambda e, g=g: e.reciprocal(out=gsb[g][:], in_=gsb[g][:]))
                    sc.op('dve', [('gsb', g), ('ps', pu)], [('gsb', g)], lambda e, g=g, pu=pu: e.tensor_tensor(
                        out=gsb[g][:], in0=gsb[g][:], in1=self.ps[pu][:, 0:TT], op=ALU.mult))
                    sc.op('dve', [('gsb', g), ('ps', pg)], ['hid'], lambda e, g=g, pg=pg, f=f: e.tensor_tensor(
                        out=hid[:, f, :], in0=gsb[g][:], in1=self.ps[pg][:, 0:TT], op=ALU.mult))
                for sub in range(NS):
                    for half in range(2):
                        po = self.next_ps()
                        _mm(sc, ('ps', po), self.ps[po][:, :],
                            [(hid[:, f, sub * 128:(sub + 1) * 128], Wfd[:, f, half * 512:(half + 1) * 512])
                             for f in range(NFC)], ['Wfd', 'hid'])
                        sc.op('dve', [('ps', po), ('h', sub)], ['hn'], lambda e, po=po, sub=sub, half=half: e.tensor_tensor(
                            out=hn[:, half * 512:(half + 1) * 512], in0=fb['h'][:, sub, half * 512:(half + 1) * 512],
                            in1=self.ps[po][:, :], op=ALU.add))
                    r0 = t0 + sub * 128
                    sc.dma(STORE_Q, h_dst[r0:r0 + 128, :], hn[:], ['hn'], [])
            sc.barrier()

    def pass_ple(self, L, h_src, h_dst, p_src, wpg_src, wpp_src, gain_src, final_gain_src):
        sc, mk, S = self.sc, self.mk, self.mk.S
        with ExitStack() as es:
            stage = [mk.sb(es, "wst%d" % i, [128, 2048], F32) for i in range(2)]
            Wpg = self.load_w(es, "Wpg", wpg_src, D, D, stage)
            Wpp = self.load_w(es, "Wpp", wpp_src, 256, D, stage)
            NS = 4
            gains = {'gain': gain_src}
            if final_gain_src is not None:
                gains['fgain'] = final_gain_src
            fb = self.front_bufs(es, NS, gains)
            pt = mk.sb(es, "p_pt", [128, 256], F32)
            ptb = mk.sb(es, "p_ptb", [128, 256], BF16)
            pT = mk.sb(es, "p_pT", [128, 2, 128], BF16)
            gsb = mk.sb(es, "p_g", [128, 512], F32)
            hn = mk.sb(es, "p_hn", [128, D], F32)
            ho = mk.sb(es, "p_ho", [128, D], F32)
            pbk = self.pb[1]
            for t0 in range(0, S, NS * 128):
                self.front(fb, h_src, t0, NS, 'gain')
                xnT = fb['xnT']
                for sub in range(NS):
                    r0 = t0 + sub * 128
                    sc.dma('sp', pt[:], p_src[r0:r0 + 128, :], [], ['pt'])
                    sc.op('pool', ['pt'], ['ptb'], lambda e: e.tensor_copy(out=ptb[:], in_=pt[:]))
                    for c in range(2):
                        sc.op('pe', ['ptb', 'ident'], ['pb1'], lambda e, c=c: e.transpose(
                            out=pbk[:, c * 128:(c + 1) * 128], in_=ptb[:, c * 128:(c + 1) * 128], identity=self.ident[:]))
                    sc.op('dve', ['pb1'], ['pT'], lambda e: e.tensor_copy(
                        out=pT[:], in_=pbk[:, 0:256].rearrange("p (c t) -> p c t", c=2)))
                    for half in range(2):
                        pg = self.next_ps()
                        _mm(sc, ('ps', pg), self.ps[pg][:, :],
                            [(xnT[:, k, sub * 128:(sub + 1) * 128], Wpg[:, k, half * 512:(half + 1) * 512])
                             for k in range(8)], ['Wpg', 'xnT'])
                        pp = self.next_ps()
                        _mm(sc, ('ps', pp), self.ps[pp][:, :],
                            [(pT[:, c, :], Wpp[:, c, half * 512:(half + 1) * 512]) for c in range(2)], ['Wpp', 'pT'])
                        sc.op('act', [('ps', pg)], ['gsb'], lambda e, pg=pg: e.activation(
                            out=gsb[:], in_=self.ps[pg][:, :], func=AF.Exp, scale=-1.0))
                        sc.op('pool', ['gsb'], ['gsb'], lambda e: e.tensor_scalar(
                            out=gsb[:], in0=gsb[:], scalar1=1.0, scalar2=None, op0=ALU.add))
                        sc.op('dve', ['gsb'], ['gsb'], lambda e: e.reciprocal(out=gsb[:], in_=gsb[:]))
                        sc.op('dve', ['gsb', ('ps', pp)], ['gsb'], lambda e, pp=pp: e.tensor_tensor(
                            out=gsb[:], in0=gsb[:], in1=self.ps[pp][:, :], op=ALU.mult))
                        sc.op('pool', ['gsb', ('h', sub)], ['hn'], lambda e, sub=sub, half=half: e.tensor_tensor(
                            out=hn[:, half * 512:(half + 1) * 512], in0=fb['h'][:, sub, half * 512:(half + 1) * 512],
                            in1=gsb[:], op=ALU.add))
                    if final_gain_src is None:
                        sc.dma(STORE_Q, h_dst[r0:r0 + 128, :], hn[:], ['hn'], [])
                    else:
                        ss, junk = fb['ss'], fb['junk']
                        self.rstd('hn', hn[:], junk, ss)
                        sc.op('dve', ['hn', 'ss', 'fgain'], ['ho'], lambda e: e.scalar_tensor_tensor(
                            out=ho[:], in0=hn[:], scalar=ss[:, 2:3], in1=fb['fgain'][:], op0=ALU.mult, op1=ALU.mult))
                        sc.dma(STORE_Q, h_dst[r0:r0 + 128, :], ho[:], ['ho'], [])
            sc.barrier()


def rep128(v):
    return np.ascontiguousarray(np.broadcast_to(np.asarray(v, np.float32)[None, :], (128, v.shape[-1])))


def build_program(S, debug=False, ext_ot=False, layers=DEPTH, skip_mixers=False, mixers=('nsa', 'hg', 'ret', 'rw'), dense=True):
    mk = MK(S)
    nc = mk.nc
    if debug:
        mk.dscr = lambda name, shape, dt=F32: nc.dram_tensor(name, list(shape), dt, kind="ExternalOutput").ap()
    x = mk.din("x", [S, D])
    p = mk.din("p", [DEPTH, S, 256])
    out = nc.dram_tensor("out", [S, D], F32, kind="ExternalOutput").ap()
    w = {}
    for L in range(layers):
        w[L] = dict(
            w1=mk.din("w1_%d" % L, [D, P1_NC]), g_mix=mk.din("g_mix_%d" % L, [128, D]),
            wg=mk.din("wg_%d" % L, [4, D, D]), wb=mk.din("wb_%d" % L, [4, 256, D]), wo=mk.din("wo_%d" % L, [D, D]),
            bg=mk.din("bg_%d" % L, [128, 32]),
            g_ffn=mk.din("g_ffn_%d" % L, [128, D]), wfg=mk.din("wfg_%d" % L, [D, DFF]),
            wfu=mk.din("wfu_%d" % L, [D, DFF]), wfd=mk.din("wfd_%d" % L, [DFF, D]),
            g_ple=mk.din("g_ple_%d" % L, [128, D]), wpg=mk.din("wpg_%d" % L, [D, D]), wpp=mk.din("wpp_%d" % L, [256, D]),
        )
    g_final = mk.din("g_final", [128, D])
    lbz = mk.din("hg_lbz", [128, DEPTH * 2])
    for L in range(layers):
        w[L].update(hgn=mk.din("hg_gn_%d" % L, [128, 2]))
        w[L].update(retgb=mk.din("ret_gb_%d" % L, [128, 4]))
        w[L].update(ns_w1k=mk.din("ns_w1k_%d" % L, [64, 32 * 128]), ns_w1v=mk.din("ns_w1v_%d" % L, [64, 32 * 128]),
                    ns_w2k=mk.din("ns_w2k_%d" % L, [128, 64]), ns_w2v=mk.din("ns_w2v_%d" % L, [128, 64]),
                    ns_pk=mk.din("ns_pk_%d" % L, [64, 32]), ns_pv=mk.din("ns_pv_%d" % L, [64, 32]))
        w[L].update(rw_wup=mk.din("rw_wup_%d" % L, [64, 256]), rw_aup=mk.din("rw_aup_%d" % L, [64, 256]),
                    rw_gup=mk.din("rw_gup_%d" % L, [128, 256]), rw_pvec=mk.din("rw_pvec_%d" % L, [128, 24]))
    UC = mk.dscr("UC", [NG, 128, S])
    UT = mk.dscr("UT", [S, P1_NTOK])
    if ext_ot:
        OT = mk.din("OT", [4, 2, 128, S], BF16)
    else:
        OT = mk.dscr("OT", [4, 2, 128, S], BF16)
    hA = mk.dscr("hA", [S, D])
    hB = mk.dscr("hB", [S, D])
    hC = mk.dscr("hC", [S, D])
    with ExitStack() as es:
        sc = Sched(nc, es)
        dn = Dense(mk, sc, es)
        gla = GLA(dn, es)
        rwk = RWKV(dn, gla, es)
        nsa = NSA(dn, gla, es)
        sc.barrier()
        h_in = x
        for L in range(layers):
            wl = w[L]
            dn.pass_p1(L, h_in, wl['w1'], wl['g_mix'], UC, UT)
            if not skip_mixers:
                with ExitStack() as esx:
                    streams = []
                    if 'nsa' in mixers:
                        streams.append(('nsa', nsa.run(L, UC, UT, OT, {
                            'w1r': [wl['ns_w1k'], wl['ns_w1v']], 'w2': [wl['ns_w2k'][:, :], wl['ns_w2v'][:, :]],
                            'posT': [wl['ns_pk'][:, :], wl['ns_pv'][:, :]]}, esx)))
                    if 'rw' in mixers:
                        streams.append(('rw', rwk.run(L, UC, OT, {'w_up': wl['rw_wup'][:, :], 'a_up': wl['rw_aup'][:, :],
                                                                  'g_up': wl['rw_gup'][:, :], 'pvec': wl['rw_pvec'][:, :]}, esx)))
                    sc.run_streams(streams)
                    sc.barrier()
                with ExitStack() as esy:
                    streams = []
                    if 'hg' in mixers:
                        streams.append(('hg', gla.run_hgrn(L, UC, UT, OT, lbz[:, :], wl['hgn'][:, :], esy)))
                    if 'ret' in mixers:
                        streams.append(('ret', gla.run_ret(L, UC, UT, OT, wl['retgb'][:, :], esy)))
                    sc.run_streams(streams)
                    sc.barrier()
            if not dense:
                continue
            dn.pass_merge(L, h_in, hA, OT, wl['wg'], wl['wb'], wl['wo'], wl['bg'], wl['g_mix'])
            dn.pass_ffn(L, hA, hB, wl['wfg'], wl['wfu'], wl['wfd'], wl['g_ffn'])
            last = (L == layers - 1)
            dn.pass_ple(L, hB, out if last else hC, p[L], wl['wpg'], wl['wpp'], wl['g_ple'], g_final if last else None)
            h_in = hC
        sc.barrier()
    mk.n_inst = sc.n_inst
    return mk


def host_inputs_shared(inp, layers=DEPTH, S=8192):
    d = {"c_ident": np.eye(128, dtype=np.float32)}
    for L in range(layers):
        d["w1_%d" % L] = np.ascontiguousarray(inp['w_in'][L][:, P1_COLS])
        d["g_mix_%d" % L] = rep128(inp['norm_mix'][L])
        d["wg_%d" % L] = np.ascontiguousarray(inp['w_gate'][L])
        d["wb_%d" % L] = np.ascontiguousarray(inp['w_branch'][L])
        d["wo_%d" % L] = np.ascontiguousarray(inp['w_out'][L])
        d["bg_%d" % L] = np.ascontiguousarray(inp['b_gate'][L].reshape(4, 8, 128).transpose(2, 0, 1).reshape(128, 32))
        d["g_ffn_%d" % L] = rep128(inp['norm_ffn'][L])
        d["wfg_%d" % L] = np.ascontiguousarray(inp['w_ffn_gate'][L])
        d["wfu_%d" % L] = np.ascontiguousarray(inp['w_ffn_up'][L])
        d["wfd_%d" % L] = np.ascontiguousarray(inp['w_ffn_down'][L])
        d["g_ple_%d" % L] = rep128(inp['norm_ple'][L])
        d["wpg_%d" % L] = np.ascontiguousarray(inp['w_ple_gate'][L])
        d["wpp_%d" % L] = np.ascontiguousarray(inp['w_ple_proj'][L])
    d["g_final"] = rep128(inp['norm_final'])
    d.update(gla_consts())
    d["c_cos"], d["c_sin"] = rope_tables(S)
    d.update(rwkv_consts())
    d.update(nsa_consts(S))
    for L in range(layers):
        r1 = lambda w: np.ascontiguousarray(w.reshape(32, 64, 128).transpose(1, 0, 2).reshape(64, 32 * 128))
        d["ns_w1k_%d" % L] = r1(inp['nsa_cmp_k1'][L])
        d["ns_w1v_%d" % L] = r1(inp['nsa_cmp_v1'][L])
        d["ns_w2k_%d" % L] = np.ascontiguousarray(inp['nsa_cmp_k2'][L])
        d["ns_w2v_%d" % L] = np.ascontiguousarray(inp['nsa_cmp_v2'][L])
        d["ns_pk_%d" % L] = np.ascontiguousarray(inp['nsa_pos_k'][L].T)
        d["ns_pv_%d" % L] = np.ascontiguousarray(inp['nsa_pos_v'][L].T)
    for L in range(layers):
        c2 = lambda v: v.reshape(2, 128).T
        pvec = np.zeros((128, 24), np.float32)
        pvec[:, 0:8] = inp['rwkv_mu'][L].reshape(8, 128).T
        for j, nm in enumerate(['rwkv_w0', 'rwkv_a0', 'rwkv_k_k', 'rwkv_k_a', 'rwkv_r_k', 'rwkv_norm_g', 'rwkv_norm_b']):
            pvec[:, 8 + 2 * j:10 + 2 * j] = c2(inp[nm][L])
        d["rw_pvec_%d" % L] = pvec
        d["rw_wup_%d" % L] = np.ascontiguousarray(inp['rwkv_w_up'][L])
        d["rw_aup_%d" % L] = np.ascontiguousarray(inp['rwkv_a_up'][L])
        d["rw_gup_%d" % L] = np.ascontiguousarray(inp['rwkv_g_up'][L])
    d["hg_lbz"] = np.ascontiguousarray(inp['hgrn_lb_logits'].reshape(DEPTH, 2, 128).transpose(2, 0, 1).reshape(128, DEPTH * 2))
    for L in range(layers):
        d["hg_gn_%d" % L] = np.ascontiguousarray(inp['hgrn_norm'][L].reshape(2, 128).T)
        d["ret_gb_%d" % L] = np.ascontiguousarray(np.concatenate(
            [inp['ret_norm_g'][L].reshape(2, 128).T, inp['ret_norm_b'][L].reshape(2, 128).T], axis=1))
    return d


GLA_C = 32
RET_LOGG = [float(np.log(1.0 - 2.0 ** (-5.0 - h))) for h in range(4)]


def gla_consts():
    s = np.arange(128)
    m = ((s[:, None] // 32 == s[None, :] // 32) & (s[:, None] <= s[None, :])).astype(np.float32)
    d = {"c_gmask": np.ascontiguousarray(np.tile(m, (1, 4)))}
    rm = np.ones((128, 128), np.float32)
    rm[:, ::32] = 0.0
    d["c_rmask"] = rm
    bd = np.zeros((128, 128), np.float32)
    bd[:64, :64] = 1.0
    bd[64:, 64:] = 1.0
    d["c_onesbd"] = bd
    i = (np.arange(128) % 32).astype(np.float64)
    ret = np.zeros((2, 3, 128, 128), np.float32)
    for hp in range(2):
        for hl in range(2):
            lg = RET_LOGG[hp * 2 + hl]
            ret[hp, 0, hl * 64:(hl + 1) * 64, :] = np.exp((i + 1) * lg)[None, :]
            ret[hp, 1, hl * 64:(hl + 1) * 64, :] = 0.125 * np.exp(-(i + 1) * lg)[None, :]
            ret[hp, 2, hl * 64:(hl + 1) * 64, :] = 0.125 * np.exp((31 - i) * lg)[None, :]
    d["c_retdec"] = ret
    g32 = np.zeros((128, 2), np.float32)
    for hp in range(2):
        for hl in range(2):
            g32[hl * 64:(hl + 1) * 64, hp] = np.exp(32 * RET_LOGG[hp * 2 + hl])
    d["c_retg32"] = g32
    return d


def rope_tables(S):
    pos = np.arange(S, dtype=np.float32)
    inv = (10000.0 ** (-np.arange(0, 64, 2, dtype=np.float32) / 64)).astype(np.float32)
    ang = pos[None, :] * inv[:, None]
    c = np.cos(ang).astype(np.float32)
    s_ = np.sin(ang).astype(np.float32)
    cos64 = np.concatenate([c, c], 0)
    sin64 = np.concatenate([-s_, s_], 0)
    return (np.ascontiguousarray(np.concatenate([cos64, cos64], 0)),
            np.ascontiguousarray(np.concatenate([sin64, sin64], 0)))


class GLA:
    def __init__(self, dn, es):
        self.dn, self.mk, self.sc, self.nc = dn, dn.mk, dn.sc, dn.nc
        mk, sc = self.mk, self.sc
        self.gmask = mk.sb(es, "gmask", [128, 512], F32)
        self.rmask = mk.sb(es, "rmask", [128, 128], F32)
        self.onesbd = mk.sb(es, "onesbd", [128, 128], BF16)
        tmpf = mk.sb(es, "g_tmpf", [128, 128], F32)
        sc.dma('sp', self.gmask[:], mk.din("c_gmask", [128, 512])[:, :], [], ['gmask'])
        sc.dma('sp', self.rmask[:], mk.din("c_rmask", [128, 128])[:, :], [], ['rmask'])
        sc.dma('sp', tmpf[:], mk.din("c_onesbd", [128, 128])[:, :], [], ['g_tmpf'])
        sc.op('dve', ['g_tmpf'], ['onesbd'], lambda e: e.tensor_copy(out=self.onesbd[:], in_=tmpf[:]))
        self.c_retdec = mk.din("c_retdec", [2, 3, 128, 128])
        self.c_retg32 = mk.din("c_retg32", [128, 2])
        self.c_cos = mk.din("c_cos", [128, mk.S])
        self.c_sin = mk.din("c_sin", [128, mk.S])

    def alloc_core(self, es):
        mk = self.mk
        b = {}
        b['PT'] = mk.sb(es, "gl_PT", [128, 512], BF16)
        b['stf'] = [mk.sb(es, "gl_stf%d" % hp, [128, 128], F32) for hp in range(2)]
        b['snap'] = [[mk.sb(es, "gl_snap%d_%d" % (par, hp), [128, 5, 128], BF16) for hp in range(2)] for par in range(2)]
        b['osb'] = mk.sb(es, "gl_osb", [128, 256], F32)
        for hp in range(2):
            self.sc.op('dve', [], [('stf', hp)], lambda e, hp=hp: e.memset(b['stf'][hp][:], 0.0))
            self.sc.op('pool', [], [('snap', 1, hp, 4)], lambda e, hp=hp: e.memset(b['snap'][1][hp][:, 4, :], 0.0))
        return b

    def core(self, b, ti, qs, ks, kd, kd3, v, dec, keys):
        sc, dn = self.sc, self.dn
        par = ti % 2
        pA = dn.next_ps()
        A = dn.ps[pA]
        for h in range(4):
            hp, hl = h // 2, h % 2
            sl = slice(hl * 64, (hl + 1) * 64)
            sc.op('pes', [keys['qs'][hp], keys['ks'][hp]], [('ps', pA)], lambda e, h=h, hp=hp, sl=sl: e.matmul(
                A[:, h * 128:(h + 1) * 128], lhsT=ks[hp][sl, :], rhs=qs[hp][sl, :], start=True, stop=True))
        sc.op('dve', [('ps', pA), 'gmask'], ['gl_PT'], lambda e: e.tensor_tensor(
            out=b['PT'][:], in0=A[:, :], in1=self.gmask[:], op=ALU.mult))
        pK = [dn.next_ps(), dn.next_ps()]
        for hp in range(2):
            for c in range(4):
                K = dn.ps[pK[hp]]
                rsl = slice(c * 32, (c + 1) * 32) if c < 3 else slice(64, 128)
                kk_ = kd if c < 3 else kd3
                sc.op('pes', [keys['kd'], keys['v']], [('ps', pK[hp])], lambda e, hp=hp, c=c, K=K, rsl=rsl, kk_=kk_: e.matmul(
                    K[:, c * 128:(c + 1) * 128], lhsT=kk_[rsl, hp * 128:(hp + 1) * 128],
                    rhs=v[rsl, hp * 128:(hp + 1) * 128], start=True, stop=True))
        for c in range(4):
            for hp in range(2):
                K = dn.ps[pK[hp]]
                sc.op('dve', [('ps', pK[hp]), ('stf', hp), keys['dec'][hp]], [('stf', hp)],
                      lambda e, hp=hp, c=c, K=K: e.scalar_tensor_tensor(
                          out=b['stf'][hp][:], in0=b['stf'][hp][:], scalar=dec(hp, c),
                          in1=K[:, c * 128:(c + 1) * 128], op0=ALU.mult, op1=ALU.add))
                sc.op('act', [('stf', hp)], [('snap', par, hp, c + 1)], lambda e, hp=hp, c=c: e.activation(
                    out=b['snap'][par][hp][:, c + 1, :], in_=b['stf'][hp][:], func=AF.Copy))
        pB = dn.next_ps()
        B = dn.ps[pB]
        for h in range(4):
            hp, hl = h // 2, h % 2
            sl = slice(hl * 64, (hl + 1) * 64)
            sc.op('pes', ['gl_PT', keys['v']], [('ps', pB)], lambda e, h=h, hp=hp, sl=sl: e.matmul(
                B[sl, hp * 128:(hp + 1) * 128], lhsT=v[:, h * 64:(h + 1) * 64], rhs=b['PT'][:, h * 128:(h + 1) * 128],
                start=True, stop=False))
            for c in range(4):
                if c == 0:
                    st = b['snap'][1 - par][hp][sl, 4, hl * 64:(hl + 1) * 64]
                    skey = ('snap', 1 - par, hp, 4)
                else:
                    st = b['snap'][par][hp][sl, c, hl * 64:(hl + 1) * 64]
                    skey = ('snap', par, hp, c)
                sc.op('pes', [skey, keys['qs'][hp]], [('ps', pB)], lambda e, hp=hp, sl=sl, c=c, st=st: e.matmul(
                    B[sl, hp * 128 + c * 32:hp * 128 + (c + 1) * 32], lhsT=st, rhs=qs[hp][sl, c * 32:(c + 1) * 32],
                    start=False, stop=(c == 3)))
        sc.op('act', [('ps', pB)], ['gl_osb'], lambda e: e.activation(out=b['osb'][:], in_=B[:, 0:256], func=AF.Copy))

    def run_hgrn(self, L, UC, UT, OT, lbz_src, hgn_src, es_ext):
        sc, mk, dn, S = self.sc, self.mk, self.dn, self.mk.S
        with _Keep(es_ext) as es:
            b = self.alloc_core(es)
            lbz = mk.sb(es, "hg_lbz", [128, DEPTH * 2], F32)
            lbe = mk.sb(es, "hg_lbe", [128, DEPTH * 2], F32)
            lbs = mk.sb(es, "hg_lbs", [128, 8], F32)
            gn = mk.sb(es, "hg_gn", [128, 2], F32)
            sc.dma('sp', lbz[:], lbz_src, [], ['lbz'])
            sc.dma('sp', gn[:], hgn_src, [], ['hg_gn'])
            sc.op('act', ['lbz'], ['lbe'], lambda e: e.activation(out=lbe[:], in_=lbz[:], func=AF.Exp))
            sc.op('dve', ['lbe'], ['lbs'], lambda e: e.tensor_tensor(
                out=lbs[:, 0:2], in0=lbe[:, 0:2], in1=lbe[:, 2:4], op=ALU.add))
            for l in range(2, DEPTH):
                sc.op('dve', ['lbe', 'lbs'], ['lbs'], lambda e, l=l: e.tensor_tensor(
                    out=lbs[:, 0:2], in0=lbs[:, 0:2], in1=lbe[:, 2 * l:2 * l + 2], op=ALU.add))
            sc.op('dve', ['lbs'], ['lbs'], lambda e: e.reciprocal(out=lbs[:, 2:4], in_=lbs[:, 0:2]))
            sc.op('dve', [], ['lbs'], lambda e: e.memset(lbs[:, 4:6], 0.0))
            for l in range(1, L + 1):
                sc.op('dve', ['lbs', 'lbe'], ['lbs'], lambda e, l=l: e.tensor_tensor(
                    out=lbs[:, 6:8], in0=lbe[:, 2 * l:2 * l + 2], in1=lbs[:, 2:4], op=ALU.mult))
                sc.op('dve', ['lbs'], ['lbs'], lambda e: e.tensor_tensor(
                    out=lbs[:, 4:6], in0=lbs[:, 4:6], in1=lbs[:, 6:8], op=ALU.add))
            sc.op('dve', ['lbs'], ['lbs'], lambda e: e.tensor_scalar(
                out=lbs[:, 6:8], in0=lbs[:, 4:6], scalar1=-1.0, scalar2=1.0, op0=ALU.mult, op1=ALU.add))
            TT = 512
            inq = [mk.sb(es, "hg_q%d" % hp, [128, TT], F32) for hp in range(2)]
            inz = [mk.sb(es, "hg_z%d" % hp, [128, TT], F32) for hp in range(2)]
            ing = [mk.sb(es, "hg_g%d" % hp, [128, TT], F32) for hp in range(2)]
            vin = mk.sb(es, "hg_vin", [128, 4, 256], F32)
            vb = mk.sb(es, "hg_vb", [128, 256], BF16)
            fT = mk.sb(es, "hg_fT", [128, 128], F32)
            kT = mk.sb(es, "hg_kT", [128, 128], F32)
            lf = mk.sb(es, "hg_lf", [128, 128], F32)
            bT = mk.sb(es, "hg_bT", [128, 128], F32)
            eb = [mk.sb(es, "hg_eb%d" % hp, [128, 128], F32) for hp in range(2)]
            enb = mk.sb(es, "hg_enb", [128, 128], F32)
            sq = mk.sb(es, "hg_sq", [128, 128], F32)
            ksf = mk.sb(es, "hg_ksf", [128, 128], F32)
            qs = [mk.sb(es, "hg_qs%d" % hp, [128, 128], BF16) for hp in range(2)]
            ks = [mk.sb(es, "hg_ks%d" % hp, [128, 128], BF16) for hp in range(2)]
            kdT = mk.sb(es, "hg_kdT", [128, 128], BF16)
            kd = mk.sb(es, "hg_kd", [128, 256], BF16)
            kd3 = mk.sb(es, "hg_kd3", [128, 256], BF16)
            sc.op('pool', [], ['kd'], lambda e: e.memset(kd3[:], 0.0))
            osq = mk.sb(es, "hg_osq", [128, 256], BF16)
            rs = mk.sb(es, "hg_rs", [128, 256], F32)
            sg = mk.sb(es, "hg_sg", [128, 256], F32)
            ob = mk.sb(es, "hg_ob", [128, 256], BF16)
            pbk = dn.pb[1]
            for t0 in range(0, S, TT):
                for hp in range(2):
                    sc.dma('sp', inq[hp][:], UC[GIDX['hg_q%d' % hp], :, t0:t0 + TT], [], [('inq', hp)])
                    sc.dma('sp', inz[hp][:], UC[GIDX['hg_f%d' % hp], :, t0:t0 + TT], [], [('inz', hp)])
                    sc.dma('sp', ing[hp][:], UC[GIDX['hg_og%d' % hp], :, t0:t0 + TT], [], [('ing', hp)])
                sc.dma('sp', vin[:], UT[t0:t0 + TT, 128:384].rearrange("(n p) c -> p n c", p=128), [], ['vin'])
                for tl in range(TT // 128):
                    ti = t0 // 128 + tl
                    ts = slice(tl * 128, (tl + 1) * 128)
                    sc.op('pool', ['vin'], ['vb'], lambda e, tl=tl: e.tensor_copy(out=vb[:], in_=vin[:, tl, :]))
                    for hp in range(2):
                        sc.op('act', [('inz', hp)], ['fT'], lambda e, hp=hp, ts=ts: e.activation(
                            out=fT[:], in_=inz[hp][:, ts], func=AF.Exp, scale=-1.0))
                        sc.op('pool', ['fT'], ['fT'], lambda e: e.tensor_scalar(
                            out=fT[:], in0=fT[:], scalar1=1.0, scalar2=None, op0=ALU.add))
                        sc.op('dve', ['fT'], ['fT'], lambda e: e.reciprocal(out=fT[:], in_=fT[:]))
                        sc.op('dve', ['fT', 'lbs'], ['fT'], lambda e, hp=hp: e.tensor_scalar(
                            out=fT[:], in0=fT[:], scalar1=lbs[:, 6 + hp:7 + hp], scalar2=lbs[:, 4 + hp:5 + hp],
                            op0=ALU.mult, op1=ALU.add))
                        sc.op('pool', ['fT'], ['kT'], lambda e: e.tensor_scalar(
                            out=kT[:], in0=fT[:], scalar1=-1.0, scalar2=1.0, op0=ALU.mult, op1=ALU.add))
                        sc.op('dve', ['fT'], ['lf'], lambda e: e.tensor_scalar(
                            out=lf[:], in0=fT[:], scalar1=1e-20, scalar2=None, op0=ALU.max))
                        sc.op('act', ['lf'], ['lf'], lambda e: e.activation(out=lf[:], in_=lf[:], func=AF.Ln))
                        sc.op('dve', ['lf', 'rmask'], ['bT'], lambda e: e.tensor_tensor_scan(
                            out=bT[:], data0=self.rmask[:], data1=lf[:], initial=0.0, op0=ALU.mult, op1=ALU.add))
                        sc.op('act', ['bT'], [('eb', hp)], lambda e, hp=hp: e.activation(out=eb[hp][:], in_=bT[:], func=AF.Exp))
                        sc.op('act', ['bT'], ['enb'], lambda e: e.activation(out=enb[:], in_=bT[:], func=AF.Exp, scale=-1.0))
                        sc.op('act', [('inq', hp)], ['sq'], lambda e, hp=hp, ts=ts: e.activation(
                            out=sq[:], in_=inq[hp][:, ts], func=AF.Exp, scale=-1.0))
                        sc.op('pool', ['sq'], ['sq'], lambda e: e.tensor_scalar(
                            out=sq[:], in0=sq[:], scalar1=1.0, scalar2=None, op0=ALU.add))
                        sc.op('dve', ['sq'], ['sq'], lambda e: e.reciprocal(out=sq[:], in_=sq[:]))
                        sc.op('pool', ['sq', ('inq', hp)], ['sq'], lambda e, hp=hp, ts=ts: e.tensor_tensor(
                            out=sq[:], in0=sq[:], in1=inq[hp][:, ts], op=ALU.mult))
                        sc.op('dve', ['sq', ('eb', hp)], [('qs', hp)], lambda e, hp=hp: e.tensor_tensor(
                            out=qs[hp][:], in0=sq[:], in1=eb[hp][:], op=ALU.mult))
                        sc.op('dve', ['kT', 'enb'], ['ksf'], lambda e: e.tensor_tensor(
                            out=ksf[:], in0=kT[:], in1=enb[:], op=ALU.mult))
                        sc.op('pool', ['ksf'], [('ks', hp)], lambda e, hp=hp: e.tensor_copy(out=ks[hp][:], in_=ksf[:]))
                        sc.op('dve', ['ksf', ('eb', hp)], ['kdT'], lambda e, hp=hp: e.tensor_tensor(
                            out=kdT[:].rearrange("p (c i) -> p c i", i=32),
                            in0=ksf[:].rearrange("p (c i) -> p c i", i=32),
                            in1=eb[hp][:].rearrange("p (c i) -> p c i", i=32)[:, :, 31:32].broadcast_to((128, 4, 32)),
                            op=ALU.mult))
                        sc.op('pe', ['kdT', 'ident'], ['pb1'], lambda e: e.transpose(
                            out=pbk[:, 0:128], in_=kdT[:], identity=dn.ident[:]))
                        sc.op('act', ['pb1'], ['kd'], lambda e, hp=hp: e.activation(
                            out=kd[:, hp * 128:(hp + 1) * 128], in_=pbk[:, 0:128], func=AF.Copy))
                        sc.op('dve', ['pb1'], ['kd'], lambda e, hp=hp: e.tensor_copy(
                            out=kd3[96:128, hp * 128:(hp + 1) * 128], in_=pbk[96:128, 0:128]))
                    yield
                    self.core(b, ti, [q[:] for q in qs], [k[:] for k in ks], kd[:], kd3[:], vb[:],
                              lambda hp, c: eb[hp][:, c * 32 + 31:c * 32 + 32],
                              {'qs': [('qs', 0), ('qs', 1)], 'ks': [('ks', 0), ('ks', 1)], 'kd': 'kd', 'v': 'vb', 'dec': [('eb', 0), ('eb', 1)]})
                    yield
                    osb = b['osb']
                    sc.op('act', ['gl_osb'], ['osq'], lambda e: e.activation(out=osq[:], in_=osb[:], func=AF.Square))
                    pS = dn.next_ps()
                    sc.op('pe', ['osq', 'onesbd'], [('ps', pS)], lambda e, pS=pS: e.matmul(
                        dn.ps[pS][:, 0:256], lhsT=self.onesbd[:], rhs=osq[:], start=True, stop=True))
                    sc.op('dve', [('ps', pS)], ['rs'], lambda e, pS=pS: e.tensor_scalar(
                        out=rs[:], in0=dn.ps[pS][:, 0:256], scalar1=1.0 / 64, scalar2=EPS, op0=ALU.mult, op1=ALU.add))
                    sc.op('act', ['rs'], ['rs'], lambda e: e.activation(out=rs[:], in_=rs[:], func=AF.Ln))
                    sc.op('act', ['rs'], ['rs'], lambda e: e.activation(out=rs[:], in_=rs[:], func=AF.Exp, scale=-0.5))
                    for hp in range(2):
                        sc.op('act', [('ing', hp)], ['sg'], lambda e, hp=hp, ts=ts: e.activation(
                            out=sg[:, hp * 128:(hp + 1) * 128], in_=ing[hp][:, ts], func=AF.Exp, scale=-1.0))
                        sc.op('pool', ['sg'], ['sg'], lambda e, hp=hp: e.tensor_scalar(
                            out=sg[:, hp * 128:(hp + 1) * 128], in0=sg[:, hp * 128:(hp + 1) * 128],
                            scalar1=1.0, scalar2=None, op0=ALU.add))
                        sc.op('dve', ['sg'], ['sg'], lambda e, hp=hp: e.reciprocal(
                            out=sg[:, hp * 128:(hp + 1) * 128], in_=sg[:, hp * 128:(hp + 1) * 128]))
                        sc.op('pool', ['sg', ('ing', hp)], ['sg'], lambda e, hp=hp, ts=ts: e.tensor_tensor(
                            out=sg[:, hp * 128:(hp + 1) * 128], in0=sg[:, hp * 128:(hp + 1) * 128],
                            in1=ing[hp][:, ts], op=ALU.mult))
                        sc.op('dve', ['rs', 'gl_osb', 'hg_gn'], ['rs'], lambda e, hp=hp: e.scalar_tensor_tensor(
                            out=rs[:, hp * 128:(hp + 1) * 128], in0=rs[:, hp * 128:(hp + 1) * 128],
                            scalar=gn[:, hp:hp + 1], in1=osb[:, hp * 128:(hp + 1) * 128], op0=ALU.mult, op1=ALU.mult))
                    sc.op('dve', ['rs', 'sg'], ['ob'], lambda e: e.tensor_tensor(out=ob[:], in0=rs[:], in1=sg[:], op=ALU.mult))
                    tg = t0 + tl * 128
                    sc.dma(STORE_Q, OT[1, :, :, tg:tg + 128].rearrange("c p t -> p c t"),
                           ob[:].rearrange("p (c t) -> p c t", c=2), ['ob'], [])
                    yield


def _run_ret(self, L, UC, UT, OT, gb_src, es_ext):
    sc, mk, dn, S = self.sc, self.mk, self.dn, self.mk.S
    with _Keep(es_ext) as es:
        b = self.alloc_core(es)
        gb = mk.sb(es, "rt_gb", [128, 4], F32)
        g32 = mk.sb(es, "rt_g32", [128, 2], F32)
        cdec = mk.sb(es, "rt_cdec", [128, 6, 128], F32)
        sc.dma('sp', gb[:], gb_src, [], ['rt_gb'])
        sc.dma('sp', g32[:], self.c_retg32[:, :], [], ['rt_g32'])
        sc.dma('sp', cdec[:], self.c_retdec.rearrange("a b p t -> p (a b) t"), [], ['rt_cdec'])
        TT = 512
        names = ['q', 'qr', 'k', 'kr', 'g']
        inb = {n: [mk.sb(es, "rt_%s%d" % (n, hp), [128, TT], F32) for hp in range(2)] for n in names}
        cs = mk.sb(es, "rt_cos", [128, TT], F32)
        sn = mk.sb(es, "rt_sin", [128, TT], F32)
        vin = mk.sb(es, "rt_vin", [128, 4, 256], F32)
        vb = mk.sb(es, "rt_vb", [128, 256], BF16)
        t1 = mk.sb(es, "rt_t1", [128, 128], F32)
        t2 = mk.sb(es, "rt_t2", [128, 128], F32)
        qro = mk.sb(es, "rt_qro", [128, 128], F32)
        kro = mk.sb(es, "rt_kro", [128, 128], F32)
        qs = [mk.sb(es, "rt_qs%d" % hp, [128, 128], BF16) for hp in range(2)]
        ks = [mk.sb(es, "rt_ks%d" % hp, [128, 128], BF16) for hp in range(2)]
        kdT = mk.sb(es, "rt_kdT", [128, 128], BF16)
        kd = mk.sb(es, "rt_kd", [128, 256], BF16)
        kd3 = mk.sb(es, "rt_kd3", [128, 256], BF16)
        sc.op('pool', [], ['kd'], lambda e: e.memset(kd3[:], 0.0))
        o16 = mk.sb(es, "rt_o16", [128, 256], BF16)
        cen = mk.sb(es, "rt_cen", [128, 256], F32)
        sq = mk.sb(es, "rt_sq", [128, 256], BF16)
        rs = mk.sb(es, "rt_rs", [128, 256], F32)
        sg = mk.sb(es, "rt_sg", [128, 256], F32)
        ob = mk.sb(es, "rt_ob", [128, 256], BF16)
        pbk = dn.pb[1]
        for t0 in range(0, S, TT):
            for hp in range(2):
                for n in names:
                    sc.dma('sp', inb[n][hp][:], UC[GIDX['ret_%s%d' % (n, hp)], :, t0:t0 + TT], [], [('rin', n, hp)])
            sc.dma('sp', cs[:], self.c_cos[:, t0:t0 + TT], [], ['rt_cos'])
            sc.dma('sp', sn[:], self.c_sin[:, t0:t0 + TT], [], ['rt_sin'])
            sc.dma('sp', vin[:], UT[t0:t0 + TT, 384:640].rearrange("(n p) c -> p n c", p=128), [], ['vin'])
            for tl in range(TT // 128):
                ti = t0 // 128 + tl
                ts = slice(tl * 128, (tl + 1) * 128)
                sc.op('pool', ['vin'], ['vb'], lambda e, tl=tl: e.tensor_copy(out=vb[:], in_=vin[:, tl, :]))
                for hp in range(2):
                    for (a, ar, dst, dkey) in (('q', 'qr', qro, 'qro'), ('k', 'kr', kro, 'kro')):
                        sc.op('dve', [('rin', a, hp), 'rt_cos'], ['t1'], lambda e, a=a, hp=hp, ts=ts: e.tensor_tensor(
                            out=t1[:], in0=inb[a][hp][:, ts], in1=cs[:, ts], op=ALU.mult))
                        sc.op('pool', [('rin', ar, hp), 'rt_sin'], ['t2'], lambda e, ar=ar, hp=hp, ts=ts: e.tensor_tensor(
                            out=t2[:], in0=inb[ar][hp][:, ts], in1=sn[:, ts], op=ALU.mult))
                        sc.op('dve', ['t1', 't2'], [dkey], lambda e, dst=dst: e.tensor_tensor(
                            out=dst[:], in0=t1[:], in1=t2[:], op=ALU.add))
                    sc.op('dve', ['qro', 'rt_cdec'], [('qs', hp)], lambda e, hp=hp: e.tensor_tensor(
                        out=qs[hp][:], in0=qro[:], in1=cdec[:, hp * 3 + 0, :], op=ALU.mult))
                    sc.op('pool', ['kro', 'rt_cdec'], [('ks', hp)], lambda e, hp=hp: e.tensor_tensor(
                        out=ks[hp][:], in0=kro[:], in1=cdec[:, hp * 3 + 1, :], op=ALU.mult))
                    sc.op('dve', ['kro', 'rt_cdec'], ['kdT'], lambda e, hp=hp: e.tensor_tensor(
                        out=kdT[:], in0=kro[:], in1=cdec[:, hp * 3 + 2, :], op=ALU.mult))
                    sc.op('pe', ['kdT', 'ident'], ['pb1'], lambda e: e.transpose(
                        out=pbk[:, 0:128], in_=kdT[:], identity=dn.ident[:]))
                    sc.op('act', ['pb1'], ['kd'], lambda e, hp=hp: e.activation(
                        out=kd[:, hp * 128:(hp + 1) * 128], in_=pbk[:, 0:128], func=AF.Copy))
                    sc.op('dve', ['pb1'], ['kd'], lambda e, hp=hp: e.tensor_copy(
                        out=kd3[96:128, hp * 128:(hp + 1) * 128], in_=pbk[96:128, 0:128]))
                yield
                self.core(b, ti, [q[:] for q in qs], [k[:] for k in ks], kd[:], kd3[:], vb[:],
                          lambda hp, c: g32[:, hp:hp + 1],
                          {'qs': [('qs', 0), ('qs', 1)], 'ks': [('ks', 0), ('ks', 1)], 'kd': 'kd', 'v': 'vb',
                           'dec': ['rt_g32', 'rt_g32']})
                yield
                osb = b['osb']
                sc.op('pool', ['gl_osb'], ['o16'], lambda e: e.tensor_copy(out=o16[:], in_=osb[:]))
                pM = dn.next_ps()
                sc.op('pe', ['o16', 'onesbd'], [('ps', pM)], lambda e, pM=pM: e.matmul(
                    dn.ps[pM][:, 0:256], lhsT=self.onesbd[:], rhs=o16[:], start=True, stop=True))
                sc.op('dve', [('ps', pM), 'gl_osb'], ['cen'], lambda e, pM=pM: e.scalar_tensor_tensor(
                    out=cen[:], in0=dn.ps[pM][:, 0:256], scalar=-1.0 / 64, in1=osb[:], op0=ALU.mult, op1=ALU.add))
                sc.op('act', ['cen'], ['sq'], lambda e: e.activation(out=sq[:], in_=cen[:], func=AF.Square))
                pV = dn.next_ps()
                sc.op('pe', ['sq', 'onesbd'], [('ps', pV)], lambda e, pV=pV: e.matmul(
                    dn.ps[pV][:, 0:256], lhsT=self.onesbd[:], rhs=sq[:], start=True, stop=True))
                sc.op('dve', [('ps', pV)], ['rs'], lambda e, pV=pV: e.tensor_scalar(
                    out=rs[:], in0=dn.ps[pV][:, 0:256], scalar1=1.0 / 64, scalar2=1e-5, op0=ALU.mult, op1=ALU.add))
                sc.op('act', ['rs'], ['rs'], lambda e: e.activation(out=rs[:], in_=rs[:], func=AF.Ln))
                sc.op('act', ['rs'], ['rs'], lambda e: e.activation(out=rs[:], in_=rs[:], func=AF.Exp, scale=-0.5))
                sc.op('dve', ['rs', 'cen'], ['cen'], lambda e: e.tensor_tensor(out=cen[:], in0=cen[:], in1=rs[:], op=ALU.mult))
                for hp in range(2):
                    sc.op('act', [('rin', 'g', hp)], ['sg'], lambda e, hp=hp, ts=ts: e.activation(
                        out=sg[:, hp * 128:(hp + 1) * 128], in_=inb['g'][hp][:, ts], func=AF.Exp, scale=-1.0))
                    sc.op('pool', ['sg'], ['sg'], lambda e, hp=hp: e.tensor_scalar(
                        out=sg[:, hp * 128:(hp + 1) * 128], in0=sg[:, hp * 128:(hp + 1) * 128],
                        scalar1=1.0, scalar2=None, op0=ALU.add))
                    sc.op('dve', ['sg'], ['sg'], lambda e, hp=hp: e.reciprocal(
                        out=sg[:, hp * 128:(hp + 1) * 128], in_=sg[:, hp * 128:(hp + 1) * 128]))
                    sc.op('pool', ['sg', ('rin', 'g', hp)], ['sg'], lambda e, hp=hp, ts=ts: e.tensor_tensor(
                        out=sg[:, hp * 128:(hp + 1) * 128], in0=sg[:, hp * 128:(hp + 1) * 128],
                        in1=inb['g'][hp][:, ts], op=ALU.mult))
                    sc.op('dve', ['cen', 'rt_gb'], ['cen'], lambda e, hp=hp: e.tensor_scalar(
                        out=cen[:, hp * 128:(hp + 1) * 128], in0=cen[:, hp * 128:(hp + 1) * 128],
                        scalar1=gb[:, hp:hp + 1], scalar2=gb[:, 2 + hp:3 + hp], op0=ALU.mult, op1=ALU.add))
                sc.op('dve', ['cen', 'sg'], ['ob'], lambda e: e.tensor_tensor(out=ob[:], in0=cen[:], in1=sg[:], op=ALU.mult))
                tg = t0 + tl * 128
                sc.dma(STORE_Q, OT[2, :, :, tg:tg + 128].rearrange("c p t -> p c t"),
                       ob[:].rearrange("p (c t) -> p c t", c=2), ['ob'], [])
                yield


GLA.run_ret = _run_ret


RW_C = 64


def rwkv_consts():
    i = np.arange(128)
    same = (i[:, None] // 64) == (i[None, :] // 64)
    su = (same & (i[:, None] % 64 < i[None, :] % 64)).astype(np.float32)
    iu = (same & (i[:, None] % 64 <= i[None, :] % 64)).astype(np.float32)
    d = {"c_rwmask4": np.ascontiguousarray(np.concatenate([su, su, iu, iu], axis=1)),
         "c_rwlmask": np.ascontiguousarray(su.T)}
    rm = np.ones((128, 256), np.float32)
    rm[:, ::64] = 0.0
    d["c_rmask64"] = rm
    return d


class RWKV:
    def __init__(self, dn, gla, es):
        self.dn, self.mk, self.sc, self.gla = dn, dn.mk, dn.sc, gla
        mk, sc = self.mk, self.sc
        self.mask4 = mk.sb(es, "rw_mask4", [128, 512], F32)
        self.lmask = mk.sb(es, "rw_lmask", [128, 128], F32)
        self.rmask = mk.sb(es, "rw_rmask", [128, 256], F32)
        sc.dma('sp', self.mask4[:], mk.din("c_rwmask4", [128, 512])[:, :], [], ['rw_mask4'])
        sc.dma('sp', self.lmask[:], mk.din("c_rwlmask", [128, 128])[:, :], [], ['rw_lmask'])
        sc.dma('sp', self.rmask[:], mk.din("c_rmask64", [128, 256])[:, :], [], ['rw_rmask'])

    def run(self, L, UC, OT, ws, es_ext, banks=(5, 6)):
        sc, mk, dn, S = self.sc, self.mk, self.dn, self.mk.S
        ident = dn.ident
        onesbd = self.gla.onesbd
        bi = [0]

        def nps():
            b_ = banks[bi[0] % len(banks)]
            bi[0] += 1
            return b_
        with _Keep(es_ext) as es:
            TT = min(256, S)
            NCH = TT // 64
            wa_f = mk.sb(es, "rw_waf", [128, 256], F32)
            WA = mk.sb(es, "rw_WA", [128, 256], BF16)
            gu_f = mk.sb(es, "rw_guf", [128, 256], F32)
            GU = mk.sb(es, "rw_GU", [128, 256], BF16)
            sc.dma('sp', wa_f[0:64, :], ws['w_up'], [], ['rw_waf'])
            sc.dma('sp', wa_f[64:128, :], ws['a_up'], [], ['rw_waf'])
            sc.dma('sp', gu_f[:], ws['g_up'], [], ['rw_guf'])
            sc.op('dve', ['rw_waf'], ['rw_WA'], lambda e: e.tensor_copy(out=WA[:], in_=wa_f[:]))
            sc.op('dve', ['rw_guf'], ['rw_GU'], lambda e: e.tensor_copy(out=GU[:], in_=gu_f[:]))
            pv = mk.sb(es, "rw_pv", [128, 28], F32)
            sc.dma('sp', pv[:, 0:24], ws['pvec'], [], ['rw_pv'])
            sc.op('dve', ['rw_pv'], ['rw_pv'], lambda e: e.tensor_scalar(
                out=pv[:, 24:28], in0=pv[:, 8:12], scalar1=-1.0, scalar2=None, op0=ALU.mult))
            sc.op('dve', ['rw_pv'], ['rw_pv'], lambda e: e.tensor_scalar(
                out=pv[:, 22:24], in0=pv[:, 14:16], scalar1=-1.0, scalar2=1.0, op0=ALU.mult, op1=ALU.add))
            raw = [mk.sb(es, "rw_raw%d" % g, [128, TT + 1], F32) for g in range(8)]
            sh = [mk.sb(es, "rw_sh%d" % g, [128, TT], F32) for g in range(8)]
            dtmp = mk.sb(es, "rw_dtmp", [128, TT], F32)
            twa = mk.sb(es, "rw_twa", [128, TT], BF16)
            sgl = mk.sb(es, "rw_sgl", [128, TT], BF16)
            F = lambda n: mk.sb(es, "rw_" + n, [128, TT], F32)
            lw, av, kk, rn, kkn, kp, lG, enG, eGp, t1 = (F(n) for n in
                ('lw', 'av', 'kk', 'rn', 'kkn', 'kp', 'lG', 'enG', 'eGp', 't1'))
            As_f, Ks_f = F('Asf'), F('Ksf')
            sq16 = mk.sb(es, "rw_sq16", [128, TT], BF16)
            y16 = mk.sb(es, "rw_y16", [128, TT], BF16)
            cen = mk.sb(es, "rw_cen", [128, TT], F32)
            rs = mk.sb(es, "rw_rs", [128, TT], F32)
            ob = mk.sb(es, "rw_ob", [128, TT], BF16)
            H = []
            for hp in range(2):
                hb = {}
                for n in ('As', 'Ks', 'Bt', 'Rt', 'Ah', 'Kh'):
                    hb['bd_' + n] = mk.sb(es, "rw_bd_%s%d" % (n, hp), [128, NCH, 128], BF16)
                    sc.op('pool', [], [('bd', n, hp)], lambda e, tl=hb['bd_' + n]: e.memset(tl[:], 0.0))
                for n in ('Bt', 'Rt', 'As', 'v'):
                    hb['r2_' + n] = mk.sb(es, "rw_r2_%s%d" % (n, hp), [128, NCH, 2, 64], BF16)
                hb['eG'] = mk.sb(es, "rw_eG%d" % hp, [128, TT], F32)
                hb['gT'] = mk.sb(es, "rw_gT%d" % hp, [128, TT], F32)
                hb['rkk16'] = mk.sb(es, "rw_rkk%d" % hp, [128, TT], BF16)
                hb['Tf'] = mk.sb(es, "rw_Tf%d" % hp, [128, 128], F32)
                hb['Tb'] = mk.sb(es, "rw_Tb%d" % hp, [128, 128], BF16)
                hb['PQ'] = [mk.sb(es, "rw_PQ%d_%d" % (hp, i), [128, 256], BF16) for i in range(2)]
                hb['NM'] = [mk.sb(es, "rw_NM%d_%d" % (hp, i), [128, 384], BF16) for i in range(2)]
                hb['Z'] = [[mk.sb(es, "rw_Z%d_%d_%d" % (hp, i, j), [128, 128], BF16) for j in range(2)] for i in range(2)]
                hb['Vtok'] = [mk.sb(es, "rw_Vtok%d_%d" % (hp, i), [128, 128], BF16) for i in range(2)]
                hb['AKtok'] = [mk.sb(es, "rw_AKtok%d_%d" % (hp, i), [128, 256], BF16) for i in range(2)]
                hb['Wsb'] = mk.sb(es, "rw_Wsb%d" % hp, [128, 128], BF16)
                hb['Usb'] = mk.sb(es, "rw_Usb%d" % hp, [128, 128], BF16)
                hb['yT'] = mk.sb(es, "rw_yT%d" % hp, [128, TT], F32)
                sc.op('dve', [], [('Tf', hp)], lambda e, hb=hb: e.memset(hb['Tf'][:], 0.0))
                sc.op('pool', [], [('Tb', hp)], lambda e, hb=hb: e.memset(hb['Tb'][:], 0.0))
                H.append(hb)
            pbk = dn.pb[1]
            c3 = lambda ap: ap.rearrange("p (c t) -> p c t", t=64)
            r4 = lambda ap: ap.rearrange("p (c t) -> p c t", t=64).unsqueeze(2).broadcast_to((128, NCH, 2, 64))

            def prep_hp(hp):
                hb = H[hp]
                hs = slice(hp * 128, (hp + 1) * 128)
                shr, shk, shv = sh[hp], sh[2 + hp], sh[4 + hp]
                kr, kk_, kv = ('sh', hp), ('sh', 2 + hp), ('sh', 4 + hp)
                eG, gT = hb['eG'], hb['gT']
                p1 = nps()
                sc.op('pes', ['rw_WA', 'twa'], [('ps', p1)], lambda e: e.matmul(
                    dn.ps[p1][:, 0:TT], lhsT=WA[0:64, hs], rhs=twa[0:64, :], start=True, stop=True))
                sc.op('act', [('ps', p1), 'rw_pv'], ['lw'], lambda e: e.activation(
                    out=lw[:], in_=dn.ps[p1][:, 0:TT], func=AF.Exp, bias=pv[:, 24 + hp:25 + hp], scale=-1.0))
                sc.op('pool', ['lw'], ['lw'], lambda e: e.tensor_scalar(
                    out=lw[:], in0=lw[:], scalar1=1.0, scalar2=None, op0=ALU.add))
                sc.op('dve', ['lw'], ['lw'], lambda e: e.reciprocal(out=lw[:], in_=lw[:]))
                sc.op('pool', ['lw'], ['lw'], lambda e: e.tensor_scalar(
                    out=lw[:], in0=lw[:], scalar1=-0.6065306597126334, scalar2=None, op0=ALU.mult))
                p2 = nps()
                sc.op('pes', ['rw_WA', 'twa'], [('ps', p2)], lambda e: e.matmul(
                    dn.ps[p2][:, 0:TT], lhsT=WA[64:128, hs], rhs=twa[64:128, :], start=True, stop=True))
                sc.op('act', [('ps', p2), 'rw_pv'], ['av'], lambda e: e.activation(
                    out=av[:], in_=dn.ps[p2][:, 0:TT], func=AF.Exp, bias=pv[:, 26 + hp:27 + hp], scale=-1.0))
                sc.op('pool', ['av'], ['av'], lambda e: e.tensor_scalar(
                    out=av[:], in0=av[:], scalar1=1.0, scalar2=None, op0=ALU.add))
                sc.op('dve', ['av'], ['av'], lambda e: e.reciprocal(out=av[:], in_=av[:]))
                p3 = nps()
                sc.op('pe', ['rw_GU', 'sgl'], [('ps', p3)], lambda e: e.matmul(
                    dn.ps[p3][:, 0:TT], lhsT=GU[:, hs], rhs=sgl[:], start=True, stop=True))
                sc.op('act', [('ps', p3)], [('gT', hp)], lambda e: e.activation(
                    out=gT[:], in_=dn.ps[p3][:, 0:TT], func=AF.Copy))
                sc.op('dve', [kk_, 'rw_pv'], ['kk'], lambda e: e.tensor_scalar(
                    out=kk[:], in0=shk[:], scalar1=pv[:, 12 + hp:13 + hp], scalar2=None, op0=ALU.mult))
                sc.op('act', ['kk'], ['sq16'], lambda e: e.activation(out=sq16[:], in_=kk[:], func=AF.Square))
                p4 = nps()
                sc.op('pe', ['sq16', 'onesbd'], [('ps', p4)], lambda e: e.matmul(
                    dn.ps[p4][:, 0:TT], lhsT=onesbd[:], rhs=sq16[:], start=True, stop=True))
                sc.op('dve', [('ps', p4)], ['rn'], lambda e: e.tensor_scalar(
                    out=rn[:], in0=dn.ps[p4][:, 0:TT], scalar1=1e-24, scalar2=None, op0=ALU.max))
                sc.op('act', ['rn'], ['rn'], lambda e: e.activation(out=rn[:], in_=rn[:], func=AF.Ln))
                sc.op('act', ['rn'], ['rn'], lambda e: e.activation(out=rn[:], in_=rn[:], func=AF.Exp, scale=-0.5))
                sc.op('dve', ['kk', 'rn'], ['kkn'], lambda e: e.tensor_tensor(out=kkn[:], in0=kk[:], in1=rn[:], op=ALU.mult))
                sc.op('pool', ['av', 'rw_pv'], ['t1'], lambda e: e.tensor_scalar(
                    out=t1[:], in0=av[:], scalar1=pv[:, 14 + hp:15 + hp], scalar2=pv[:, 22 + hp:23 + hp],
                    op0=ALU.mult, op1=ALU.add))
                sc.op('dve', ['t1', kk_], ['kp'], lambda e: e.tensor_tensor(out=kp[:], in0=shk[:], in1=t1[:], op=ALU.mult))
                sc.op('dve', ['lw', 'rw_rmask'], ['lG'], lambda e: e.tensor_tensor_scan(
                    out=lG[:], data0=self.rmask[:, 0:TT], data1=lw[:], initial=0.0, op0=ALU.mult, op1=ALU.add))
                sc.op('act', ['lG'], [('eG', hp)], lambda e: e.activation(out=eG[:], in_=lG[:], func=AF.Exp))
                sc.op('act', ['lG'], ['enG'], lambda e: e.activation(out=enG[:], in_=lG[:], func=AF.Exp, scale=-1.0))
                sc.op('pool', ['lG', 'lw'], ['t1'], lambda e: e.tensor_tensor(out=t1[:], in0=lG[:], in1=lw[:], op=ALU.subtract))
                sc.op('act', ['t1'], ['eGp'], lambda e: e.activation(out=eGp[:], in_=t1[:], func=AF.Exp))
                sc.op('dve', ['kkn', 'av'], ['t1'], lambda e: e.tensor_tensor(out=t1[:], in0=kkn[:], in1=av[:], op=ALU.mult))
                sc.op('dve', ['t1', 'enG'], ['Asf'], lambda e: e.tensor_tensor(out=As_f[:], in0=t1[:], in1=enG[:], op=ALU.mult))
                sc.op('pool', ['kp', 'enG'], ['Ksf'], lambda e: e.tensor_tensor(out=Ks_f[:], in0=kp[:], in1=enG[:], op=ALU.mult))
                sc.op('dve', ['kkn', 'eGp'], ['eGp'], lambda e: e.scalar_tensor_tensor(
                    out=eGp[:], in0=kkn[:], scalar=-1.0, in1=eGp[:], op0=ALU.mult, op1=ALU.mult))
                sc.op('pool', [kr, ('eG', hp)], ['t1'], lambda e: e.tensor_tensor(out=t1[:], in0=shr[:], in1=eG[:], op=ALU.mult))
                for half in range(2):
                    psl = slice(half * 64, (half + 1) * 64)
                    csl = slice(half * 64, (half + 1) * 64)
                    eng = 'dve' if half == 0 else 'pool'
                    for (nm, src, skey) in (('As', As_f, 'Asf'), ('Ks', Ks_f, 'Ksf'), ('Bt', eGp, 'eGp'), ('Rt', t1, 't1')):
                        sc.op(eng, [skey], [('bd', nm, hp)], lambda e, nm=nm, src=src, psl=psl, csl=csl: e.tensor_copy(
                            out=hb['bd_' + nm][psl, :, csl], in_=c3(src[psl, :])))
                    for (nm, src, skey) in (('Ah', As_f, 'Asf'), ('Kh', Ks_f, 'Ksf')):
                        sc.op('dve', [skey, ('eG', hp)], [('bd', nm, hp)], lambda e, nm=nm, src=src, psl=psl, csl=csl: e.tensor_tensor(
                            out=hb['bd_' + nm][psl, :, csl], in0=c3(src[psl, :]),
                            in1=c3(eG[psl, :])[:, :, 63:64].broadcast_to((64, NCH, 64)), op=ALU.mult))
                sc.op('pool', ['eGp'], [('r2', 'Bt', hp)], lambda e: e.tensor_copy(out=hb['r2_Bt'][:], in_=r4(eGp[:])))
                sc.op('pool', ['t1'], [('r2', 'Rt', hp)], lambda e: e.tensor_copy(out=hb['r2_Rt'][:], in_=r4(t1[:])))
                sc.op('dve', ['Asf'], [('r2', 'As', hp)], lambda e: e.tensor_copy(out=hb['r2_As'][:], in_=r4(As_f[:])))
                sc.op('pool', [kv], [('r2', 'v', hp)], lambda e: e.tensor_copy(out=hb['r2_v'][:], in_=r4(shv[:])))
                sc.op('dve', [kr, 'kp', 'rw_pv'], [('rkk16', hp)], lambda e: e.scalar_tensor_tensor(
                    out=hb['rkk16'][:], in0=shr[:], scalar=pv[:, 16 + hp:17 + hp], in1=kp[:], op0=ALU.mult, op1=ALU.mult))

            def chunk(hp, c, par):
                hb = H[hp]
                PQ, NM, Z, Vtok, AKtok, Wsb, Usb, Tf, Tb, yT, eG = (hb[k] for k in
                    ('PQ', 'NM', 'Z', 'Vtok', 'AKtok', 'Wsb', 'Usb', 'Tf', 'Tb', 'yT', 'eG'))
                f2 = lambda t: t[:, c, :, :].rearrange("p a t -> p (a t)")
                K_ = lambda *a: a + (hp,)
                pX = nps()
                X = dn.ps[pX]
                for j, (lt, rt) in enumerate((('As', 'Bt'), ('Ks', 'Bt'), ('As', 'Rt'), ('Ks', 'Rt'))):
                    sc.op('pe', [('bd', lt, hp), ('r2', rt, hp)], [('ps', pX)], lambda e, j=j, lt=lt, rt=rt: e.matmul(
                        X[:, j * 128:(j + 1) * 128], lhsT=hb['bd_' + lt][:, c, :], rhs=f2(hb['r2_' + rt]), start=True, stop=True))
                pQ = nps()
                sc.op('pe', [('bd', 'Bt', hp), ('r2', 'As', hp)], [('ps', pQ)], lambda e: e.matmul(
                    dn.ps[pQ][:, 0:128], lhsT=hb['bd_Bt'][:, c, :], rhs=f2(hb['r2_As']), start=True, stop=True))
                sc.op('dve', [('ps', pX), 'rw_mask4'], [K_('PQ', 0)], lambda e: e.tensor_tensor(
                    out=PQ[0][:, 0:128], in0=X[:, 0:128], in1=self.mask4[:, 0:128], op=ALU.mult))
                sc.op('dve', [('ps', pX), 'rw_mask4'], [K_('NM', par)], lambda e: e.tensor_tensor(
                    out=NM[par][:], in0=X[:, 128:512], in1=self.mask4[:, 128:512], op=ALU.mult))
                sc.op('dve', [('ps', pQ), 'rw_lmask'], [K_('PQ', 0)], lambda e: e.tensor_tensor(
                    out=PQ[0][:, 128:256], in0=dn.ps[pQ][:, 0:128], in1=self.lmask[:], op=ALU.mult))
                zi = 0
                sc.op('pool', [K_('PQ', 0), 'ident'], [K_('Z', par, zi)], lambda e: e.tensor_tensor(
                    out=Z[par][0][:], in0=PQ[0][:, 0:128], in1=ident[:], op=ALU.add))
                yield
                cur = 0
                for lvl in range(5):
                    last = (lvl == 4)
                    nxt = 1 - cur
                    pP = nps()
                    Pp = dn.ps[pP]
                    if not last:
                        sc.op('pe', [K_('PQ', cur)], [('ps', pP)], lambda e, cur=cur, Pp=Pp: e.matmul(
                            Pp[:, 0:128], lhsT=PQ[cur][:, 128:256], rhs=PQ[cur][:, 0:128], start=True, stop=True))
                    sc.op('pe', [K_('PQ', cur)], [('ps', pP)], lambda e, cur=cur, Pp=Pp: e.matmul(
                        Pp[:, 128:256], lhsT=PQ[cur][:, 0:128], rhs=PQ[cur][:, 128:256], start=True, stop=True))
                    if not last:
                        sc.op('act', [('ps', pP)], [K_('PQ', nxt)], lambda e, nxt=nxt, Pp=Pp: e.activation(
                            out=PQ[nxt][:], in_=Pp[:, 0:256], func=AF.Copy))
                    else:
                        sc.op('act', [('ps', pP)], [K_('PQ', nxt)], lambda e, nxt=nxt, Pp=Pp: e.activation(
                            out=PQ[nxt][:, 128:256], in_=Pp[:, 128:256], func=AF.Copy))
                    yield
                    pZ = nps()
                    sc.op('pe', [K_('PQ', nxt), K_('Z', par, zi)], [('ps', pZ)], lambda e, nxt=nxt, pZ=pZ, zi=zi: e.matmul(
                        dn.ps[pZ][:, 0:128], lhsT=PQ[nxt][:, 128:256], rhs=Z[par][zi][:], start=True, stop=True))
                    sc.op('dve', [('ps', pZ), K_('Z', par, zi)], [K_('Z', par, 1 - zi)], lambda e, pZ=pZ, zi=zi: e.tensor_tensor(
                        out=Z[par][1 - zi][:], in0=dn.ps[pZ][:, 0:128], in1=Z[par][zi][:], op=ALU.add))
                    yield
                    zi = 1 - zi
                    cur = nxt
                Zf = Z[par][zi]
                zkey = K_('Z', par, zi)
                pV = nps()
                sc.op('pe', [('r2', 'v', hp), 'ident'], [('ps', pV)], lambda e: e.matmul(
                    dn.ps[pV][:, 0:128], lhsT=f2(hb['r2_v']), rhs=ident[:], start=True, stop=True))
                sc.op('act', [('ps', pV)], [K_('Vtok', par)], lambda e: e.activation(
                    out=Vtok[par][:], in_=dn.ps[pV][:, 0:128], func=AF.Copy))
                sc.op('pe', [('bd', 'Ah', hp), 'ident'], ['pb1'], lambda e: e.transpose(
                    out=pbk[:, 0:128], in_=hb['bd_Ah'][:, c, :], identity=ident[:]))
                sc.op('pe', [('bd', 'Kh', hp), 'ident'], ['pb1'], lambda e: e.transpose(
                    out=pbk[:, 128:256], in_=hb['bd_Kh'][:, c, :], identity=ident[:]))
                sc.op('dve', ['pb1'], [K_('AKtok', par)], lambda e: e.tensor_copy(out=AKtok[par][:], in_=pbk[:, 0:256]))
                yield
                pW = nps()
                sc.op('pe', [('bd', 'Bt', hp), ('Tb', hp)], [('ps', pW)], lambda e: e.matmul(
                    dn.ps[pW][:, 0:128], lhsT=hb['bd_Bt'][:, c, :], rhs=Tb[:], start=True, stop=False))
                sc.op('pe', [K_('NM', par), K_('Vtok', par)], [('ps', pW)], lambda e: e.matmul(
                    dn.ps[pW][:, 0:128], lhsT=NM[par][:, 0:128], rhs=Vtok[par][:], start=False, stop=True))
                sc.op('act', [('ps', pW)], [K_('Wsb')], lambda e: e.activation(out=Wsb[:], in_=dn.ps[pW][:, 0:128], func=AF.Copy))
                yield
                pU = nps()
                sc.op('pe', [zkey, K_('Wsb')], [('ps', pU)], lambda e: e.matmul(
                    dn.ps[pU][:, 0:128], lhsT=Zf[:], rhs=Wsb[:], start=True, stop=True))
                sc.op('dve', [('ps', pU)], [K_('Usb')], lambda e: e.tensor_copy(out=Usb[:], in_=dn.ps[pU][:, 0:128]))
                yield
                pT = nps()
                sc.op('pe', [K_('AKtok', par), K_('Usb')], [('ps', pT)], lambda e: e.matmul(
                    dn.ps[pT][:, 0:128], lhsT=AKtok[par][:, 0:128], rhs=Usb[:], start=True, stop=False))
                sc.op('pe', [K_('AKtok', par), K_('Vtok', par)], [('ps', pT)], lambda e: e.matmul(
                    dn.ps[pT][:, 0:128], lhsT=AKtok[par][:, 128:256], rhs=Vtok[par][:], start=False, stop=True))
                pY = nps()
                sc.op('pe', [('Tb', hp), ('bd', 'Rt', hp)], [('ps', pY)], lambda e: e.matmul(
                    dn.ps[pY][:, 0:128], lhsT=Tb[:], rhs=hb['bd_Rt'][:, c, :], start=True, stop=False))
                sc.op('pe', [K_('Usb'), K_('NM', par)], [('ps', pY)], lambda e: e.matmul(
                    dn.ps[pY][:, 0:128], lhsT=Usb[:], rhs=NM[par][:, 128:256], start=False, stop=False))
                sc.op('pe', [K_('Vtok', par), K_('NM', par)], [('ps', pY)], lambda e: e.matmul(
                    dn.ps[pY][:, 0:128], lhsT=Vtok[par][:], rhs=NM[par][:, 256:384], start=False, stop=True))
                sc.op('dve', [('ps', pT), ('Tf', hp), ('eG', hp)], [('Tf', hp)], lambda e: e.scalar_tensor_tensor(
                    out=Tf[:], in0=Tf[:], scalar=eG[:, c * 64 + 63:c * 64 + 64], in1=dn.ps[pT][:, 0:128],
                    op0=ALU.mult, op1=ALU.add))
                sc.op('act', [('Tf', hp)], [('Tb', hp)], lambda e: e.activation(out=Tb[:], in_=Tf[:], func=AF.Copy))
                sc.op('act', [('ps', pY)], [('yT', hp)], lambda e: e.activation(
                    out=yT[0:64, c * 64:(c + 1) * 64], in_=dn.ps[pY][0:64, 0:64], func=AF.Copy))
                sc.op('act', [('ps', pY)], [('yT', hp)], lambda e: e.activation(
                    out=yT[64:128, c * 64:(c + 1) * 64], in_=dn.ps[pY][64:128, 64:128], func=AF.Copy))
                yield

            def epilogue(hp, t0):
                hb = H[hp]
                yT, gT, shv, kv = hb['yT'], hb['gT'], sh[4 + hp], ('sh', 4 + hp)
                sc.op('pool', [('yT', hp)], ['y16'], lambda e: e.tensor_copy(out=y16[:], in_=yT[:]))
                pM = nps()
                sc.op('pe', ['y16', 'onesbd'], [('ps', pM)], lambda e: e.matmul(
                    dn.ps[pM][:, 0:TT], lhsT=onesbd[:], rhs=y16[:], start=True, stop=True))
                sc.op('dve', [('ps', pM), ('yT', hp)], ['cen'], lambda e: e.scalar_tensor_tensor(
                    out=cen[:], in0=dn.ps[pM][:, 0:TT], scalar=-1.0 / 64, in1=yT[:], op0=ALU.mult, op1=ALU.add))
                sc.op('act', ['cen'], ['y16'], lambda e: e.activation(out=y16[:], in_=cen[:], func=AF.Square))
                pV2 = nps()
                sc.op('pe', ['y16', 'onesbd'], [('ps', pV2)], lambda e: e.matmul(
                    dn.ps[pV2][:, 0:TT], lhsT=onesbd[:], rhs=y16[:], start=True, stop=True))
                sc.op('dve', [('ps', pV2)], ['rs'], lambda e: e.tensor_scalar(
                    out=rs[:], in0=dn.ps[pV2][:, 0:TT], scalar1=1.0 / 64, scalar2=64e-5, op0=ALU.mult, op1=ALU.add))
                sc.op('act', ['rs'], ['rs'], lambda e: e.activation(out=rs[:], in_=rs[:], func=AF.Ln))
                sc.op('act', ['rs'], ['rs'], lambda e: e.activation(out=rs[:], in_=rs[:], func=AF.Exp, scale=-0.5))
                sc.op('dve', ['rs', 'cen'], ['cen'], lambda e: e.tensor_tensor(out=cen[:], in0=cen[:], in1=rs[:], op=ALU.mult))
                sc.op('dve', ['cen', 'rw_pv'], ['cen'], lambda e: e.tensor_scalar(
                    out=cen[:], in0=cen[:], scalar1=pv[:, 18 + hp:19 + hp], scalar2=pv[:, 20 + hp:21 + hp],
                    op0=ALU.mult, op1=ALU.add))
                pBn = nps()
                sc.op('pe', [('rkk16', hp), 'onesbd'], [('ps', pBn)], lambda e: e.matmul(
                    dn.ps[pBn][:, 0:TT], lhsT=onesbd[:], rhs=hb['rkk16'][:], start=True, stop=True))
                sc.op('dve', [('ps', pBn), kv], ['rs'], lambda e: e.tensor_tensor(
                    out=rs[:], in0=dn.ps[pBn][:, 0:TT], in1=shv[:], op=ALU.mult))
                sc.op('pool', ['rs', 'cen'], ['cen'], lambda e: e.tensor_tensor(out=cen[:], in0=cen[:], in1=rs[:], op=ALU.add))
                sc.op('dve', ['cen', ('gT', hp)], ['ob'], lambda e: e.tensor_tensor(out=ob[:], in0=cen[:], in1=gT[:], op=ALU.mult))
                sc.dma(STORE_Q, OT[3, hp, :, t0:t0 + TT], ob[:], ['ob'], [])

            ci = 0
            for t0 in range(0, S, TT):
                for g in range(8):
                    gi = GIDX['rw%d' % g]
                    if t0 == 0:
                        sc.op('pool', [], [('raw', g)], lambda e, g=g: e.memset(raw[g][:, 0:1], 0.0))
                        sc.dma('sp', raw[g][:, 1:TT + 1], UC[gi, :, 0:TT], [], [('raw', g)])
                    else:
                        sc.dma('sp', raw[g][:, :], UC[gi, :, t0 - 1:t0 + TT], [], [('raw', g)])
                    sc.op('pool', [('raw', g)], ['dtmp'], lambda e, g=g: e.tensor_tensor(
                        out=dtmp[:], in0=raw[g][:, 0:TT], in1=raw[g][:, 1:TT + 1], op=ALU.subtract))
                    sc.op('dve', ['dtmp', ('raw', g), 'rw_pv'], [('sh', g)], lambda e, g=g: e.scalar_tensor_tensor(
                        out=sh[g][:], in0=dtmp[:], scalar=pv[:, g:g + 1], in1=raw[g][:, 1:TT + 1],
                        op0=ALU.mult, op1=ALU.add))
                    if g % 2:
                        yield
                sc.op('act', [('sh', 6)], ['dtmp'], lambda e: e.activation(out=dtmp[0:64, :], in_=sh[6][0:64, :], func=AF.Exp, scale=2.0))
                sc.op('pool', ['dtmp'], ['dtmp'], lambda e: e.tensor_scalar(
                    out=dtmp[0:64, :], in0=dtmp[0:64, :], scalar1=1.0, scalar2=None, op0=ALU.add))
                sc.op('dve', ['dtmp'], ['dtmp'], lambda e: e.reciprocal(out=dtmp[0:64, :], in_=dtmp[0:64, :]))
                sc.op('dve', ['dtmp'], ['twa'], lambda e: e.tensor_scalar(
                    out=twa[0:64, :], in0=dtmp[0:64, :], scalar1=-2.0, scalar2=1.0, op0=ALU.mult, op1=ALU.add))
                sc.op('pool', [('sh', 6)], ['twa'], lambda e: e.tensor_copy(out=twa[64:128, :], in_=sh[6][64:128, :]))
                sc.op('act', [('sh', 7)], ['kk'], lambda e: e.activation(out=kk[:], in_=sh[7][:], func=AF.Exp, scale=-1.0))
                sc.op('pool', ['kk'], ['kk'], lambda e: e.tensor_scalar(
                    out=kk[:], in0=kk[:], scalar1=1.0, scalar2=None, op0=ALU.add))
                sc.op('dve', ['kk'], ['kk'], lambda e: e.reciprocal(out=kk[:], in_=kk[:]))
                sc.op('pool', ['kk'], ['sgl'], lambda e: e.tensor_copy(out=sgl[:], in_=kk[:]))
                for hp in range(2):
                    prep_hp(hp)
                    yield
                for c in range(NCH):
                    par = ci % 2
                    ci += 1
                    gens = [chunk(hp, c, par) for hp in range(2)]
                    while gens:
                        for g_ in list(gens):
                            try:
                                next(g_)
                            except StopIteration:
                                gens.remove(g_)
                        yield
                for hp in range(2):
                    epilogue(hp, t0)
                    yield


NEG = -30000.0
BIGV = 1e30


def nsa_consts(S):
    NB, NKT = S // 64, S // 128
    n_cmp = (S - 32) // 16 + 1
    NT = (n_cmp + 127) // 128
    n = np.arange(128)
    t = np.arange(128)
    d = {}
    cp = np.zeros((128, 17, 128), np.float32)
    for k in range(17):
        cp[:, k, :] = np.where(16 * n[:, None] + 31 <= 128 * k + t[None, :], 0.0, NEG)
    d["c_cpat"] = cp
    d["c_causneg"] = np.where(n[:, None] > t[None, :], NEG, 0.0).astype(np.float32)
    d["c_bandneg"] = np.where(n[:, None] <= t[None, :], NEG, 0.0).astype(np.float32)
    E = np.zeros((128, NKT, 128), np.float32)
    for j in range(NKT):
        for sl in range(128):
            bb = 2 * j + sl // 64
            E[bb, j, sl] = 1.0
    d["c_E"] = E[:max(NB, 1)] if NB <= 128 else E
    ov = np.zeros((128, NT, NB + 1), np.float32)
    for jn in range(NT):
        nn = jn * 128 + n
        valid = nn < n_cmp
        cs = 16 * nn
        for b in range(NB):
            ov[:, jn, b] = ((cs < 64 * (b + 1)) & (cs + 32 > 64 * b) & valid).astype(np.float32)
        ov[:, jn, NB] = valid.astype(np.float32)
    d["c_ovl"] = ov
    jj = np.arange(2 * NB)
    j = jj - NB
    hi = (t >= 64).astype(np.int64)
    allowed = j[None, :] <= hi[:, None]
    forced = (j[None, :] == hi[:, None]) | (j[None, :] == hi[:, None] - 1)
    d["c_amnf"] = (allowed & ~forced).astype(np.float32)
    d["c_fbna"] = np.where(forced, BIGV, np.where(allowed, 0.0, -1.0)).astype(np.float32)
    return d


class NSA:
    def __init__(self, dn, gla, es):
        self.dn, self.mk, self.sc, self.gla = dn, dn.mk, dn.sc, gla
        mk, S = self.mk, self.mk.S
        self.NB, self.NKT = S // 64, S // 128
        self.n_cmp = (S - 32) // 16 + 1
        self.NT = (self.n_cmp + 127) // 128
        self.cin = {n: mk.din(n, shp) for n, shp in (
            ("c_cpat", [128, 17, 128]), ("c_causneg", [128, 128]), ("c_bandneg", [128, 128]),
            ("c_E", [self.NB, self.NKT, 128]), ("c_ovl", [128, self.NT, self.NB + 1]),
            ("c_amnf", [128, 2 * self.NB]), ("c_fbna", [128, 2 * self.NB]))}

    def run(self, L, UC, UT, OT, ws, es):
        sc, mk, dn, S = self.sc, self.mk, self.dn, self.mk.S
        NB, NKT, NT, n_cmp = self.NB, self.NKT, self.NT, self.n_cmp
        ident = dn.ident
        PS = dn.ps
        sc.ns = 'nsa'
        T = {}
        T['cpat'] = mk.sb(es, "ns_cpat", [128, 17 * 128], BF16)
        T['caus'] = mk.sb(es, "ns_caus", [128, 128], BF16)
        T['band'] = mk.sb(es, "ns_band", [128, 128], BF16)
        T['Eb'] = mk.sb(es, "ns_E", [NB, NKT * 128], BF16)
        T['ovl'] = mk.sb(es, "ns_ovl", [128, NT * (NB + 1)], BF16)
        T['amnf'] = mk.sb(es, "ns_amnf", [128, 2 * NB], F32)
        T['fbna'] = mk.sb(es, "ns_fbna", [128, 2 * NB], F32)
        T['ksT'] = mk.sb(es, "ns_ksT", [64, S], BF16)
        T['kwT'] = mk.sb(es, "ns_kwT", [64, S], BF16)
        T['vs'] = mk.sb(es, "ns_vs", [128, NKT, 128], BF16)
        T['vw'] = mk.sb(es, "ns_vw", [128, NKT, 128], BF16)
        T['kcmpT'] = mk.sb(es, "ns_kcmpT", [64, NT * 128], BF16)
        T['vcmp'] = mk.sb(es, "ns_vcmp", [128, NT, 128], BF16)
        T['qf'] = mk.sb(es, "ns_qf", [64, 4, 128], F32)
        T['q16'] = mk.sb(es, "ns_q16", [64, 512], BF16)
        T['gf'] = mk.sb(es, "ns_gf", [64, 12, 128], F32)
        T['Pc'] = [mk.sb(es, "ns_Pc%d" % i, [128, 512], BF16) for i in range(NT)]
        T["Pr"] = [mk.sb(es, "ns_Pr%d" % i, [128, 512], BF16) for i in range(4)]
        T['ox'] = [mk.sb(es, "ns_ox%d" % i, [64, 512], F32) for i in range(3)]
        T['zx'] = [mk.sb(es, "ns_zx%d" % i, [64, 512], F32) for i in range(3)]
        T['zr'] = mk.sb(es, "ns_zr", [128, 8], F32)
        T['imp'] = mk.sb(es, "ns_imp", [128, NB], F32)
        T['sc1'] = mk.sb(es, "ns_sc1", [128, NB], F32)
        T['sc2'] = mk.sb(es, "ns_sc2", [128, NB], F32)
        T['m8'] = mk.sb(es, "ns_m8", [128, 16], F32)
        T['selm'] = mk.sb(es, "ns_selm", [128, 128], BF16)
        T['selT'] = mk.sb(es, "ns_selT", [128, 128], BF16)
        T['acc'] = T['ox'][0]
        T['ob'] = mk.sb(es, "ns_ob", [64, 512], BF16)
        with ExitStack() as es2:
            stage = [mk.sb(es2, "ns_st%d" % i, [128, 2048], F32) for i in range(2)]
            sti = [0]

            def load_cast(dst_ap, src_ap, rows, cols, key):
                for c0 in range(0, cols, 2048):
                    c1 = min(cols, c0 + 2048)
                    si = sti[0] % 2
                    sti[0] += 1
                    sc.dma('sp', stage[si][0:rows, 0:c1 - c0], src_ap[:, c0:c1], [], [('nst', si)])
                    eng = ('dve', 'pool')[sti[0] % 2]
                    sc.op(eng, [('nst', si)], [key], lambda e, si=si, c0=c0, c1=c1: e.tensor_copy(
                        out=dst_ap[:, c0:c1], in_=stage[si][0:rows, 0:c1 - c0]))

            load_cast(T['cpat'][:], self.cin["c_cpat"].rearrange("p k t -> p (k t)"), 128, 17 * 128, 'cpat')
            load_cast(T['caus'][:], self.cin["c_causneg"], 128, 128, 'caus')
            load_cast(T['band'][:], self.cin["c_bandneg"], 128, 128, 'band')
            load_cast(T['Eb'][:], self.cin["c_E"].rearrange("p k t -> p (k t)"), NB, NKT * 128, 'E')
            load_cast(T['ovl'][:], self.cin["c_ovl"].rearrange("p k t -> p (k t)"), 128, NT * (NB + 1), 'ovl')
            sc.dma('sp', T['amnf'][:], self.cin["c_amnf"][:, :], [], ['amnf'])
            sc.dma('sp', T['fbna'][:], self.cin["c_fbna"][:, :], [], ['fbna'])
            load_cast(T['ksT'][:], UC[GIDX['nsa_ks'], 0:64, :], 64, S, 'ksT')
            load_cast(T['kwT'][:], UC[GIDX['nsa_kw'], 0:64, :], 64, S, 'kwT')
            sc.op('pool', [], ['vs'], lambda e: e.memset(T['vs'][:], 1.0))
            sc.op('pool', [], ['vw'], lambda e: e.memset(T['vw'][:], 1.0))
            sc.op('pool', [], ['vcmp'], lambda e: e.memset(T['vcmp'][:], 1.0))
            for n0 in range(0, NKT, 8):
                n1 = min(NKT, n0 + 8)
                si = sti[0] % 2
                sti[0] += 1
                stv = stage[si][:, 0:(n1 - n0) * 128].rearrange("p (n c) -> p n c", c=128)
                sc.dma('sp', stv, UT[n0 * 128:n1 * 128, 0:128].rearrange("(n p) c -> p n c", p=128), [], [('nst', si)])
                sc.op('dve', [('nst', si)], ['vs'], lambda e, n0=n0, n1=n1, stv=stv: e.tensor_copy(
                    out=T['vs'][:, n0:n1, 0:64], in_=stv[:, :, 0:64]))
                sc.op('pool', [('nst', si)], ['vw'], lambda e, n0=n0, n1=n1, stv=stv: e.tensor_copy(
                    out=T['vw'][:, n0:n1, 0:64], in_=stv[:, :, 64:128]))
            kcmpT, vcmp = T['kcmpT'], T['vcmp']
            sc.op('pool', [], ['kcmpT'], lambda e: e.memset(kcmpT[:], 0.0))
            kc16 = mk.sb(es2, "ns_kc16", [64, S], BF16)
            W1 = mk.sb(es2, "ns_W1", [64, 32 * 128], BF16)
            W2f = mk.sb(es2, "ns_W2f", [128, 64], F32)
            W2 = mk.sb(es2, "ns_W2", [128, 64], BF16)
            posf = mk.sb(es2, "ns_posf", [64, 32], F32)
            pos16 = mk.sb(es2, "ns_pos16", [64, 32], BF16)
            cb = mk.sb(es2, "ns_cb", [128, 1], F32)
            hid = mk.sb(es2, "ns_hid", [128, NT * 128], BF16)
            hpre = mk.sb(es2, "ns_hpre", [128, NT * 128], F32)
            hexp = mk.sb(es2, "ns_hexp", [128, NT * 128], F32)
            for which in range(2):
                gname = 'nsa_kc' if which == 0 else 'nsa_vc'
                load_cast(kc16[:], UC[GIDX[gname], 0:64, :], 64, S, 'kc16')
                load_cast(W1[:], ws['w1r'][which], 64, 32 * 128, 'W1')
                sc.dma('sp', W2f[:], ws['w2'][which], [], ['W2f'])
                sc.op('dve', ['W2f'], ['W2'], lambda e: e.tensor_copy(out=W2[:], in_=W2f[:]))
                sc.dma('sp', posf[:], ws['posT'][which], [], ['posf'])
                sc.op('dve', ['posf'], ['pos16'], lambda e: e.tensor_copy(out=pos16[:], in_=posf[:]))
                pc = 0
                for j in range(32):
                    sc.op('pes', ['W1', 'pos16'], [('ps', pc)], lambda e, j=j: e.matmul(
                        PS[pc][:, 0:1], lhsT=W1[:, j * 128:(j + 1) * 128], rhs=pos16[:, j:j + 1],
                        start=(j == 0), stop=(j == 31)))
                sc.op('dve', [('ps', pc)], ['cb'], lambda e: e.tensor_copy(out=cb[:], in_=PS[pc][:, 0:1]))
                ph = 1
                for j in range(32):
                    sc.op('pes', ['W1', 'kc16'], [('ps', ph)], lambda e, j=j: e.matmul(
                        PS[ph][:, 0:n_cmp], lhsT=W1[:, j * 128:(j + 1) * 128],
                        rhs=kc16[:, j:j + 16 * (n_cmp - 1) + 1:16], start=(j == 0), stop=(j == 31)))
                sc.op('pool', [], ['hid'], lambda e: e.memset(hid[:], 0.0))
                sc.op('act', [('ps', ph), 'cb'], ['hpre'], lambda e: e.activation(
                    out=hpre[:, 0:n_cmp], in_=PS[ph][:, 0:n_cmp], func=AF.Identity, bias=cb[:, 0:1], scale=1.0))
                sc.op('act', ['hpre'], ['hexp'], lambda e: e.activation(
                    out=hexp[:, 0:n_cmp], in_=hpre[:, 0:n_cmp], func=AF.Exp, scale=-1.0))
                sc.op('pool', ['hexp'], ['hexp'], lambda e: e.tensor_scalar(
                    out=hexp[:, 0:n_cmp], in0=hexp[:, 0:n_cmp], scalar1=1.0, scalar2=None, op0=ALU.add))
                sc.op('dve', ['hexp'], ['hexp'], lambda e: e.reciprocal(out=hexp[:, 0:n_cmp], in_=hexp[:, 0:n_cmp]))
                sc.op('dve', ['hexp', 'hpre'], ['hid'], lambda e: e.tensor_tensor(
                    out=hid[:, 0:n_cmp], in0=hexp[:, 0:n_cmp], in1=hpre[:, 0:n_cmp], op=ALU.mult))
                if which == 0:
                    po = 2
                    sc.op('pe', ['W2', 'hid'], [('ps', po)], lambda e: e.matmul(
                        PS[po][0:64, 0:n_cmp], lhsT=W2[:], rhs=hid[:, 0:n_cmp], start=True, stop=True))
                    sc.op('act', [('ps', po)], ['kcmpT'], lambda e: e.activation(
                        out=kcmpT[:, 0:n_cmp], in_=PS[po][0:64, 0:n_cmp], func=AF.Copy))
                else:
                    for jn in range(NT):
                        po = 2 + jn % 2
                        sc.op('pe', ['W2', 'hid'], [('ps', po)], lambda e, jn=jn, po=po: e.matmul(
                            PS[po][:, 0:64], lhsT=hid[:, jn * 128:(jn + 1) * 128], rhs=W2[:], start=True, stop=True))
                        sc.op('act', [('ps', po)], ['vcmp'], lambda e, jn=jn, po=po: e.activation(
                            out=vcmp[:, jn, 0:64], in_=PS[po][:, 0:64], func=AF.Copy))
            sc.ns = None
            sc.barrier()
        return self.tiles(UC, OT, T)

    def tiles(self, UC, OT, T):
        sc, mk, dn, S = self.sc, self.mk, self.dn, self.mk.S
        NB, NKT, NT, n_cmp = self.NB, self.NKT, self.NT, self.n_cmp
        ident = dn.ident
        PS = dn.ps
        cpat, caus, band, Eb, ovl, amnf, fbna = (T[k] for k in ('cpat', 'caus', 'band', 'Eb', 'ovl', 'amnf', 'fbna'))
        ksT, kwT, vs, vw, kcmpT, vcmp = (T[k] for k in ('ksT', 'kwT', 'vs', 'vw', 'kcmpT', 'vcmp'))
        qf, q16, gf, Pc, Pr, ox, zx, zr, imp, sc1, sc2, m8, selm, selT, acc, ob = (T[k] for k in (
            'qf', 'q16', 'gf', 'Pc', 'Pr', 'ox', 'zx', 'zr', 'imp', 'sc1', 'sc2', 'm8', 'selm', 'selT', 'acc', 'ob'))
        sc.op('pool', [], ['selm'], lambda e: e.memset(selm[:], 0.0))
        pbk = dn.pb[1]
        SB_ = [0, 1, 3, 4]
        IB_ = [0, 1]
        OZ = 2
        DEPTH_P = 3
        pr_i = [0]
        cnt = [0, 0]

        def bc4(ap):
            return ap.unsqueeze(1).broadcast_to((ap.shape[0], 4, 128))

        def issue(kT_ap, kkey, addmask):
            sbk = SB_[cnt[0] % len(SB_)]
            cnt[0] += 1
            nmm = 1 + len(addmask)
            sc.op('pe', [kkey, 'q16'], [('ps', sbk)], lambda e: e.matmul(
                PS[sbk][:, :], lhsT=kT_ap, rhs=q16[:], start=True, stop=(nmm == 1)))
            for mi, (ml, mr, mkeys) in enumerate(addmask):
                sc.op('pe', mkeys, [('ps', sbk)], lambda e, ml=ml, mr=mr, mi=mi: e.matmul(
                    PS[sbk][:, :], lhsT=ml, rhs=mr, start=False, stop=(mi == nmm - 2)))
            pt = Pr[pr_i[0] % len(Pr)]
            pkey = ('Pr', pr_i[0] % len(Pr))
            pr_i[0] += 1
            sc.op('act', [('ps', sbk)], [pkey], lambda e: e.activation(out=pt[:], in_=PS[sbk][:, :], func=AF.Exp))
            return pt, pkey

        def consume(pt, pkey, vlhs, vkey, first, last):
            sc.op('pe', [pkey, vkey], [('ps', OZ)], lambda e: e.matmul(
                PS[OZ][:, :], lhsT=vlhs, rhs=pt[:], start=first, stop=last))

        def branch(pairs):
            pend = []
            n = len(pairs)
            done = 0
            for idx, (kT_ap, kkey, addmask, vlhs, vkey) in enumerate(pairs):
                pt, pkey = issue(kT_ap, kkey, addmask)
                pend.append((pt, pkey, vlhs, vkey))
                if len(pend) > DEPTH_P:
                    a = pend.pop(0)
                    consume(a[0], a[1], a[2], a[3], done == 0, done == n - 1)
                    done += 1
                yield
            while pend:
                a = pend.pop(0)
                consume(a[0], a[1], a[2], a[3], done == 0, done == n - 1)
                done += 1
            yield

        def finish(x):
            sc.op('act', [('ps', OZ)], [('ox', x)], lambda e: e.activation(out=ox[x][:], in_=PS[OZ][0:64, :], func=AF.Copy))
            sc.op('dve', [('ps', OZ)], [('zx', x)], lambda e: e.tensor_scalar(
                out=zx[x][:], in0=PS[OZ][64:128, :], scalar1=1e-30, scalar2=None, op0=ALU.max))
            sc.op('dve', [('zx', x)], [('zx', x)], lambda e: e.reciprocal(out=zx[x][:], in_=zx[x][:]))

        for i in range(NKT):
            t0 = i * 128
            for h in range(4):
                sc.dma('sp', qf[:, h, :], UC[GIDX['nsa_q%d' % h], 0:64, t0:t0 + 128], [], ['qf'])
            for gi in range(12):
                sc.dma('sp', gf[:, gi, :], UC[GIDX['nsa_g%d' % gi], 0:64, t0:t0 + 128], [], ['gf'])
            sc.op('act', ['qf'], ['q16'], lambda e: e.activation(
                out=q16[:], in_=qf[:].rearrange("p h t -> p (h t)"), func=AF.Copy, scale=0.125))
            sc.op('act', ['gf'], ['gf'], lambda e: e.activation(out=gf[:], in_=gf[:], func=AF.Exp, scale=-1.0))
            sc.op('pool', ['gf'], ['gf'], lambda e: e.tensor_scalar(
                out=gf[:], in0=gf[:], scalar1=1.0, scalar2=None, op0=ALU.add))
            sc.op('dve', ['gf'], ['gf'], lambda e: e.reciprocal(out=gf[:], in_=gf[:]))
            yield
            jmax = min(NT - 1, (8 * i + 6) // 128)
            for jn in range(jmax + 1):
                sbk = SB_[cnt[0] % len(SB_)]
                cnt[0] += 1
                k = i - 16 * jn
                need_mask = k <= 16
                sc.op('pe', ['kcmpT', 'q16'], [('ps', sbk)], lambda e, jn=jn, sbk=sbk, need_mask=need_mask: e.matmul(
                    PS[sbk][:, :], lhsT=kcmpT[:, jn * 128:(jn + 1) * 128], rhs=q16[:], start=True, stop=(not need_mask)))
                if need_mask:
                    sc.op('pe', ['cpat', 'ident'], [('ps', sbk)], lambda e, k=k, sbk=sbk: e.matmul(
                        PS[sbk][:, :], lhsT=ident[:], rhs=bc4(cpat[:, k * 128:(k + 1) * 128]), start=False, stop=True))
                sc.op('act', [('ps', sbk)], [('Pc', jn)], lambda e, jn=jn, sbk=sbk: e.activation(
                    out=Pc[jn][:], in_=PS[sbk][:, :], func=AF.Exp))
                yield
            for jn in range(jmax + 1):
                sc.op('pe', [('Pc', jn), 'vcmp'], [('ps', OZ)], lambda e, jn=jn: e.matmul(
                    PS[OZ][:, :], lhsT=vcmp[:, jn, :], rhs=Pc[jn][:], start=(jn == 0), stop=(jn == jmax)))
            finish(0)
            for h in range(4):
                ib = IB_[h // 2]
                c0 = (h % 2) * (NB + 1)
                for jn in range(jmax + 1):
                    sc.op('pe', [('Pc', jn), 'ovl'], [('ps', ib)], lambda e, jn=jn, h=h, ib=ib, c0=c0: e.matmul(
                        PS[ib][:, c0:c0 + NB + 1], lhsT=Pc[jn][:, h * 128:(h + 1) * 128],
                        rhs=ovl[:, jn * (NB + 1):(jn + 1) * (NB + 1)], start=(jn == 0), stop=(jn == jmax)))
            for h in range(4):
                ib = IB_[h // 2]
                c0 = (h % 2) * (NB + 1)
                sc.op('dve', [('ps', ib)], ['zr'], lambda e, h=h, ib=ib, c0=c0: e.tensor_scalar(
                    out=zr[:, h:h + 1], in0=PS[ib][:, c0 + NB:c0 + NB + 1], scalar1=1e-30, scalar2=None, op0=ALU.max))
            sc.op('dve', ['zr'], ['zr'], lambda e: e.reciprocal(out=zr[:, 4:8], in_=zr[:, 0:4]))
            for h in range(4):
                ib = IB_[h // 2]
                c0 = (h % 2) * (NB + 1)
                if h == 0:
                    sc.op('dve', [('ps', ib), 'zr'], ['imp'], lambda e, ib=ib, c0=c0: e.tensor_scalar(
                        out=imp[:], in0=PS[ib][:, c0:c0 + NB], scalar1=zr[:, 4:5], scalar2=None, op0=ALU.mult))
                else:
                    sc.op('dve', [('ps', ib), 'zr', 'imp'], ['imp'], lambda e, h=h, ib=ib, c0=c0: e.scalar_tensor_tensor(
                        out=imp[:], in0=PS[ib][:, c0:c0 + NB], scalar=zr[:, 4 + h:5 + h], in1=imp[:],
                        op0=ALU.mult, op1=ALU.add))
            yield
            jsl = slice(NB - 2 * i, 2 * NB - 2 * i)
            sc.op('dve', ['imp', 'amnf'], ['sc1'], lambda e, jsl=jsl: e.tensor_tensor(
                out=sc1[:], in0=imp[:], in1=amnf[:, jsl], op=ALU.mult))
            sc.op('dve', ['sc1', 'fbna'], ['sc1'], lambda e, jsl=jsl: e.tensor_tensor(
                out=sc1[:], in0=sc1[:], in1=fbna[:, jsl], op=ALU.add))
            sc.op('dve', ['sc1'], ['sc1'], lambda e: e.memset(sc1[:, 0:1], BIGV))
            sc.op('dve', ['sc1'], ['m8'], lambda e: e.max(out=m8[:, 0:8], in_=sc1[:]))
            sc.op('dve', ['sc1', 'm8'], ['sc2'], lambda e: e.match_replace(
                out=sc2[:], in_to_replace=m8[:, 0:8], in_values=sc1[:], imm_value=-2.0))
            sc.op('dve', ['sc2'], ['m8'], lambda e: e.max(out=m8[:, 8:16], in_=sc2[:]))
            sc.op('dve', ['sc1', 'm8'], ['sc2'], lambda e: e.tensor_scalar(
                out=sc2[:], in0=sc1[:], scalar1=m8[:, 15:16], scalar2=None, op0=ALU.is_ge))
            sc.op('dve', ['sc2'], ['selm'], lambda e: e.tensor_scalar(
                out=selm[:, 0:NB], in0=sc2[:], scalar1=-1.0, scalar2=-NEG, op0=ALU.add, op1=ALU.mult))
            yield
            j0 = max(0, i - 4)
            pairs = []
            for j in range(j0, i + 1):
                am = []
                if j == i:
                    am.append((ident[:], bc4(caus[:]), ['ident', 'caus']))
                elif j == i - 4:
                    am.append((ident[:], bc4(band[:]), ['ident', 'band']))
                pairs.append((kwT[:, j * 128:(j + 1) * 128], 'kwT', am, vw[:, j, :], 'vw'))
            yield from branch(pairs)
            finish(2)
            sc.op('pe', ['selm', 'ident'], ['pb1'], lambda e: e.transpose(
                out=pbk[:, 0:128], in_=selm[:], identity=ident[:]))
            sc.op('act', ['pb1'], ['selT'], lambda e: e.activation(out=selT[:], in_=pbk[:, 0:128], func=AF.Copy))
            pairs = []
            for j in range(i + 1):
                am = [(Eb[0:NB, j * 128:(j + 1) * 128], bc4(selT[0:NB, :]), ['E', 'selT'])]
                if j == i:
                    am.append((ident[:], bc4(caus[:]), ['ident', 'caus']))
                pairs.append((ksT[:, j * 128:(j + 1) * 128], 'ksT', am, vs[:, j, :], 'vs'))
            yield from branch(pairs)
            finish(1)
            for x in range(3):
                sc.op('dve', [('zx', x), 'gf'], [('zx', x)], lambda e, x=x: e.tensor_tensor(
                    out=zx[x][:], in0=zx[x][:], in1=gf[:, 4 * x:4 * x + 4, :].rearrange("p h t -> p (h t)"), op=ALU.mult))
                if x == 0:
                    sc.op('pool', [('zx', x), ('ox', x)], [('ox', 0)], lambda e, x=x: e.tensor_tensor(
                        out=acc[:], in0=ox[x][:], in1=zx[x][:], op=ALU.mult))
                else:
                    sc.op('pool', [('zx', x), ('ox', x)], [('ox', x)], lambda e, x=x: e.tensor_tensor(
                        out=ox[x][:], in0=ox[x][:], in1=zx[x][:], op=ALU.mult))
                    if x == 1:
                        sc.op('dve', [('ox', 0), ('ox', x)], [('ox', 0)], lambda e, x=x: e.tensor_tensor(
                            out=acc[:], in0=acc[:], in1=ox[x][:], op=ALU.add))
                    else:
                        sc.op('dve', [('ox', 0), ('ox', x)], ['ob'], lambda e, x=x: e.tensor_tensor(
                            out=ob[:], in0=acc[:], in1=ox[x][:], op=ALU.add))
            sc.dma(STORE_Q, OT[0].rearrange("c (hl e) t -> e (c hl) t", e=64)[:, :, t0:t0 + 128],
                   ob[:].rearrange("p (h t) -> p h t", h=4), ['ob'], [])
            yield


_CACHE = {}


def kernel(**inputs):
    S = 8192
    ncores = 8
    if 'mk' not in _CACHE:
        _CACHE['mk'] = build_program(S)
    mk = _CACHE['mk']
    inp = {k: np.asarray(v) for k, v in inputs.items()}
    shared = host_inputs_shared(inp, S=S)
    shared = {k: np.ascontiguousarray(v, dtype=np.float32) for k, v in shared.items() if k in mk.inputs}
    in_maps = []
    for c in range(ncores):
        d = dict(shared)
        d['x'] = np.ascontiguousarray(inp['x'][c], dtype=np.float32)
        d['p'] = np.ascontiguousarray(inp['p'][:, c], dtype=np.float32)
        in_maps.append(d)
    res = run_bass_kernel_spmd(mk.nc, in_maps, core_ids=list(range(ncores)))
    return np.stack([np.asarray(r['out'], dtype=np.float32) for r in res.results], axis=0)
```

```python
import numpy as np
from contextlib import ExitStack
import concourse.bass as bass
import concourse.mybir as mybir
from concourse.bass_utils import run_bass_kernel_spmd

F32 = mybir.dt.float32
BF16 = mybir.dt.bfloat16
AF = mybir.ActivationFunctionType
ALU = mybir.AluOpType
AX = mybir.AxisListType

D = 1024
DEPTH = 2
DFF = 2816
NFC = DFF // 128
NSA_BASE, HG_BASE, RET_BASE, RW_BASE = 0, 652, 1676, 2700
EPS = 1e-6
STORE_Q = 'pool'


class _Keep:
    def __init__(self, es):
        self.es = es

    def __enter__(self):
        return self.es

    def __exit__(self, *a):
        return False


class Sched:
    EPOCH = 8000
    NDMA = 24

    def __init__(self, nc, es):
        self.nc = nc
        self.es = es
        self.E = {'pe': nc.tensor, 'dve': nc.vector, 'act': nc.scalar, 'pool': nc.gpsimd, 'sp': nc.sync}
        self.sems = []
        self.prog = {}
        for e in self.E:
            self.prog[e] = [self.new_sem(), 0]
        self.waited = {e: {} for e in self.E}
        self.lastw = {}
        self.readers = {}
        self.dma_pool = [self.new_sem() for _ in range(self.NDMA)]
        self.dma_val = {s: 0 for s in self.dma_pool}
        self.dma_next = 0
        self.n_inst = 0
        self.ns = None
        self.pe_sync = False

    def new_sem(self):
        h = self.es.enter_context(self.nc.semaphore("s%d" % len(self.sems)))
        self.sems.append(h)
        return len(self.sems) - 1

    def _wait(self, eng, deps):
        w = self.waited[eng]
        for s, v in deps.items():
            if w.get(s, 0) >= v:
                continue
            self.E[eng].wait_ge(self.sems[s], v)
            w[s] = v

    def _deps(self, reads, writes):
        deps = {}
        for k in reads:
            lw = self.lastw.get(k)
            if lw and deps.get(lw[0], 0) < lw[1]:
                deps[lw[0]] = lw[1]
        for k in writes:
            lw = self.lastw.get(k)
            if lw and deps.get(lw[0], 0) < lw[1]:
                deps[lw[0]] = lw[1]
            rd = self.readers.get(k)
            if rd:
                for s, v in rd.items():
                    if deps.get(s, 0) < v:
                        deps[s] = v
        return deps

    def _record(self, reads, writes, s, v):
        for k in writes:
            self.lastw[k] = (s, v)
            self.readers[k] = {}
        for k in reads:
            self.readers.setdefault(k, {})[s] = v

    def op(self, eng, r, w, fn):
        r = [self.nk(k) for k in r]
        w = [self.nk(k) for k in w]
        pk = [k for k in r if (isinstance(k, tuple) and k[0] == 'ps') or k == 'pb0']
        if pk:
            r = [k for k in r if k not in pk]
            w = list(w) + pk
        sync = False
        if eng == 'pes':
            eng, sync = 'pe', True
        deps = self._deps(r, w)
        pr = self.prog[eng]
        if eng == 'pe' and not sync:
            deps.pop(pr[0], None)
        self._wait(eng, deps)
        inst = fn(self.E[eng])
        pr[1] += 1
        inst.then_inc(self.sems[pr[0]], 1)
        self._record(r, w, pr[0], pr[1])
        self.n_inst += 1
        ret = (pr[0], pr[1])
        if pr[1] >= self.EPOCH:
            self.prog[eng] = [self.new_sem(), 0]
        return ret

    GLOBAL_KEYS = {'pb0', 'ident', 'identf', 'onesbd', 'gmask', 'rmask', 'rw_mask4', 'rw_lmask', 'rw_rmask'}

    def nk(self, k):
        if k == 'pb1':
            return 'pb0'
        if self.ns is None or k in self.GLOBAL_KEYS or (isinstance(k, tuple) and k[0] == 'ps'):
            return k
        return (self.ns, k)

    def run_streams(self, streams):
        live = list(streams)
        while live:
            for item in list(live):
                self.ns = item[0]
                try:
                    next(item[1])
                except StopIteration:
                    live.remove(item)
        self.ns = None

    def dma(self, eng, out, in_, r, w):
        r = [self.nk(k) for k in r]
        w = [self.nk(k) for k in w]
        s = self.dma_pool[self.dma_next % self.NDMA]
        self.dma_next += 1
        deps = self._deps(r, w)
        pv = self.dma_val[s]
        if pv and deps.get(s, 0) < pv:
            deps[s] = pv
        self._wait(eng, deps)
        self.E[eng].dma_start(out=out, in_=in_).then_inc(self.sems[s], 16)
        self.dma_val[s] = pv + 16
        self._record(r, w, s, pv + 16)
        self.n_inst += 1

    def barrier(self):
        allv = {}
        for e, (s, v) in self.prog.items():
            if v:
                allv[s] = v
        for s, v in self.dma_val.items():
            if v:
                allv[s] = v
        for e in self.E:
            d = dict(allv)
            self._wait(e, d)
        self.lastw = {}
        self.readers = {}


class MK:
    def __init__(self, S, last_layer_final=True):
        self.S = S
        self.nc = bass.Bass("TRN2", target_bir_lowering=False)
        self.inputs = {}

    def din(self, name, shape, dt=F32):
        t = self.nc.dram_tensor(name, list(shape), dt, kind="ExternalInput").ap()
        self.inputs[name] = t
        return t

    def dscr(self, name, shape, dt=F32):
        return self.nc.dram_tensor(name, list(shape), dt, kind="Internal").ap()

    def sb(self, es, name, shape, dt):
        self.uid = getattr(self, 'uid', 0) + 1
        return es.enter_context(self.nc.sbuf_tensor("%s_u%d" % (name, self.uid), list(shape), dt))


def _mm(sc, ps_key, ps_ap, pairs, rkeys):
    n = len(pairs)
    for i, (l, r) in enumerate(pairs):
        sc.op('pe', rkeys, [ps_key],
              lambda e, l=l, r=r, i=i: e.matmul(ps_ap, lhsT=l, rhs=r, start=(i == 0), stop=(i == n - 1)))


def p1_plan():
    cols = []
    groups = []

    def add(name, idx):
        groups.append((name, len(cols), len(idx)))
        cols.extend(idx)

    b = NSA_BASE
    for h in range(4):
        add('nsa_q%d' % h, list(range(b + 64 * h, b + 64 * h + 64)))
    add('nsa_kc', list(range(b + 256, b + 320)))
    add('nsa_vc', list(range(b + 320, b + 384)))
    add('nsa_ks', list(range(b + 384, b + 448)))
    add('nsa_kw', list(range(b + 512, b + 576)))
    for gi in range(12):
        add('nsa_g%d' % gi, [b + 640 + gi] * 64)
    b = HG_BASE
    for nm, off in (('hg_q', 0), ('hg_f', 256), ('hg_og', 768)):
        for c in range(2):
            add('%s%d' % (nm, c), list(range(b + off + 128 * c, b + off + 128 * c + 128)))
    b = RET_BASE

    def rot(base):
        out = []
        for h in range(4):
            for d in range(64):
                out.append(base + 64 * h + (d + 32) % 64)
        return out
    for nm, idx in (('ret_q', list(range(b, b + 256))), ('ret_qr', rot(b)),
                    ('ret_k', list(range(b + 256, b + 512))), ('ret_kr', rot(b + 256)),
                    ('ret_g', list(range(b + 768, b + 1024)))):
        for c in range(2):
            add('%s%d' % (nm, c), idx[128 * c:128 * c + 128])
    b = RW_BASE
    for c in range(8):
        add('rw%d' % c, list(range(b + 128 * c, b + 128 * c + 128)))
    nch = len(cols)
    tok = (list(range(NSA_BASE + 448, NSA_BASE + 512)) + list(range(NSA_BASE + 576, NSA_BASE + 640))
           + list(range(HG_BASE + 512, HG_BASE + 768)) + list(range(RET_BASE + 512, RET_BASE + 768)))
    cols.extend(tok)
    return np.array(cols, dtype=np.int64), groups, nch, len(tok)


P1_COLS, P1_GROUPS, P1_NCH, P1_NTOK = p1_plan()
P1_NC = len(P1_COLS)
GIDX = {g[0]: i for i, g in enumerate(P1_GROUPS)}
NG = len(P1_GROUPS)


class Dense:
    def __init__(self, mk, sc, es):
        self.mk, self.sc, self.nc = mk, sc, mk.nc
        nc = self.nc
        self.ps = [es.enter_context(nc.psum_tensor("ps%d" % i, [128, 512], F32)) for i in range(7)]
        pb = es.enter_context(nc.psum_tensor("pb0", [128, 1024], BF16))
        self.pb = [pb, pb]
        self.ps_pool = list(range(7))
        self.ps_i = 0
        self.ident = mk.sb(es, "ident", [128, 128], BF16)
        self.identf = mk.sb(es, "identf", [128, 128], F32)
        idin = mk.din("c_ident", [128, 128])
        sc.dma('sp', self.identf[:], idin[:, :], [], ['identf'])
        sc.op('dve', ['identf'], ['ident'], lambda e: e.tensor_copy(out=self.ident[:], in_=self.identf[:]))
        self.ev = 0

    def next_ps(self):
        i = self.ps_pool[self.ps_i % len(self.ps_pool)]
        self.ps_i += 1
        return i

    def evac_eng(self):
        self.ev += 1
        return 'act' if self.ev % 2 else 'dve'

    def copy(self, eng, out, in_, r, w):
        if eng == 'act':
            self.sc.op('act', r, w, lambda e: e.activation(out=out, in_=in_, func=AF.Copy))
        else:
            self.sc.op(eng, r, w, lambda e: e.tensor_copy(out=out, in_=in_))

    def load_w(self, es, name, src, K, N, stage):
        sc = self.sc
        kc = K // 128
        dst = self.mk.sb(es, name, [128, kc, N], BF16)
        engs = ['pool', 'dve', 'act']
        for c in range(kc):
            for n0 in range(0, N, 2048):
                n1 = min(N, n0 + 2048)
                si = self.ev % 2
                self.ev += 1
                st = stage[si]
                sc.dma('sp', st[:, 0:n1 - n0], src[c * 128:(c + 1) * 128, n0:n1], [], [('wst', si)])
                self.copy(engs[self.ev % 3], dst[:, c, n0:n1], st[:, 0:n1 - n0], [('wst', si)], [name])
        return dst

    def front(self, bufs, h_src, t0, nsub, gain_key):
        sc = self.sc
        hk, xnT, xn, junk, ss, gain = bufs['h'], bufs['xnT'], bufs['xn'], bufs['junk'], bufs['ss'], bufs[gain_key]
        pb = self.pb[0]
        for sub in range(nsub):
            r0 = t0 + sub * 128
            sc.dma('sp', hk[:, sub, :], h_src[r0:r0 + 128, :], [], [('h', sub)])
            self.norm_T(bufs, hk[:, sub, :], ('h', sub), gain, gain_key, sub)

    def rstd(self, h_key, h_ap, junk, ss):
        sc = self.sc
        sc.op('act', [h_key], ['junk', 'ss'], lambda e: e.activation(
            out=junk[:], in_=h_ap, func=AF.Square, scale=1.0 / 32.0, accum_out=ss[:, 0:1]))
        sc.op('dve', ['ss'], ['ss'], lambda e: e.tensor_scalar(
            out=ss[:, 1:2], in0=ss[:, 0:1], scalar1=EPS, scalar2=None, op0=ALU.add))
        sc.op('act', ['ss'], ['ss'], lambda e: e.activation(out=ss[:, 3:4], in_=ss[:, 1:2], func=AF.Sqrt))
        sc.op('dve', ['ss'], ['ss'], lambda e: e.reciprocal(out=ss[:, 2:3], in_=ss[:, 3:4]))

    def norm_T(self, bufs, h_ap, h_key, gain, gain_key, sub):
        sc = self.sc
        xnT, xn, junk, ss = bufs['xnT'], bufs['xn'], bufs['junk'], bufs['ss']
        pb = self.pb[0]
        self.rstd(h_key, h_ap, junk, ss)
        sc.op('dve', [h_key, 'ss', gain_key], ['xn'], lambda e: e.scalar_tensor_tensor(
            out=xn[:], in0=h_ap, scalar=ss[:, 2:3], in1=gain[:], op0=ALU.mult, op1=ALU.mult))
        for k in range(8):
            sc.op('pe', ['xn', 'ident'], ['pb0'], lambda e, k=k: e.transpose(
                out=pb[:, k * 128:(k + 1) * 128], in_=xn[:, k * 128:(k + 1) * 128], identity=self.ident[:]))
        sc.op('act', ['pb0'], ['xnT'], lambda e: e.activation(
            out=xnT[:, :, sub * 128:(sub + 1) * 128], in_=pb[:].rearrange("p (k t) -> p k t", k=8), func=AF.Copy))

    def front_bufs(self, es, nsub, gains):
        mk = self.mk
        b = {'h': mk.sb(es, "f_h", [128, nsub, D], F32), 'xnT': mk.sb(es, "f_xnT", [128, 8, nsub * 128], BF16),
             'xn': mk.sb(es, "f_xn", [128, D], BF16), 'junk': mk.sb(es, "f_junk", [128, D], BF16),
             'ss': mk.sb(es, "f_ss", [128, 4], F32)}
        for key, src in gains.items():
            b[key] = mk.sb(es, "f_" + key, [128, D], F32)
            self.sc.dma('sp', b[key][:], src, [], [key])
        return b

    def pass_p1(self, L, h_src, w1_src, gain_src, UC, UT):
        sc, mk, S = self.sc, self.mk, self.mk.S
        with ExitStack() as es:
            stage = [mk.sb(es, "wst%d" % i, [128, 2048], F32) for i in range(2)]
            W1 = self.load_w(es, "W1", w1_src, D, P1_NC, stage)
            fb = self.front_bufs(es, 4, {'gain': gain_src})
            ost = [mk.sb(es, "ost%d" % i, [128, 640], F32) for i in range(4)]
            oi = 0
            for t0 in range(0, S, 512):
                self.front(fb, h_src, t0, 4, 'gain')
                xnT = fb['xnT']
                for gi, (name, off, M) in enumerate(P1_GROUPS):
                    pi = self.next_ps()
                    ps = self.ps[pi]
                    _mm(sc, ('ps', pi), ps[0:M, :],
                        [(W1[:, k, off:off + M], xnT[:, k, :]) for k in range(8)], ['W1', 'xnT'])
                    o = oi % 4
                    oi += 1
                    self.copy(self.evac_eng(), ost[o][0:M, 0:512], ps[0:M, :], [('ps', pi)], [('ost', o)])
                    sc.dma(STORE_Q, UC[gi, 0:M, t0:t0 + 512], ost[o][0:M, 0:512], [('ost', o)], [])
                for sub in range(4):
                    o = oi % 4
                    oi += 1
                    for (c0, c1) in ((0, 512), (512, P1_NTOK)):
                        pi = self.next_ps()
                        ps = self.ps[pi]
                        _mm(sc, ('ps', pi), ps[:, 0:c1 - c0],
                            [(xnT[:, k, sub * 128:(sub + 1) * 128], W1[:, k, P1_NCH + c0:P1_NCH + c1])
                             for k in range(8)], ['W1', 'xnT'])
                        self.copy(self.evac_eng(), ost[o][:, c0:c1], ps[:, 0:c1 - c0], [('ps', pi)], [('ost', o)])
                    r0 = t0 + sub * 128
                    sc.dma(STORE_Q, UT[r0:r0 + 128, :], ost[o][:, 0:P1_NTOK], [('ost', o)], [])
            sc.barrier()

    def pass_merge(self, L, h_src, h_dst, OT, wg_src, wb_src, wo_src, bg_src, gain_src):
        sc, mk, S = self.sc, self.mk, self.mk.S
        with ExitStack() as es:
            stage = [mk.sb(es, "wst%d" % i, [128, 2048], F32) for i in range(2)]
            Wg = [self.load_w(es, "Wg%d" % m, wg_src[m], D, D, stage) for m in range(4)]
            Wb = [self.load_w(es, "Wb%d" % m, wb_src[m], 256, D, stage) for m in range(4)]
            Wo = self.load_w(es, "Wo", wo_src, D, D, stage)
            bg = mk.sb(es, "bg", [128, 32], F32)
            sc.dma('sp', bg[:], bg_src, [], ['bg'])
            NS = 2
            TT = NS * 128
            fb = self.front_bufs(es, NS, {'gain': gain_src})
            ot = mk.sb(es, "m_ot", [128, 8, TT], BF16)
            gsb = [mk.sb(es, "m_g%d" % i, [128, TT], F32) for i in range(2)]
            acc = mk.sb(es, "m_acc", [128, TT], F32)
            tmp = mk.sb(es, "m_tmp", [128, TT], F32)
            mT = mk.sb(es, "m_mT", [128, 8, TT], BF16)
            hn = mk.sb(es, "m_hn", [128, D], F32)
            gi_ = 0
            for t0 in range(0, S, TT):
                self.front(fb, h_src, t0, NS, 'gain')
                xnT = fb['xnT']
                for m in range(4):
                    for c in range(2):
                        sc.dma('sp', ot[:, m * 2 + c, :], OT[m, c, :, t0:t0 + TT], [], ['ot'])
                for j in range(8):
                    for m in range(4):
                        pg = self.next_ps()
                        _mm(sc, ('ps', pg), self.ps[pg][:, 0:TT],
                            [(Wg[m][:, k, j * 128:(j + 1) * 128], xnT[:, k, :]) for k in range(8)],
                            ['Wg%d' % m, 'xnT'])
                        pbr = self.next_ps()
                        _mm(sc, ('ps', pbr), self.ps[pbr][:, 0:TT],
                            [(Wb[m][:, c, j * 128:(j + 1) * 128], ot[:, m * 2 + c, :]) for c in range(2)],
                            ['Wb%d' % m, 'ot'])
                        g = gi_ % 2
                        gi_ += 1
                        sc.op('act', [('ps', pg), 'bg'], [('gsb', g)], lambda e, g=g, pg=pg, m=m, j=j: e.activation(
                            out=gsb[g][:], in_=self.ps[pg][:, 0:TT], func=AF.Sigmoid,
                            bias=bg[:, m * 8 + j:m * 8 + j + 1], scale=1.0))
                        if m == 0:
                            sc.op('dve', [('gsb', g), ('ps', pbr)], ['acc'], lambda e, g=g, pbr=pbr: e.tensor_tensor(
                                out=acc[:], in0=gsb[g][:], in1=self.ps[pbr][:, 0:TT], op=ALU.mult))
                        else:
                            sc.op('dve', [('gsb', g), ('ps', pbr)], ['tmp'], lambda e, g=g, pbr=pbr: e.tensor_tensor(
                                out=tmp[:], in0=gsb[g][:], in1=self.ps[pbr][:, 0:TT], op=ALU.mult))
                            if m < 3:
                                sc.op('pool', ['tmp', 'acc'], ['acc'], lambda e: e.tensor_tensor(
                                    out=acc[:], in0=acc[:], in1=tmp[:], op=ALU.add))
                            else:
                                sc.op('pool', ['tmp', 'acc'], ['mT'], lambda e, j=j: e.tensor_tensor(
                                    out=mT[:, j, :], in0=acc[:], in1=tmp[:], op=ALU.add))
                for sub in range(NS):
                    for half in range(2):
                        po = self.next_ps()
                        _mm(sc, ('ps', po), self.ps[po][:, :],
                            [(mT[:, k, sub * 128:(sub + 1) * 128], Wo[:, k, half * 512:(half + 1) * 512])
                             for k in range(8)], ['Wo', 'mT'])
                        sc.op('dve', [('ps', po), ('h', sub)], ['hn'], lambda e, po=po, sub=sub, half=half: e.tensor_tensor(
                            out=hn[:, half * 512:(half + 1) * 512], in0=fb['h'][:, sub, half * 512:(half + 1) * 512],
                            in1=self.ps[po][:, :], op=ALU.add))
                    r0 = t0 + sub * 128
                    sc.dma(STORE_Q, h_dst[r0:r0 + 128, :], hn[:], ['hn'], [])
            sc.barrier()

    def pass_ffn(self, L, h_src, h_dst, wfg_src, wfu_src, wfd_src, gain_src):
        sc, mk, S = self.sc, self.mk, self.mk.S
        with ExitStack() as es:
            stage = [mk.sb(es, "wst%d" % i, [128, 2048], F32) for i in range(2)]
            Wfg = self.load_w(es, "Wfg", wfg_src, D, DFF, stage)
            Wfu = self.load_w(es, "Wfu", wfu_src, D, DFF, stage)
            Wfd = self.load_w(es, "Wfd", wfd_src, DFF, D, stage)
            NS = 2
            TT = NS * 128
            fb = self.front_bufs(es, NS, {'gain': gain_src})
            hid = mk.sb(es, "f_hid", [128, NFC, TT], BF16)
            gsb = [mk.sb(es, "f_g%d" % i, [128, TT], F32) for i in range(2)]
            hn = mk.sb(es, "f_hn", [128, D], F32)
            gi_ = 0
            for t0 in range(0, S, TT):
                self.front(fb, h_src, t0, NS, 'gain')
                xnT = fb['xnT']
                for f in range(NFC):
                    pg = self.next_ps()
                    _mm(sc, ('ps', pg), self.ps[pg][:, 0:TT],
                        [(Wfg[:, k, f * 128:(f + 1) * 128], xnT[:, k, :]) for k in range(8)], ['Wfg', 'xnT'])
                    pu = self.next_ps()
                    _mm(sc, ('ps', pu), self.ps[pu][:, 0:TT],
                        [(Wfu[:, k, f * 128:(f + 1) * 128], xnT[:, k, :]) for k in range(8)], ['Wfu', 'xnT'])
                    g = gi_ % 2
                    gi_ += 1
                    sc.op('act', [('ps', pg)], [('gsb', g)], lambda e, g=g, pg=pg: e.activation(
                        out=gsb[g][:], in_=self.ps[pg][:, 0:TT], func=AF.Silu))
                    sc.op('dve', [('gsb', g), ('ps', pu)], ['hid'], lambda e, g=g, pu=pu, f=f: e.tensor_tensor(
                        out=hid[:, f, :], in0=gsb[g][:], in1=self.ps[pu][:, 0:TT], op=ALU.mult))
                for sub in range(NS):
                    for half in range(2):
                        po = self.next_ps()
                        _mm(sc, ('ps', po), self.ps[po][:, :],
                            [(hid[:, f, sub * 128:(sub + 1) * 128], Wfd[:, f, half * 512:(half + 1) * 512])
                             for f in range(NFC)], ['Wfd', 'hid'])
                        sc.op('dve', [('ps', po), ('h', sub)], ['hn'], lambda e, po=po, sub=sub, half=half: e.tensor_tensor(
                            out=hn[:, half * 512:(half + 1) * 512], in0=fb['h'][:, sub, half * 512:(half + 1) * 512],
                            in1=self.ps[po][:, :], op=ALU.add))
                    r0 = t0 + sub * 128
                    sc.dma(STORE_Q, h_dst[r0:r0 + 128, :], hn[:], ['hn'], [])
            sc.barrier()

    def pass_ple(self, L, h_src, h_dst, p_src, wpg_src, wpp_src, gain_src, final_gain_src):
        sc, mk, S = self.sc, self.mk, self.mk.S
        with ExitStack() as es:
            stage = [mk.sb(es, "wst%d" % i, [128, 2048], F32) for i in range(2)]
            Wpg = self.load_w(es, "Wpg", wpg_src, D, D, stage)
            Wpp = self.load_w(es, "Wpp", wpp_src, 256, D, stage)
            NS = 4
            gains = {'gain': gain_src}
            if final_gain_src is not None:
                gains['fgain'] = final_gain_src
            fb = self.front_bufs(es, NS, gains)
            pt = mk.sb(es, "p_pt", [128, 256], F32)
            ptb = mk.sb(es, "p_ptb", [128, 256], BF16)
            pT = mk.sb(es, "p_pT", [128, 2, 128], BF16)
            gsb = mk.sb(es, "p_g", [128, 512], F32)
            hn = mk.sb(es, "p_hn", [128, D], F32)
            ho = mk.sb(es, "p_ho", [128, D], F32)
            pbk = self.pb[1]
            for t0 in range(0, S, NS * 128):
                self.front(fb, h_src, t0, NS, 'gain')
                xnT = fb['xnT']
                for sub in range(NS):
                    r0 = t0 + sub * 128
                    sc.dma('sp', pt[:], p_src[r0:r0 + 128, :], [], ['pt'])
                    sc.op('pool', ['pt'], ['ptb'], lambda e: e.tensor_copy(out=ptb[:], in_=pt[:]))
                    for c in range(2):
                        sc.op('pe', ['ptb', 'ident'], ['pb1'], lambda e, c=c: e.transpose(
                            out=pbk[:, c * 128:(c + 1) * 128], in_=ptb[:, c * 128:(c + 1) * 128], identity=self.ident[:]))
                    sc.op('dve', ['pb1'], ['pT'], lambda e: e.tensor_copy(
                        out=pT[:], in_=pbk[:, 0:256].rearrange("p (c t) -> p c t", c=2)))
                    for half in range(2):
                        pg = self.next_ps()
                        _mm(sc, ('ps', pg), self.ps[pg][:, :],
                            [(xnT[:, k, sub * 128:(sub + 1) * 128], Wpg[:, k, half * 512:(half + 1) * 512])
                             for k in range(8)], ['Wpg', 'xnT'])
                        pp = self.next_ps()
                        _mm(sc, ('ps', pp), self.ps[pp][:, :],
                            [(pT[:, c, :], Wpp[:, c, half * 512:(half + 1) * 512]) for c in range(2)], ['Wpp', 'pT'])
                        sc.op('act', [('ps', pg)], ['gsb'], lambda e, pg=pg: e.activation(
                            out=gsb[:], in_=self.ps[pg][:, :], func=AF.Sigmoid))
                        sc.op('dve', ['gsb', ('ps', pp)], ['gsb'], lambda e, pp=pp: e.tensor_tensor(
                            out=gsb[:], in0=gsb[:], in1=self.ps[pp][:, :], op=ALU.mult))
                        sc.op('pool', ['gsb', ('h', sub)], ['hn'], lambda e, sub=sub, half=half: e.tensor_tensor(
                            out=hn[:, half * 512:(half + 1) * 512], in0=fb['h'][:, sub, half * 512:(half + 1) * 512],
                            in1=gsb[:], op=ALU.add))
                    if final_gain_src is None:
                        sc.dma(STORE_Q, h_dst[r0:r0 + 128, :], hn[:], ['hn'], [])
                    else:
                        ss, junk = fb['ss'], fb['junk']
                        self.rstd('hn', hn[:], junk, ss)
                        sc.op('dve', ['hn', 'ss', 'fgain'], ['ho'], lambda e: e.scalar_tensor_tensor(
                            out=ho[:], in0=hn[:], scalar=ss[:, 2:3], in1=fb['fgain'][:], op0=ALU.mult, op1=ALU.mult))
                        sc.dma(STORE_Q, h_dst[r0:r0 + 128, :], ho[:], ['ho'], [])
            sc.barrier()


def rep128(v):
    return np.ascontiguousarray(np.broadcast_to(np.asarray(v, np.float32)[None, :], (128, v.shape[-1])))


def build_program(S, debug=False, ext_ot=False, layers=DEPTH, skip_mixers=False, mixers=('nsa', 'hg', 'ret', 'rw'), dense=True):
    mk = MK(S)
    nc = mk.nc
    if debug:
        mk.dscr = lambda name, shape, dt=F32: nc.dram_tensor(name, list(shape), dt, kind="ExternalOutput").ap()
    x = mk.din("x", [S, D])
    p = mk.din("p", [DEPTH, S, 256])
    out = nc.dram_tensor("out", [S, D], F32, kind="ExternalOutput").ap()
    w = {}
    for L in range(layers):
        w[L] = dict(
            w1=mk.din("w1_%d" % L, [D, P1_NC]), g_mix=mk.din("g_mix_%d" % L, [128, D]),
            wg=mk.din("wg_%d" % L, [4, D, D]), wb=mk.din("wb_%d" % L, [4, 256, D]), wo=mk.din("wo_%d" % L, [D, D]),
            bg=mk.din("bg_%d" % L, [128, 32]),
            g_ffn=mk.din("g_ffn_%d" % L, [128, D]), wfg=mk.din("wfg_%d" % L, [D, DFF]),
            wfu=mk.din("wfu_%d" % L, [D, DFF]), wfd=mk.din("wfd_%d" % L, [DFF, D]),
            g_ple=mk.din("g_ple_%d" % L, [128, D]), wpg=mk.din("wpg_%d" % L, [D, D]), wpp=mk.din("wpp_%d" % L, [256, D]),
        )
    g_final = mk.din("g_final", [128, D])
    lbz = mk.din("hg_lbz", [128, DEPTH * 2])
    for L in range(layers):
        w[L].update(hgn=mk.din("hg_gn_%d" % L, [128, 2]))
        w[L].update(retgb=mk.din("ret_gb_%d" % L, [128, 4]))
        w[L].update(ns_w1k=mk.din("ns_w1k_%d" % L, [64, 32 * 128]), ns_w1v=mk.din("ns_w1v_%d" % L, [64, 32 * 128]),
                    ns_w2k=mk.din("ns_w2k_%d" % L, [128, 64]), ns_w2v=mk.din("ns_w2v_%d" % L, [128, 64]),
                    ns_pk=mk.din("ns_pk_%d" % L, [64, 32]), ns_pv=mk.din("ns_pv_%d" % L, [64, 32]))
        w[L].update(rw_wup=mk.din("rw_wup_%d" % L, [64, 256]), rw_aup=mk.din("rw_aup_%d" % L, [64, 256]),
                    rw_gup=mk.din("rw_gup_%d" % L, [128, 256]), rw_pvec=mk.din("rw_pvec_%d" % L, [128, 24]))
    UC = mk.dscr("UC", [NG, 128, S])
    UT = mk.dscr("UT", [S, P1_NTOK])
    if ext_ot:
        OT = mk.din("OT", [4, 2, 128, S], BF16)
    else:
        OT = mk.dscr("OT", [4, 2, 128, S], BF16)
    hA = mk.dscr("hA", [S, D])
    hB = mk.dscr("hB", [S, D])
    hC = mk.dscr("hC", [S, D])
    with ExitStack() as es:
        sc = Sched(nc, es)
        dn = Dense(mk, sc, es)
        gla = GLA(dn, es)
        rwk = RWKV(dn, gla, es)
        nsa = NSA(dn, gla, es)
        sc.barrier()
        h_in = x
        for L in range(layers):
            wl = w[L]
            dn.pass_p1(L, h_in, wl['w1'], wl['g_mix'], UC, UT)
            if not skip_mixers:
                with ExitStack() as esx:
                    streams = []
                    if 'nsa' in mixers:
                        streams.append(('nsa', nsa.run(L, UC, UT, OT, {
                            'w1r': [wl['ns_w1k'], wl['ns_w1v']], 'w2': [wl['ns_w2k'][:, :], wl['ns_w2v'][:, :]],
                            'posT': [wl['ns_pk'][:, :], wl['ns_pv'][:, :]]}, esx)))
                    if 'rw' in mixers:
                        streams.append(('rw', rwk.run(L, UC, OT, {'w_up': wl['rw_wup'][:, :], 'a_up': wl['rw_aup'][:, :],
                                                                  'g_up': wl['rw_gup'][:, :], 'pvec': wl['rw_pvec'][:, :]}, esx)))
                    sc.run_streams(streams)
                    sc.barrier()
                with ExitStack() as esy:
                    streams = []
                    if 'hg' in mixers:
                        streams.append(('hg', gla.run_hgrn(L, UC, UT, OT, lbz[:, :], wl['hgn'][:, :], esy)))
                    if 'ret' in mixers:
                        streams.append(('ret', gla.run_ret(L, UC, UT, OT, wl['retgb'][:, :], esy)))
                    sc.run_streams(streams)
                    sc.barrier()
            if not dense:
                continue
            dn.pass_merge(L, h_in, hA, OT, wl['wg'], wl['wb'], wl['wo'], wl['bg'], wl['g_mix'])
            dn.pass_ffn(L, hA, hB, wl['wfg'], wl['wfu'], wl['wfd'], wl['g_ffn'])
            last = (L == layers - 1)
            dn.pass_ple(L, hB, out if last else hC, p[L], wl['wpg'], wl['wpp'], wl['g_ple'], g_final if last else None)
            h_in = hC
        sc.barrier()
    mk.n_inst = sc.n_inst
    return mk


def host_inputs_shared(inp, layers=DEPTH, S=8192):
    d = {"c_ident": np.eye(128, dtype=np.float32)}
    for L in range(layers):
        d["w1_%d" % L] = np.ascontiguousarray(inp['w_in'][L][:, P1_COLS])
        d["g_mix_%d" % L] = rep128(inp['norm_mix'][L])
        d["wg_%d" % L] = np.ascontiguousarray(inp['w_gate'][L])
        d["wb_%d" % L] = np.ascontiguousarray(inp['w_branch'][L])
        d["wo_%d" % L] = np.ascontiguousarray(inp['w_out'][L])
        d["bg_%d" % L] = np.ascontiguousarray(inp['b_gate'][L].reshape(4, 8, 128).transpose(2, 0, 1).reshape(128, 32))
        d["g_ffn_%d" % L] = rep128(inp['norm_ffn'][L])
        d["wfg_%d" % L] = np.ascontiguousarray(inp['w_ffn_gate'][L])
        d["wfu_%d" % L] = np.ascontiguousarray(inp['w_ffn_up'][L])
        d["wfd_%d" % L] = np.ascontiguousarray(inp['w_ffn_down'][L])
        d["g_ple_%d" % L] = rep128(inp['norm_ple'][L])
        d["wpg_%d" % L] = np.ascontiguousarray(inp['w_ple_gate'][L])
        d["wpp_%d" % L] = np.ascontiguousarray(inp['w_ple_proj'][L])
    d["g_final"] = rep128(inp['norm_final'])
    d.update(gla_consts())
    d["c_cos"], d["c_sin"] = rope_tables(S)
    d.update(rwkv_consts())
    d.update(nsa_consts(S))
    for L in range(layers):
        r1 = lambda w: np.ascontiguousarray(w.reshape(32, 64, 128).transpose(1, 0, 2).reshape(64, 32 * 128))
        d["ns_w1k_%d" % L] = r1(inp['nsa_cmp_k1'][L])
        d["ns_w1v_%d" % L] = r1(inp['nsa_cmp_v1'][L])
        d["ns_w2k_%d" % L] = np.ascontiguousarray(inp['nsa_cmp_k2'][L])
        d["ns_w2v_%d" % L] = np.ascontiguousarray(inp['nsa_cmp_v2'][L])
        d["ns_pk_%d" % L] = np.ascontiguousarray(inp['nsa_pos_k'][L].T)
        d["ns_pv_%d" % L] = np.ascontiguousarray(inp['nsa_pos_v'][L].T)
    for L in range(layers):
        c2 = lambda v: v.reshape(2, 128).T
        pvec = np.zeros((128, 24), np.float32)
        pvec[:, 0:8] = inp['rwkv_mu'][L].reshape(8, 128).T
        for j, nm in enumerate(['rwkv_w0', 'rwkv_a0', 'rwkv_k_k', 'rwkv_k_a', 'rwkv_r_k', 'rwkv_norm_g', 'rwkv_norm_b']):
            pvec[:, 8 + 2 * j:10 + 2 * j] = c2(inp[nm][L])
        d["rw_pvec_%d" % L] = pvec
        d["rw_wup_%d" % L] = np.ascontiguousarray(inp['rwkv_w_up'][L])
        d["rw_aup_%d" % L] = np.ascontiguousarray(inp['rwkv_a_up'][L])
        d["rw_gup_%d" % L] = np.ascontiguousarray(inp['rwkv_g_up'][L])
    d["hg_lbz"] = np.ascontiguousarray(inp['hgrn_lb_logits'].reshape(DEPTH, 2, 128).transpose(2, 0, 1).reshape(128, DEPTH * 2))
    for L in range(layers):
        d["hg_gn_%d" % L] = np.ascontiguousarray(inp['hgrn_norm'][L].reshape(2, 128).T)
        d["ret_gb_%d" % L] = np.ascontiguousarray(np.concatenate(
            [inp['ret_norm_g'][L].reshape(2, 128).T, inp['ret_norm_b'][L].reshape(2, 128).T], axis=1))
    return d


GLA_C = 32
RET_LOGG = [float(np.log(1.0 - 2.0 ** (-5.0 - h))) for h in range(4)]


def gla_consts():
    s = np.arange(128)
    m = ((s[:, None] // 32 == s[None, :] // 32) & (s[:, None] <= s[None, :])).astype(np.float32)
    d = {"c_gmask": np.ascontiguousarray(np.tile(m, (1, 4)))}
    rm = np.ones((128, 128), np.float32)
    rm[:, ::32] = 0.0
    d["c_rmask"] = rm
    bd = np.zeros((128, 128), np.float32)
    bd[:64, :64] = 1.0
    bd[64:, 64:] = 1.0
    d["c_onesbd"] = bd
    i = (np.arange(128) % 32).astype(np.float64)
    ret = np.zeros((2, 3, 128, 128), np.float32)
    for hp in range(2):
        for hl in range(2):
            lg = RET_LOGG[hp * 2 + hl]
            ret[hp, 0, hl * 64:(hl + 1) * 64, :] = np.exp((i + 1) * lg)[None, :]
            ret[hp, 1, hl * 64:(hl + 1) * 64, :] = 0.125 * np.exp(-(i + 1) * lg)[None, :]
            ret[hp, 2, hl * 64:(hl + 1) * 64, :] = 0.125 * np.exp((31 - i) * lg)[None, :]
    d["c_retdec"] = ret
    g32 = np.zeros((128, 2), np.float32)
    for hp in range(2):
        for hl in range(2):
            g32[hl * 64:(hl + 1) * 64, hp] = np.exp(32 * RET_LOGG[hp * 2 + hl])
    d["c_retg32"] = g32
    return d


def rope_tables(S):
    pos = np.arange(S, dtype=np.float32)
    inv = (10000.0 ** (-np.arange(0, 64, 2, dtype=np.float32) / 64)).astype(np.float32)
    ang = pos[None, :] * inv[:, None]
    c = np.cos(ang).astype(np.float32)
    s_ = np.sin(ang).astype(np.float32)
    cos64 = np.concatenate([c, c], 0)
    sin64 = np.concatenate([-s_, s_], 0)
    return (np.ascontiguousarray(np.concatenate([cos64, cos64], 0)),
            np.ascontiguousarray(np.concatenate([sin64, sin64], 0)))


class GLA:
    def __init__(self, dn, es):
        self.dn, self.mk, self.sc, self.nc = dn, dn.mk, dn.sc, dn.nc
        mk, sc = self.mk, self.sc
        self.gmask = mk.sb(es, "gmask", [128, 512], F32)
        self.rmask = mk.sb(es, "rmask", [128, 128], F32)
        self.onesbd = mk.sb(es, "onesbd", [128, 128], BF16)
        tmpf = mk.sb(es, "g_tmpf", [128, 128], F32)
        sc.dma('sp', self.gmask[:], mk.din("c_gmask", [128, 512])[:, :], [], ['gmask'])
        sc.dma('sp', self.rmask[:], mk.din("c_rmask", [128, 128])[:, :], [], ['rmask'])
        sc.dma('sp', tmpf[:], mk.din("c_onesbd", [128, 128])[:, :], [], ['g_tmpf'])
        sc.op('dve', ['g_tmpf'], ['onesbd'], lambda e: e.tensor_copy(out=self.onesbd[:], in_=tmpf[:]))
        self.c_retdec = mk.din("c_retdec", [2, 3, 128, 128])
        self.c_retg32 = mk.din("c_retg32", [128, 2])
        self.c_cos = mk.din("c_cos", [128, mk.S])
        self.c_sin = mk.din("c_sin", [128, mk.S])

    def alloc_core(self, es):
        mk = self.mk
        b = {}
        b['PT'] = mk.sb(es, "gl_PT", [128, 512], BF16)
        b['stf'] = [mk.sb(es, "gl_stf%d" % hp, [128, 128], F32) for hp in range(2)]
        b['snap'] = [[mk.sb(es, "gl_snap%d_%d" % (par, hp), [128, 5, 128], BF16) for hp in range(2)] for par in range(2)]
        b['osb'] = mk.sb(es, "gl_osb", [128, 256], F32)
        for hp in range(2):
            self.sc.op('dve', [], [('stf', hp)], lambda e, hp=hp: e.memset(b['stf'][hp][:], 0.0))
            self.sc.op('pool', [], [('snap', 1, hp, 4)], lambda e, hp=hp: e.memset(b['snap'][1][hp][:, 4, :], 0.0))
        return b

    def core(self, b, ti, qs, ks, kd, kd3, v, dec, keys):
        sc, dn = self.sc, self.dn
        par = ti % 2
        pA = dn.next_ps()
        A = dn.ps[pA]
        for h in range(4):
            hp, hl = h // 2, h % 2
            sl = slice(hl * 64, (hl + 1) * 64)
            sc.op('pes', [keys['qs'][hp], keys['ks'][hp]], [('ps', pA)], lambda e, h=h, hp=hp, sl=sl: e.matmul(
                A[:, h * 128:(h + 1) * 128], lhsT=ks[hp][sl, :], rhs=qs[hp][sl, :], start=True, stop=True))
        sc.op('dve', [('ps', pA), 'gmask'], ['gl_PT'], lambda e: e.tensor_tensor(
            out=b['PT'][:], in0=A[:, :], in1=self.gmask[:], op=ALU.mult))
        pK = [dn.next_ps(), dn.next_ps()]
        for hp in range(2):
            for c in range(4):
                K = dn.ps[pK[hp]]
                rsl = slice(c * 32, (c + 1) * 32) if c < 3 else slice(64, 128)
                kk_ = kd if c < 3 else kd3
                sc.op('pes', [keys['kd'], keys['v']], [('ps', pK[hp])], lambda e, hp=hp, c=c, K=K, rsl=rsl, kk_=kk_: e.matmul(
                    K[:, c * 128:(c + 1) * 128], lhsT=kk_[rsl, hp * 128:(hp + 1) * 128],
                    rhs=v[rsl, hp * 128:(hp + 1) * 128], start=True, stop=True))
        for c in range(4):
            for hp in range(2):
                K = dn.ps[pK[hp]]
                sc.op('dve', [('ps', pK[hp]), ('stf', hp), keys['dec'][hp]], [('stf', hp)],
                      lambda e, hp=hp, c=c, K=K: e.scalar_tensor_tensor(
                          out=b['stf'][hp][:], in0=b['stf'][hp][:], scalar=dec(hp, c),
                          in1=K[:, c * 128:(c + 1) * 128], op0=ALU.mult, op1=ALU.add))
                sc.op('act', [('stf', hp)], [('snap', par, hp, c + 1)], lambda e, hp=hp, c=c: e.activation(
                    out=b['snap'][par][hp][:, c + 1, :], in_=b['stf'][hp][:], func=AF.Copy))
        pB = dn.next_ps()
        B = dn.ps[pB]
        for h in range(4):
            hp, hl = h // 2, h % 2
            sl = slice(hl * 64, (hl + 1) * 64)
            sc.op('pes', ['gl_PT', keys['v']], [('ps', pB)], lambda e, h=h, hp=hp, sl=sl: e.matmul(
                B[sl, hp * 128:(hp + 1) * 128], lhsT=v[:, h * 64:(h + 1) * 64], rhs=b['PT'][:, h * 128:(h + 1) * 128],
                start=True, stop=False))
            for c in range(4):
                if c == 0:
                    st = b['snap'][1 - par][hp][sl, 4, hl * 64:(hl + 1) * 64]
                    skey = ('snap', 1 - par, hp, 4)
                else:
                    st = b['snap'][par][hp][sl, c, hl * 64:(hl + 1) * 64]
                    skey = ('snap', par, hp, c)
                sc.op('pes', [skey, keys['qs'][hp]], [('ps', pB)], lambda e, hp=hp, sl=sl, c=c, st=st: e.matmul(
                    B[sl, hp * 128 + c * 32:hp * 128 + (c + 1) * 32], lhsT=st, rhs=qs[hp][sl, c * 32:(c + 1) * 32],
                    start=False, stop=(c == 3)))
        sc.op('act', [('ps', pB)], ['gl_osb'], lambda e: e.activation(out=b['osb'][:], in_=B[:, 0:256], func=AF.Copy))

    def run_hgrn(self, L, UC, UT, OT, lbz_src, hgn_src, es_ext):
        sc, mk, dn, S = self.sc, self.mk, self.dn, self.mk.S
        with _Keep(es_ext) as es:
            b = self.alloc_core(es)
            lbz = mk.sb(es, "hg_lbz", [128, DEPTH * 2], F32)
            lbe = mk.sb(es, "hg_lbe", [128, DEPTH * 2], F32)
            lbs = mk.sb(es, "hg_lbs", [128, 8], F32)
            gn = mk.sb(es, "hg_gn", [128, 2], F32)
            sc.dma('sp', lbz[:], lbz_src, [], ['lbz'])
            sc.dma('sp', gn[:], hgn_src, [], ['hg_gn'])
            sc.op('act', ['lbz'], ['lbe'], lambda e: e.activation(out=lbe[:], in_=lbz[:], func=AF.Exp))
            sc.op('dve', ['lbe'], ['lbs'], lambda e: e.tensor_tensor(
                out=lbs[:, 0:2], in0=lbe[:, 0:2], in1=lbe[:, 2:4], op=ALU.add))
            for l in range(2, DEPTH):
                sc.op('dve', ['lbe', 'lbs'], ['lbs'], lambda e, l=l: e.tensor_tensor(
                    out=lbs[:, 0:2], in0=lbs[:, 0:2], in1=lbe[:, 2 * l:2 * l + 2], op=ALU.add))
            sc.op('dve', ['lbs'], ['lbs'], lambda e: e.reciprocal(out=lbs[:, 2:4], in_=lbs[:, 0:2]))
            sc.op('dve', [], ['lbs'], lambda e: e.memset(lbs[:, 4:6], 0.0))
            for l in range(1, L + 1):
                sc.op('dve', ['lbs', 'lbe'], ['lbs'], lambda e, l=l: e.tensor_tensor(
                    out=lbs[:, 6:8], in0=lbe[:, 2 * l:2 * l + 2], in1=lbs[:, 2:4], op=ALU.mult))
                sc.op('dve', ['lbs'], ['lbs'], lambda e: e.tensor_tensor(
                    out=lbs[:, 4:6], in0=lbs[:, 4:6], in1=lbs[:, 6:8], op=ALU.add))
            sc.op('dve', ['lbs'], ['lbs'], lambda e: e.tensor_scalar(
                out=lbs[:, 6:8], in0=lbs[:, 4:6], scalar1=-1.0, scalar2=1.0, op0=ALU.mult, op1=ALU.add))
            TT = 512
            inq = [mk.sb(es, "hg_q%d" % hp, [128, TT], F32) for hp in range(2)]
            inz = [mk.sb(es, "hg_z%d" % hp, [128, TT], F32) for hp in range(2)]
            ing = [mk.sb(es, "hg_g%d" % hp, [128, TT], F32) for hp in range(2)]
            vin = mk.sb(es, "hg_vin", [128, 4, 256], F32)
            vb = mk.sb(es, "hg_vb", [128, 256], BF16)
            fT = mk.sb(es, "hg_fT", [128, 128], F32)
            kT = mk.sb(es, "hg_kT", [128, 128], F32)
            lf = mk.sb(es, "hg_lf", [128, 128], F32)
            bT = mk.sb(es, "hg_bT", [128, 128], F32)
            eb = [mk.sb(es, "hg_eb%d" % hp, [128, 128], F32) for hp in range(2)]
            enb = mk.sb(es, "hg_enb", [128, 128], F32)
            sq = mk.sb(es, "hg_sq", [128, 128], F32)
            ksf = mk.sb(es, "hg_ksf", [128, 128], F32)
            qs = [mk.sb(es, "hg_qs%d" % hp, [128, 128], BF16) for hp in range(2)]
            ks = [mk.sb(es, "hg_ks%d" % hp, [128, 128], BF16) for hp in range(2)]
            kdT = mk.sb(es, "hg_kdT", [128, 128], BF16)
            kd = mk.sb(es, "hg_kd", [128, 256], BF16)
            kd3 = mk.sb(es, "hg_kd3", [128, 256], BF16)
            sc.op('pool', [], ['kd'], lambda e: e.memset(kd3[:], 0.0))
            osq = mk.sb(es, "hg_osq", [128, 256], BF16)
            rs = mk.sb(es, "hg_rs", [128, 256], F32)
            sg = mk.sb(es, "hg_sg", [128, 256], F32)
            ob = mk.sb(es, "hg_ob", [128, 256], BF16)
            pbk = dn.pb[1]
            for t0 in range(0, S, TT):
                for hp in range(2):
                    sc.dma('sp', inq[hp][:], UC[GIDX['hg_q%d' % hp], :, t0:t0 + TT], [], [('inq', hp)])
                    sc.dma('sp', inz[hp][:], UC[GIDX['hg_f%d' % hp], :, t0:t0 + TT], [], [('inz', hp)])
                    sc.dma('sp', ing[hp][:], UC[GIDX['hg_og%d' % hp], :, t0:t0 + TT], [], [('ing', hp)])
                sc.dma('sp', vin[:], UT[t0:t0 + TT, 128:384].rearrange("(n p) c -> p n c", p=128), [], ['vin'])
                for tl in range(TT // 128):
                    ti = t0 // 128 + tl
                    ts = slice(tl * 128, (tl + 1) * 128)
                    sc.op('pool', ['vin'], ['vb'], lambda e, tl=tl: e.tensor_copy(out=vb[:], in_=vin[:, tl, :]))
                    for hp in range(2):
                        sc.op('act', [('inz', hp)], ['fT'], lambda e, hp=hp, ts=ts: e.activation(
                            out=fT[:], in_=inz[hp][:, ts], func=AF.Sigmoid))
                        sc.op('dve', ['fT', 'lbs'], ['fT'], lambda e, hp=hp: e.tensor_scalar(
                            out=fT[:], in0=fT[:], scalar1=lbs[:, 6 + hp:7 + hp], scalar2=lbs[:, 4 + hp:5 + hp],
                            op0=ALU.mult, op1=ALU.add))
                        sc.op('pool', ['fT'], ['kT'], lambda e: e.tensor_scalar(
                            out=kT[:], in0=fT[:], scalar1=-1.0, scalar2=1.0, op0=ALU.mult, op1=ALU.add))
                        sc.op('dve', ['fT'], ['lf'], lambda e: e.tensor_scalar(
                            out=lf[:], in0=fT[:], scalar1=1e-20, scalar2=None, op0=ALU.max))
                        sc.op('act', ['lf'], ['lf'], lambda e: e.activation(out=lf[:], in_=lf[:], func=AF.Ln))
                        sc.op('dve', ['lf', 'rmask'], ['bT'], lambda e: e.tensor_tensor_scan(
                            out=bT[:], data0=self.rmask[:], data1=lf[:], initial=0.0, op0=ALU.mult, op1=ALU.add))
                        sc.op('act', ['bT'], [('eb', hp)], lambda e, hp=hp: e.activation(out=eb[hp][:], in_=bT[:], func=AF.Exp))
                        sc.op('act', ['bT'], ['enb'], lambda e: e.activation(out=enb[:], in_=bT[:], func=AF.Exp, scale=-1.0))
                        sc.op('act', [('inq', hp)], ['sq'], lambda e, hp=hp, ts=ts: e.activation(
                            out=sq[:], in_=inq[hp][:, ts], func=AF.Silu))
                        sc.op('dve', ['sq', ('eb', hp)], [('qs', hp)], lambda e, hp=hp: e.tensor_tensor(
                            out=qs[hp][:], in0=sq[:], in1=eb[hp][:], op=ALU.mult))
                        sc.op('dve', ['kT', 'enb'], ['ksf'], lambda e: e.tensor_tensor(
                            out=ksf[:], in0=kT[:], in1=enb[:], op=ALU.mult))
                        sc.op('pool', ['ksf'], [('ks', hp)], lambda e, hp=hp: e.tensor_copy(out=ks[hp][:], in_=ksf[:]))
                        sc.op('dve', ['ksf', ('eb', hp)], ['kdT'], lambda e, hp=hp: e.tensor_tensor(
                            out=kdT[:].rearrange("p (c i) -> p c i", i=32),
                            in0=ksf[:].rearrange("p (c i) -> p c i", i=32),
                            in1=eb[hp][:].rearrange("p (c i) -> p c i", i=32)[:, :, 31:32].broadcast_to((128, 4, 32)),
                            op=ALU.mult))
                        sc.op('pe', ['kdT', 'ident'], ['pb1'], lambda e: e.transpose(
                            out=pbk[:, 0:128], in_=kdT[:], identity=dn.ident[:]))
                        sc.op('act', ['pb1'], ['kd'], lambda e, hp=hp: e.activation(
                            out=kd[:, hp * 128:(hp + 1) * 128], in_=pbk[:, 0:128], func=AF.Copy))
                        sc.op('dve', ['pb1'], ['kd'], lambda e, hp=hp: e.tensor_copy(
                            out=kd3[96:128, hp * 128:(hp + 1) * 128], in_=pbk[96:128, 0:128]))
                    yield
                    self.core(b, ti, [q[:] for q in qs], [k[:] for k in ks], kd[:], kd3[:], vb[:],
                              lambda hp, c: eb[hp][:, c * 32 + 31:c * 32 + 32],
                              {'qs': [('qs', 0), ('qs', 1)], 'ks': [('ks', 0), ('ks', 1)], 'kd': 'kd', 'v': 'vb', 'dec': [('eb', 0), ('eb', 1)]})
                    yield
                    osb = b['osb']
                    sc.op('act', ['gl_osb'], ['osq'], lambda e: e.activation(out=osq[:], in_=osb[:], func=AF.Square))
                    pS = dn.next_ps()
                    sc.op('pe', ['osq', 'onesbd'], [('ps', pS)], lambda e, pS=pS: e.matmul(
                        dn.ps[pS][:, 0:256], lhsT=self.onesbd[:], rhs=osq[:], start=True, stop=True))
                    sc.op('dve', [('ps', pS)], ['rs'], lambda e, pS=pS: e.tensor_scalar(
                        out=rs[:], in0=dn.ps[pS][:, 0:256], scalar1=1.0 / 64, scalar2=EPS, op0=ALU.mult, op1=ALU.add))
                    sc.op('act', ['rs'], ['rs'], lambda e: e.activation(out=rs[:], in_=rs[:], func=AF.Sqrt))
                    sc.op('dve', ['rs'], ['rs'], lambda e: e.reciprocal(out=rs[:], in_=rs[:]))
                    for hp in range(2):
                        sc.op('act', [('ing', hp)], ['sg'], lambda e, hp=hp, ts=ts: e.activation(
                            out=sg[:, hp * 128:(hp + 1) * 128], in_=ing[hp][:, ts], func=AF.Silu))
                        sc.op('dve', ['rs', 'gl_osb', 'hg_gn'], ['rs'], lambda e, hp=hp: e.scalar_tensor_tensor(
                            out=rs[:, hp * 128:(hp + 1) * 128], in0=rs[:, hp * 128:(hp + 1) * 128],
                            scalar=gn[:, hp:hp + 1], in1=osb[:, hp * 128:(hp + 1) * 128], op0=ALU.mult, op1=ALU.mult))
                    sc.op('dve', ['rs', 'sg'], ['ob'], lambda e: e.tensor_tensor(out=ob[:], in0=rs[:], in1=sg[:], op=ALU.mult))
                    tg = t0 + tl * 128
                    sc.dma(STORE_Q, OT[1, :, :, tg:tg + 128].rearrange("c p t -> p c t"),
                           ob[:].rearrange("p (c t) -> p c t", c=2), ['ob'], [])
                    yield


def _run_ret(self, L, UC, UT, OT, gb_src, es_ext):
    sc, mk, dn, S = self.sc, self.mk, self.dn, self.mk.S
    with _Keep(es_ext) as es:
        b = self.alloc_core(es)
        gb = mk.sb(es, "rt_gb", [128, 4], F32)
        g32 = mk.sb(es, "rt_g32", [128, 2], F32)
        cdec = mk.sb(es, "rt_cdec", [128, 6, 128], F32)
        sc.dma('sp', gb[:], gb_src, [], ['rt_gb'])
        sc.dma('sp', g32[:], self.c_retg32[:, :], [], ['rt_g32'])
        sc.dma('sp', cdec[:], self.c_retdec.rearrange("a b p t -> p (a b) t"), [], ['rt_cdec'])
        TT = 512
        names = ['q', 'qr', 'k', 'kr', 'g']
        inb = {n: [mk.sb(es, "rt_%s%d" % (n, hp), [128, TT], F32) for hp in range(2)] for n in names}
        cs = mk.sb(es, "rt_cos", [128, TT], F32)
        sn = mk.sb(es, "rt_sin", [128, TT], F32)
        vin = mk.sb(es, "rt_vin", [128, 4, 256], F32)
        vb = mk.sb(es, "rt_vb", [128, 256], BF16)
        t1 = mk.sb(es, "rt_t1", [128, 128], F32)
        t2 = mk.sb(es, "rt_t2", [128, 128], F32)
        qro = mk.sb(es, "rt_qro", [128, 128], F32)
        kro = mk.sb(es, "rt_kro", [128, 128], F32)
        qs = [mk.sb(es, "rt_qs%d" % hp, [128, 128], BF16) for hp in range(2)]
        ks = [mk.sb(es, "rt_ks%d" % hp, [128, 128], BF16) for hp in range(2)]
        kdT = mk.sb(es, "rt_kdT", [128, 128], BF16)
        kd = mk.sb(es, "rt_kd", [128, 256], BF16)
        kd3 = mk.sb(es, "rt_kd3", [128, 256], BF16)
        sc.op('pool', [], ['kd'], lambda e: e.memset(kd3[:], 0.0))
        o16 = mk.sb(es, "rt_o16", [128, 256], BF16)
        cen = mk.sb(es, "rt_cen", [128, 256], F32)
        sq = mk.sb(es, "rt_sq", [128, 256], BF16)
        rs = mk.sb(es, "rt_rs", [128, 256], F32)
        sg = mk.sb(es, "rt_sg", [128, 256], F32)
        ob = mk.sb(es, "rt_ob", [128, 256], BF16)
        pbk = dn.pb[1]
        for t0 in range(0, S, TT):
            for hp in range(2):
                for n in names:
                    sc.dma('sp', inb[n][hp][:], UC[GIDX['ret_%s%d' % (n, hp)], :, t0:t0 + TT], [], [('rin', n, hp)])
            sc.dma('sp', cs[:], self.c_cos[:, t0:t0 + TT], [], ['rt_cos'])
            sc.dma('sp', sn[:], self.c_sin[:, t0:t0 + TT], [], ['rt_sin'])
            sc.dma('sp', vin[:], UT[t0:t0 + TT, 384:640].rearrange("(n p) c -> p n c", p=128), [], ['vin'])
            for tl in range(TT // 128):
                ti = t0 // 128 + tl
                ts = slice(tl * 128, (tl + 1) * 128)
                sc.op('pool', ['vin'], ['vb'], lambda e, tl=tl: e.tensor_copy(out=vb[:], in_=vin[:, tl, :]))
                for hp in range(2):
                    for (a, ar, dst, dkey) in (('q', 'qr', qro, 'qro'), ('k', 'kr', kro, 'kro')):
                        sc.op('dve', [('rin', a, hp), 'rt_cos'], ['t1'], lambda e, a=a, hp=hp, ts=ts: e.tensor_tensor(
                            out=t1[:], in0=inb[a][hp][:, ts], in1=cs[:, ts], op=ALU.mult))
                        sc.op('pool', [('rin', ar, hp), 'rt_sin'], ['t2'], lambda e, ar=ar, hp=hp, ts=ts: e.tensor_tensor(
                            out=t2[:], in0=inb[ar][hp][:, ts], in1=sn[:, ts], op=ALU.mult))
                        sc.op('dve', ['t1', 't2'], [dkey], lambda e, dst=dst: e.tensor_tensor(
                            out=dst[:], in0=t1[:], in1=t2[:], op=ALU.add))
                    sc.op('dve', ['qro', 'rt_cdec'], [('qs', hp)], lambda e, hp=hp: e.tensor_tensor(
                        out=qs[hp][:], in0=qro[:], in1=cdec[:, hp * 3 + 0, :], op=ALU.mult))
                    sc.op('pool', ['kro', 'rt_cdec'], [('ks', hp)], lambda e, hp=hp: e.tensor_tensor(
                        out=ks[hp][:], in0=kro[:], in1=cdec[:, hp * 3 + 1, :], op=ALU.mult))
                    sc.op('dve', ['kro', 'rt_cdec'], ['kdT'], lambda e, hp=hp: e.tensor_tensor(
                        out=kdT[:], in0=kro[:], in1=cdec[:, hp * 3 + 2, :], op=ALU.mult))
                    sc.op('pe', ['kdT', 'ident'], ['pb1'], lambda e: e.transpose(
                        out=pbk[:, 0:128], in_=kdT[:], identity=dn.ident[:]))
                    sc.op('act', ['pb1'], ['kd'], lambda e, hp=hp: e.activation(
                        out=kd[:, hp * 128:(hp + 1) * 128], in_=pbk[:, 0:128], func=AF.Copy))
                    sc.op('dve', ['pb1'], ['kd'], lambda e, hp=hp: e.tensor_copy(
                        out=kd3[96:128, hp * 128:(hp + 1) * 128], in_=pbk[96:128, 0:128]))
                yield
                self.core(b, ti, [q[:] for q in qs], [k[:] for k in ks], kd[:], kd3[:], vb[:],
                          lambda hp, c: g32[:, hp:hp + 1],
                          {'qs': [('qs', 0), ('qs', 1)], 'ks': [('ks', 0), ('ks', 1)], 'kd': 'kd', 'v': 'vb',
                           'dec': ['rt_g32', 'rt_g32']})
                yield
                osb = b['osb']
                sc.op('pool', ['gl_osb'], ['o16'], lambda e: e.tensor_copy(out=o16[:], in_=osb[:]))
                pM = dn.next_ps()
                sc.op('pe', ['o16', 'onesbd'], [('ps', pM)], lambda e, pM=pM: e.matmul(
                    dn.ps[pM][:, 0:256], lhsT=self.onesbd[:], rhs=o16[:], start=True, stop=True))
                sc.op('dve', [('ps', pM), 'gl_osb'], ['cen'], lambda e, pM=pM: e.scalar_tensor_tensor(
                    out=cen[:], in0=dn.ps[pM][:, 0:256], scalar=-1.0 / 64, in1=osb[:], op0=ALU.mult, op1=ALU.add))
                sc.op('act', ['cen'], ['sq'], lambda e: e.activation(out=sq[:], in_=cen[:], func=AF.Square))
                pV = dn.next_ps()
                sc.op('pe', ['sq', 'onesbd'], [('ps', pV)], lambda e, pV=pV: e.matmul(
                    dn.ps[pV][:, 0:256], lhsT=self.onesbd[:], rhs=sq[:], start=True, stop=True))
                sc.op('dve', [('ps', pV)], ['rs'], lambda e, pV=pV: e.tensor_scalar(
                    out=rs[:], in0=dn.ps[pV][:, 0:256], scalar1=1.0 / 64, scalar2=1e-5, op0=ALU.mult, op1=ALU.add))
                sc.op('act', ['rs'], ['rs'], lambda e: e.activation(out=rs[:], in_=rs[:], func=AF.Sqrt))
                sc.op('dve', ['rs'], ['rs'], lambda e: e.reciprocal(out=rs[:], in_=rs[:]))
                sc.op('dve', ['rs', 'cen'], ['cen'], lambda e: e.tensor_tensor(out=cen[:], in0=cen[:], in1=rs[:], op=ALU.mult))
                for hp in range(2):
                    sc.op('act', [('rin', 'g', hp)], ['sg'], lambda e, hp=hp, ts=ts: e.activation(
                        out=sg[:, hp * 128:(hp + 1) * 128], in_=inb['g'][hp][:, ts], func=AF.Silu))
                    sc.op('dve', ['cen', 'rt_gb'], ['cen'], lambda e, hp=hp: e.tensor_scalar(
                        out=cen[:, hp * 128:(hp + 1) * 128], in0=cen[:, hp * 128:(hp + 1) * 128],
                        scalar1=gb[:, hp:hp + 1], scalar2=gb[:, 2 + hp:3 + hp], op0=ALU.mult, op1=ALU.add))
                sc.op('dve', ['cen', 'sg'], ['ob'], lambda e: e.tensor_tensor(out=ob[:], in0=cen[:], in1=sg[:], op=ALU.mult))
                tg = t0 + tl * 128
                sc.dma(STORE_Q, OT[2, :, :, tg:tg + 128].rearrange("c p t -> p c t"),
                       ob[:].rearrange("p (c t) -> p c t", c=2), ['ob'], [])
                yield


GLA.run_ret = _run_ret


RW_C = 64


def rwkv_consts():
    i = np.arange(128)
    same = (i[:, None] // 64) == (i[None, :] // 64)
    su = (same & (i[:, None] % 64 < i[None, :] % 64)).astype(np.float32)
    iu = (same & (i[:, None] % 64 <= i[None, :] % 64)).astype(np.float32)
    d = {"c_rwmask4": np.ascontiguousarray(np.concatenate([su, su, iu, iu], axis=1)),
         "c_rwlmask": np.ascontiguousarray(su.T)}
    rm = np.ones((128, 256), np.float32)
    rm[:, ::64] = 0.0
    d["c_rmask64"] = rm
    return d


class RWKV:
    def __init__(self, dn, gla, es):
        self.dn, self.mk, self.sc, self.gla = dn, dn.mk, dn.sc, gla
        mk, sc = self.mk, self.sc
        self.mask4 = mk.sb(es, "rw_mask4", [128, 512], F32)
        self.lmask = mk.sb(es, "rw_lmask", [128, 128], F32)
        self.rmask = mk.sb(es, "rw_rmask", [128, 256], F32)
        sc.dma('sp', self.mask4[:], mk.din("c_rwmask4", [128, 512])[:, :], [], ['rw_mask4'])
        sc.dma('sp', self.lmask[:], mk.din("c_rwlmask", [128, 128])[:, :], [], ['rw_lmask'])
        sc.dma('sp', self.rmask[:], mk.din("c_rmask64", [128, 256])[:, :], [], ['rw_rmask'])

    def run(self, L, UC, OT, ws, es_ext, banks=(5, 6)):
        sc, mk, dn, S = self.sc, self.mk, self.dn, self.mk.S
        ident = dn.ident
        onesbd = self.gla.onesbd
        bi = [0]

        def nps():
            b_ = banks[bi[0] % len(banks)]
            bi[0] += 1
            return b_
        with _Keep(es_ext) as es:
            TT = min(256, S)
            NCH = TT // 64
            wa_f = mk.sb(es, "rw_waf", [128, 256], F32)
            WA = mk.sb(es, "rw_WA", [128, 256], BF16)
            gu_f = mk.sb(es, "rw_guf", [128, 256], F32)
            GU = mk.sb(es, "rw_GU", [128, 256], BF16)
            sc.dma('sp', wa_f[0:64, :], ws['w_up'], [], ['rw_waf'])
            sc.dma('sp', wa_f[64:128, :], ws['a_up'], [], ['rw_waf'])
            sc.dma('sp', gu_f[:], ws['g_up'], [], ['rw_guf'])
            sc.op('dve', ['rw_waf'], ['rw_WA'], lambda e: e.tensor_copy(out=WA[:], in_=wa_f[:]))
            sc.op('dve', ['rw_guf'], ['rw_GU'], lambda e: e.tensor_copy(out=GU[:], in_=gu_f[:]))
            pv = mk.sb(es, "rw_pv", [128, 24], F32)
            sc.dma('sp', pv[:], ws['pvec'], [], ['rw_pv'])
            sc.op('dve', ['rw_pv'], ['rw_pv'], lambda e: e.tensor_scalar(
                out=pv[:, 22:24], in0=pv[:, 14:16], scalar1=-1.0, scalar2=1.0, op0=ALU.mult, op1=ALU.add))
            raw = [mk.sb(es, "rw_raw%d" % g, [128, TT + 1], F32) for g in range(8)]
            sh = [mk.sb(es, "rw_sh%d" % g, [128, TT], F32) for g in range(8)]
            dtmp = mk.sb(es, "rw_dtmp", [128, TT], F32)
            twa = mk.sb(es, "rw_twa", [128, TT], BF16)
            sgl = mk.sb(es, "rw_sgl", [128, TT], BF16)
            F = lambda n: mk.sb(es, "rw_" + n, [128, TT], F32)
            lw, av, kk, rn, kkn, kp, lG, enG, eGp, t1 = (F(n) for n in
                ('lw', 'av', 'kk', 'rn', 'kkn', 'kp', 'lG', 'enG', 'eGp', 't1'))
            As_f, Ks_f = F('Asf'), F('Ksf')
            sq16 = mk.sb(es, "rw_sq16", [128, TT], BF16)
            y16 = mk.sb(es, "rw_y16", [128, TT], BF16)
            cen = mk.sb(es, "rw_cen", [128, TT], F32)
            rs = mk.sb(es, "rw_rs", [128, TT], F32)
            ob = mk.sb(es, "rw_ob", [128, TT], BF16)
            H = []
            for hp in range(2):
                hb = {}
                for n in ('As', 'Ks', 'Bt', 'Rt', 'Ah', 'Kh'):
                    hb['bd_' + n] = mk.sb(es, "rw_bd_%s%d" % (n, hp), [128, NCH, 128], BF16)
                    sc.op('pool', [], [('bd', n, hp)], lambda e, tl=hb['bd_' + n]: e.memset(tl[:], 0.0))
                for n in ('Bt', 'Rt', 'As', 'v'):
                    hb['r2_' + n] = mk.sb(es, "rw_r2_%s%d" % (n, hp), [128, NCH, 2, 64], BF16)
                hb['eG'] = mk.sb(es, "rw_eG%d" % hp, [128, TT], F32)
                hb['gT'] = mk.sb(es, "rw_gT%d" % hp, [128, TT], F32)
                hb['rkk16'] = mk.sb(es, "rw_rkk%d" % hp, [128, TT], BF16)
                hb['Tf'] = mk.sb(es, "rw_Tf%d" % hp, [128, 128], F32)
                hb['Tb'] = mk.sb(es, "rw_Tb%d" % hp, [128, 128], BF16)
                hb['PQ'] = [mk.sb(es, "rw_PQ%d_%d" % (hp, i), [128, 256], BF16) for i in range(2)]
                hb['NM'] = [mk.sb(es, "rw_NM%d_%d" % (hp, i), [128, 384], BF16) for i in range(2)]
                hb['Z'] = [[mk.sb(es, "rw_Z%d_%d_%d" % (hp, i, j), [128, 128], BF16) for j in range(2)] for i in range(2)]
                hb['Vtok'] = [mk.sb(es, "rw_Vtok%d_%d" % (hp, i), [128, 128], BF16) for i in range(2)]
                hb['AKtok'] = [mk.sb(es, "rw_AKtok%d_%d" % (hp, i), [128, 256], BF16) for i in range(2)]
                hb['Wsb'] = mk.sb(es, "rw_Wsb%d" % hp, [128, 128], BF16)
                hb['Usb'] = mk.sb(es, "rw_Usb%d" % hp, [128, 128], BF16)
                hb['yT'] = mk.sb(es, "rw_yT%d" % hp, [128, TT], F32)
                sc.op('dve', [], [('Tf', hp)], lambda e, hb=hb: e.memset(hb['Tf'][:], 0.0))
                sc.op('pool', [], [('Tb', hp)], lambda e, hb=hb: e.memset(hb['Tb'][:], 0.0))
                H.append(hb)
            pbk = dn.pb[1]
            c3 = lambda ap: ap.rearrange("p (c t) -> p c t", t=64)
            r4 = lambda ap: ap.rearrange("p (c t) -> p c t", t=64).unsqueeze(2).broadcast_to((128, NCH, 2, 64))

            def prep_hp(hp):
                hb = H[hp]
                hs = slice(hp * 128, (hp + 1) * 128)
                shr, shk, shv = sh[hp], sh[2 + hp], sh[4 + hp]
                kr, kk_, kv = ('sh', hp), ('sh', 2 + hp), ('sh', 4 + hp)
                eG, gT = hb['eG'], hb['gT']
                p1 = nps()
                sc.op('pes', ['rw_WA', 'twa'], [('ps', p1)], lambda e: e.matmul(
                    dn.ps[p1][:, 0:TT], lhsT=WA[0:64, hs], rhs=twa[0:64, :], start=True, stop=True))
                sc.op('act', [('ps', p1), 'rw_pv'], ['lw'], lambda e: e.activation(
                    out=lw[:], in_=dn.ps[p1][:, 0:TT], func=AF.Sigmoid, bias=pv[:, 8 + hp:9 + hp], scale=1.0))
                sc.op('pool', ['lw'], ['lw'], lambda e: e.tensor_scalar(
                    out=lw[:], in0=lw[:], scalar1=-0.6065306597126334, scalar2=None, op0=ALU.mult))
                p2 = nps()
                sc.op('pes', ['rw_WA', 'twa'], [('ps', p2)], lambda e: e.matmul(
                    dn.ps[p2][:, 0:TT], lhsT=WA[64:128, hs], rhs=twa[64:128, :], start=True, stop=True))
                sc.op('act', [('ps', p2), 'rw_pv'], ['av'], lambda e: e.activation(
                    out=av[:], in_=dn.ps[p2][:, 0:TT], func=AF.Sigmoid, bias=pv[:, 10 + hp:11 + hp], scale=1.0))
                p3 = nps()
                sc.op('pe', ['rw_GU', 'sgl'], [('ps', p3)], lambda e: e.matmul(
                    dn.ps[p3][:, 0:TT], lhsT=GU[:, hs], rhs=sgl[:], start=True, stop=True))
                sc.op('act', [('ps', p3)], [('gT', hp)], lambda e: e.activation(
                    out=gT[:], in_=dn.ps[p3][:, 0:TT], func=AF.Copy))
                sc.op('dve', [kk_, 'rw_pv'], ['kk'], lambda e: e.tensor_scalar(
                    out=kk[:], in0=shk[:], scalar1=pv[:, 12 + hp:13 + hp], scalar2=None, op0=ALU.mult))
                sc.op('act', ['kk'], ['sq16'], lambda e: e.activation(out=sq16[:], in_=kk[:], func=AF.Square))
                p4 = nps()
                sc.op('pe', ['sq16', 'onesbd'], [('ps', p4)], lambda e: e.matmul(
                    dn.ps[p4][:, 0:TT], lhsT=onesbd[:], rhs=sq16[:], start=True, stop=True))
                sc.op('dve', [('ps', p4)], ['rn'], lambda e: e.tensor_scalar(
                    out=rn[:], in0=dn.ps[p4][:, 0:TT], scalar1=1e-24, scalar2=None, op0=ALU.max))
                sc.op('act', ['rn'], ['rn'], lambda e: e.activation(out=rn[:], in_=rn[:], func=AF.Sqrt))
                sc.op('dve', ['rn'], ['rn'], lambda e: e.reciprocal(out=rn[:], in_=rn[:]))
                sc.op('dve', ['kk', 'rn'], ['kkn'], lambda e: e.tensor_tensor(out=kkn[:], in0=kk[:], in1=rn[:], op=ALU.mult))
                sc.op('pool', ['av', 'rw_pv'], ['t1'], lambda e: e.tensor_scalar(
                    out=t1[:], in0=av[:], scalar1=pv[:, 14 + hp:15 + hp], scalar2=pv[:, 22 + hp:23 + hp],
                    op0=ALU.mult, op1=ALU.add))
                sc.op('dve', ['t1', kk_], ['kp'], lambda e: e.tensor_tensor(out=kp[:], in0=shk[:], in1=t1[:], op=ALU.mult))
                sc.op('dve', ['lw', 'rw_rmask'], ['lG'], lambda e: e.tensor_tensor_scan(
                    out=lG[:], data0=self.rmask[:, 0:TT], data1=lw[:], initial=0.0, op0=ALU.mult, op1=ALU.add))
                sc.op('act', ['lG'], [('eG', hp)], lambda e: e.activation(out=eG[:], in_=lG[:], func=AF.Exp))
                sc.op('act', ['lG'], ['enG'], lambda e: e.activation(out=enG[:], in_=lG[:], func=AF.Exp, scale=-1.0))
                sc.op('pool', ['lG', 'lw'], ['t1'], lambda e: e.tensor_tensor(out=t1[:], in0=lG[:], in1=lw[:], op=ALU.subtract))
                sc.op('act', ['t1'], ['eGp'], lambda e: e.activation(out=eGp[:], in_=t1[:], func=AF.Exp))
                sc.op('dve', ['kkn', 'av'], ['t1'], lambda e: e.tensor_tensor(out=t1[:], in0=kkn[:], in1=av[:], op=ALU.mult))
                sc.op('dve', ['t1', 'enG'], ['Asf'], lambda e: e.tensor_tensor(out=As_f[:], in0=t1[:], in1=enG[:], op=ALU.mult))
                sc.op('pool', ['kp', 'enG'], ['Ksf'], lambda e: e.tensor_tensor(out=Ks_f[:], in0=kp[:], in1=enG[:], op=ALU.mult))
                sc.op('dve', ['kkn', 'eGp'], ['eGp'], lambda e: e.scalar_tensor_tensor(
                    out=eGp[:], in0=kkn[:], scalar=-1.0, in1=eGp[:], op0=ALU.mult, op1=ALU.mult))
                sc.op('pool', [kr, ('eG', hp)], ['t1'], lambda e: e.tensor_tensor(out=t1[:], in0=shr[:], in1=eG[:], op=ALU.mult))
                for half in range(2):
                    psl = slice(half * 64, (half + 1) * 64)
                    csl = slice(half * 64, (half + 1) * 64)
                    eng = 'dve' if half == 0 else 'pool'
                    for (nm, src, skey) in (('As', As_f, 'Asf'), ('Ks', Ks_f, 'Ksf'), ('Bt', eGp, 'eGp'), ('Rt', t1, 't1')):
                        sc.op(eng, [skey], [('bd', nm, hp)], lambda e, nm=nm, src=src, psl=psl, csl=csl: e.tensor_copy(
                            out=hb['bd_' + nm][psl, :, csl], in_=c3(src[psl, :])))
                    for (nm, src, skey) in (('Ah', As_f, 'Asf'), ('Kh', Ks_f, 'Ksf')):
                        sc.op('dve', [skey, ('eG', hp)], [('bd', nm, hp)], lambda e, nm=nm, src=src, psl=psl, csl=csl: e.tensor_tensor(
                            out=hb['bd_' + nm][psl, :, csl], in0=c3(src[psl, :]),
                            in1=c3(eG[psl, :])[:, :, 63:64].broadcast_to((64, NCH, 64)), op=ALU.mult))
                sc.op('pool', ['eGp'], [('r2', 'Bt', hp)], lambda e: e.tensor_copy(out=hb['r2_Bt'][:], in_=r4(eGp[:])))
                sc.op('pool', ['t1'], [('r2', 'Rt', hp)], lambda e: e.tensor_copy(out=hb['r2_Rt'][:], in_=r4(t1[:])))
                sc.op('dve', ['Asf'], [('r2', 'As', hp)], lambda e: e.tensor_copy(out=hb['r2_As'][:], in_=r4(As_f[:])))
                sc.op('pool', [kv], [('r2', 'v', hp)], lambda e: e.tensor_copy(out=hb['r2_v'][:], in_=r4(shv[:])))
                sc.op('dve', [kr, 'kp', 'rw_pv'], [('rkk16', hp)], lambda e: e.scalar_tensor_tensor(
                    out=hb['rkk16'][:], in0=shr[:], scalar=pv[:, 16 + hp:17 + hp], in1=kp[:], op0=ALU.mult, op1=ALU.mult))

            def chunk(hp, c, par):
                hb = H[hp]
                PQ, NM, Z, Vtok, AKtok, Wsb, Usb, Tf, Tb, yT, eG = (hb[k] for k in
                    ('PQ', 'NM', 'Z', 'Vtok', 'AKtok', 'Wsb', 'Usb', 'Tf', 'Tb', 'yT', 'eG'))
                f2 = lambda t: t[:, c, :, :].rearrange("p a t -> p (a t)")
                K_ = lambda *a: a + (hp,)
                pX = nps()
                X = dn.ps[pX]
                for j, (lt, rt) in enumerate((('As', 'Bt'), ('Ks', 'Bt'), ('As', 'Rt'), ('Ks', 'Rt'))):
                    sc.op('pe', [('bd', lt, hp), ('r2', rt, hp)], [('ps', pX)], lambda e, j=j, lt=lt, rt=rt: e.matmul(
                        X[:, j * 128:(j + 1) * 128], lhsT=hb['bd_' + lt][:, c, :], rhs=f2(hb['r2_' + rt]), start=True, stop=True))
                pQ = nps()
                sc.op('pe', [('bd', 'Bt', hp), ('r2', 'As', hp)], [('ps', pQ)], lambda e: e.matmul(
                    dn.ps[pQ][:, 0:128], lhsT=hb['bd_Bt'][:, c, :], rhs=f2(hb['r2_As']), start=True, stop=True))
                sc.op('dve', [('ps', pX), 'rw_mask4'], [K_('PQ', 0)], lambda e: e.tensor_tensor(
                    out=PQ[0][:, 0:128], in0=X[:, 0:128], in1=self.mask4[:, 0:128], op=ALU.mult))
                sc.op('dve', [('ps', pX), 'rw_mask4'], [K_('NM', par)], lambda e: e.tensor_tensor(
                    out=NM[par][:], in0=X[:, 128:512], in1=self.mask4[:, 128:512], op=ALU.mult))
                sc.op('dve', [('ps', pQ), 'rw_lmask'], [K_('PQ', 0)], lambda e: e.tensor_tensor(
                    out=PQ[0][:, 128:256], in0=dn.ps[pQ][:, 0:128], in1=self.lmask[:], op=ALU.mult))
                zi = 0
                sc.op('pool', [K_('PQ', 0), 'ident'], [K_('Z', par, zi)], lambda e: e.tensor_tensor(
                    out=Z[par][0][:], in0=PQ[0][:, 0:128], in1=ident[:], op=ALU.add))
                yield
                cur = 0
                for lvl in range(5):
                    last = (lvl == 4)
                    nxt = 1 - cur
                    pP = nps()
                    Pp = dn.ps[pP]
                    if not last:
                        sc.op('pe', [K_('PQ', cur)], [('ps', pP)], lambda e, cur=cur, Pp=Pp: e.matmul(
                            Pp[:, 0:128], lhsT=PQ[cur][:, 128:256], rhs=PQ[cur][:, 0:128], start=True, stop=True))
                    sc.op('pe', [K_('PQ', cur)], [('ps', pP)], lambda e, cur=cur, Pp=Pp: e.matmul(
                        Pp[:, 128:256], lhsT=PQ[cur][:, 0:128], rhs=PQ[cur][:, 128:256], start=True, stop=True))
                    if not last:
                        sc.op('act', [('ps', pP)], [K_('PQ', nxt)], lambda e, nxt=nxt, Pp=Pp: e.activation(
                            out=PQ[nxt][:], in_=Pp[:, 0:256], func=AF.Copy))
                    else:
                        sc.op('act', [('ps', pP)], [K_('PQ', nxt)], lambda e, nxt=nxt, Pp=Pp: e.activation(
                            out=PQ[nxt][:, 128:256], in_=Pp[:, 128:256], func=AF.Copy))
                    yield
                    pZ = nps()
                    sc.op('pe', [K_('PQ', nxt), K_('Z', par, zi)], [('ps', pZ)], lambda e, nxt=nxt, pZ=pZ, zi=zi: e.matmul(
                        dn.ps[pZ][:, 0:128], lhsT=PQ[nxt][:, 128:256], rhs=Z[par][zi][:], start=True, stop=True))
                    sc.op('dve', [('ps', pZ), K_('Z', par, zi)], [K_('Z', par, 1 - zi)], lambda e, pZ=pZ, zi=zi: e.tensor_tensor(
                        out=Z[par][1 - zi][:], in0=dn.ps[pZ][:, 0:128], in1=Z[par][zi][:], op=ALU.add))
                    yield
                    zi = 1 - zi
                    cur = nxt
                Zf = Z[par][zi]
                zkey = K_('Z', par, zi)
                pV = nps()
                sc.op('pe', [('r2', 'v', hp), 'ident'], [('ps', pV)], lambda e: e.matmul(
                    dn.ps[pV][:, 0:128], lhsT=f2(hb['r2_v']), rhs=ident[:], start=True, stop=True))
                sc.op('act', [('ps', pV)], [K_('Vtok', par)], lambda e: e.activation(
                    out=Vtok[par][:], in_=dn.ps[pV][:, 0:128], func=AF.Copy))
                sc.op('pe', [('bd', 'Ah', hp), 'ident'], ['pb1'], lambda e: e.transpose(
                    out=pbk[:, 0:128], in_=hb['bd_Ah'][:, c, :], identity=ident[:]))
                sc.op('pe', [('bd', 'Kh', hp), 'ident'], ['pb1'], lambda e: e.transpose(
                    out=pbk[:, 128:256], in_=hb['bd_Kh'][:, c, :], identity=ident[:]))
                sc.op('dve', ['pb1'], [K_('AKtok', par)], lambda e: e.tensor_copy(out=AKtok[par][:], in_=pbk[:, 0:256]))
                yield
                pW = nps()
                sc.op('pe', [('bd', 'Bt', hp), ('Tb', hp)], [('ps', pW)], lambda e: e.matmul(
                    dn.ps[pW][:, 0:128], lhsT=hb['bd_Bt'][:, c, :], rhs=Tb[:], start=True, stop=False))
                sc.op('pe', [K_('NM', par), K_('Vtok', par)], [('ps', pW)], lambda e: e.matmul(
                    dn.ps[pW][:, 0:128], lhsT=NM[par][:, 0:128], rhs=Vtok[par][:], start=False, stop=True))
                sc.op('act', [('ps', pW)], [K_('Wsb')], lambda e: e.activation(out=Wsb[:], in_=dn.ps[pW][:, 0:128], func=AF.Copy))
                yield
                pU = nps()
                sc.op('pe', [zkey, K_('Wsb')], [('ps', pU)], lambda e: e.matmul(
                    dn.ps[pU][:, 0:128], lhsT=Zf[:], rhs=Wsb[:], start=True, stop=True))
                sc.op('dve', [('ps', pU)], [K_('Usb')], lambda e: e.tensor_copy(out=Usb[:], in_=dn.ps[pU][:, 0:128]))
                yield
                pT = nps()
                sc.op('pe', [K_('AKtok', par), K_('Usb')], [('ps', pT)], lambda e: e.matmul(
                    dn.ps[pT][:, 0:128], lhsT=AKtok[par][:, 0:128], rhs=Usb[:], start=True, stop=False))
                sc.op('pe', [K_('AKtok', par), K_('Vtok', par)], [('ps', pT)], lambda e: e.matmul(
                    dn.ps[pT][:, 0:128], lhsT=AKtok[par][:, 128:256], rhs=Vtok[par][:], start=False, stop=True))
                pY = nps()
                sc.op('pe', [('Tb', hp), ('bd', 'Rt', hp)], [('ps', pY)], lambda e: e.matmul(
                    dn.ps[pY][:, 0:128], lhsT=Tb[:], rhs=hb['bd_Rt'][:, c, :], start=True, stop=False))
                sc.op('pe', [K_('Usb'), K_('NM', par)], [('ps', pY)], lambda e: e.matmul(
                    dn.ps[pY][:, 0:128], lhsT=Usb[:], rhs=NM[par][:, 128:256], start=False, stop=False))
                sc.op('pe', [K_('Vtok', par), K_('NM', par)], [('ps', pY)], lambda e: e.matmul(
                    dn.ps[pY][:, 0:128], lhsT=Vtok[par][:], rhs=NM[par][:, 256:384], start=False, stop=True))
                sc.op('dve', [('ps', pT), ('Tf', hp), ('eG', hp)], [('Tf', hp)], lambda e: e.scalar_tensor_tensor(
                    out=Tf[:], in0=Tf[:], scalar=eG[:, c * 64 + 63:c * 64 + 64], in1=dn.ps[pT][:, 0:128],
                    op0=ALU.mult, op1=ALU.add))
                sc.op('act', [('Tf', hp)], [('Tb', hp)], lambda e: e.activation(out=Tb[:], in_=Tf[:], func=AF.Copy))
                sc.op('act', [('ps', pY)], [('yT', hp)], lambda e: e.activation(
                    out=yT[0:64, c * 64:(c + 1) * 64], in_=dn.ps[pY][0:64, 0:64], func=AF.Copy))
                sc.op('act', [('ps', pY)], [('yT', hp)], lambda e: e.activation(
                    out=yT[64:128, c * 64:(c + 1) * 64], in_=dn.ps[pY][64:128, 64:128], func=AF.Copy))
                yield

            def epilogue(hp, t0):
                hb = H[hp]
                yT, gT, shv, kv = hb['yT'], hb['gT'], sh[4 + hp], ('sh', 4 + hp)
                sc.op('pool', [('yT', hp)], ['y16'], lambda e: e.tensor_copy(out=y16[:], in_=yT[:]))
                pM = nps()
                sc.op('pe', ['y16', 'onesbd'], [('ps', pM)], lambda e: e.matmul(
                    dn.ps[pM][:, 0:TT], lhsT=onesbd[:], rhs=y16[:], start=True, stop=True))
                sc.op('dve', [('ps', pM), ('yT', hp)], ['cen'], lambda e: e.scalar_tensor_tensor(
                    out=cen[:], in0=dn.ps[pM][:, 0:TT], scalar=-1.0 / 64, in1=yT[:], op0=ALU.mult, op1=ALU.add))
                sc.op('act', ['cen'], ['y16'], lambda e: e.activation(out=y16[:], in_=cen[:], func=AF.Square))
                pV2 = nps()
                sc.op('pe', ['y16', 'onesbd'], [('ps', pV2)], lambda e: e.matmul(
                    dn.ps[pV2][:, 0:TT], lhsT=onesbd[:], rhs=y16[:], start=True, stop=True))
                sc.op('dve', [('ps', pV2)], ['rs'], lambda e: e.tensor_scalar(
                    out=rs[:], in0=dn.ps[pV2][:, 0:TT], scalar1=1.0 / 64, scalar2=64e-5, op0=ALU.mult, op1=ALU.add))
                sc.op('act', ['rs'], ['rs'], lambda e: e.activation(out=rs[:], in_=rs[:], func=AF.Sqrt))
                sc.op('dve', ['rs'], ['rs'], lambda e: e.reciprocal(out=rs[:], in_=rs[:]))
                sc.op('dve', ['rs', 'cen'], ['cen'], lambda e: e.tensor_tensor(out=cen[:], in0=cen[:], in1=rs[:], op=ALU.mult))
                sc.op('dve', ['cen', 'rw_pv'], ['cen'], lambda e: e.tensor_scalar(
                    out=cen[:], in0=cen[:], scalar1=pv[:, 18 + hp:19 + hp], scalar2=pv[:, 20 + hp:21 + hp],
                    op0=ALU.mult, op1=ALU.add))
                pBn = nps()
                sc.op('pe', [('rkk16', hp), 'onesbd'], [('ps', pBn)], lambda e: e.matmul(
                    dn.ps[pBn][:, 0:TT], lhsT=onesbd[:], rhs=hb['rkk16'][:], start=True, stop=True))
                sc.op('dve', [('ps', pBn), kv], ['rs'], lambda e: e.tensor_tensor(
                    out=rs[:], in0=dn.ps[pBn][:, 0:TT], in1=shv[:], op=ALU.mult))
                sc.op('pool', ['rs', 'cen'], ['cen'], lambda e: e.tensor_tensor(out=cen[:], in0=cen[:], in1=rs[:], op=ALU.add))
                sc.op('dve', ['cen', ('gT', hp)], ['ob'], lambda e: e.tensor_tensor(out=ob[:], in0=cen[:], in1=gT[:], op=ALU.mult))
                sc.dma(STORE_Q, OT[3, hp, :, t0:t0 + TT], ob[:], ['ob'], [])

            ci = 0
            for t0 in range(0, S, TT):
                for g in range(8):
                    gi = GIDX['rw%d' % g]
                    if t0 == 0:
                        sc.op('pool', [], [('raw', g)], lambda e, g=g: e.memset(raw[g][:, 0:1], 0.0))
                        sc.dma('sp', raw[g][:, 1:TT + 1], UC[gi, :, 0:TT], [], [('raw', g)])
                    else:
                        sc.dma('sp', raw[g][:, :], UC[gi, :, t0 - 1:t0 + TT], [], [('raw', g)])
                    sc.op('pool', [('raw', g)], ['dtmp'], lambda e, g=g: e.tensor_tensor(
                        out=dtmp[:], in0=raw[g][:, 0:TT], in1=raw[g][:, 1:TT + 1], op=ALU.subtract))
                    sc.op('dve', ['dtmp', ('raw', g), 'rw_pv'], [('sh', g)], lambda e, g=g: e.scalar_tensor_tensor(
                        out=sh[g][:], in0=dtmp[:], scalar=pv[:, g:g + 1], in1=raw[g][:, 1:TT + 1],
                        op0=ALU.mult, op1=ALU.add))
                    if g % 2:
                        yield
                sc.op('act', [('sh', 6)], ['twa'], lambda e: e.activation(out=twa[0:64, :], in_=sh[6][0:64, :], func=AF.Tanh))
                sc.op('pool', [('sh', 6)], ['twa'], lambda e: e.tensor_copy(out=twa[64:128, :], in_=sh[6][64:128, :]))
                sc.op('act', [('sh', 7)], ['sgl'], lambda e: e.activation(out=sgl[:], in_=sh[7][:], func=AF.Sigmoid))
                for hp in range(2):
                    prep_hp(hp)
                    yield
                for c in range(NCH):
                    par = ci % 2
                    ci += 1
                    gens = [chunk(hp, c, par) for hp in range(2)]
                    while gens:
                        for g_ in list(gens):
                            try:
                                next(g_)
                            except StopIteration:
                                gens.remove(g_)
                        yield
                for hp in range(2):
                    epilogue(hp, t0)
                    yield


NEG = -30000.0
BIGV = 1e30


def nsa_consts(S):
    NB, NKT = S // 64, S // 128
    n_cmp = (S - 32) // 16 + 1
    NT = (n_cmp + 127) // 128
    n = np.arange(128)
    t = np.arange(128)
    d = {}
    cp = np.zeros((128, 17, 128), np.float32)
    for k in range(17):
        cp[:, k, :] = np.where(16 * n[:, None] + 31 <= 128 * k + t[None, :], 0.0, NEG)
    d["c_cpat"] = cp
    d["c_causneg"] = np.where(n[:, None] > t[None, :], NEG, 0.0).astype(np.float32)
    d["c_bandneg"] = np.where(n[:, None] <= t[None, :], NEG, 0.0).astype(np.float32)
    E = np.zeros((128, NKT, 128), np.float32)
    for j in range(NKT):
        for sl in range(128):
            bb = 2 * j + sl // 64
            E[bb, j, sl] = 1.0
    d["c_E"] = E[:max(NB, 1)] if NB <= 128 else E
    ov = np.zeros((128, NT, NB + 1), np.float32)
    for jn in range(NT):
        nn = jn * 128 + n
        valid = nn < n_cmp
        cs = 16 * nn
        for b in range(NB):
            ov[:, jn, b] = ((cs < 64 * (b + 1)) & (cs + 32 > 64 * b) & valid).astype(np.float32)
        ov[:, jn, NB] = valid.astype(np.float32)
    d["c_ovl"] = ov
    jj = np.arange(2 * NB)
    j = jj - NB
    hi = (t >= 64).astype(np.int64)
    allowed = j[None, :] <= hi[:, None]
    forced = (j[None, :] == hi[:, None]) | (j[None, :] == hi[:, None] - 1)
    d["c_amnf"] = (allowed & ~forced).astype(np.float32)
    d["c_fbna"] = np.where(forced, BIGV, np.where(allowed, 0.0, -1.0)).astype(np.float32)
    return d


class NSA:
    def __init__(self, dn, gla, es):
        self.dn, self.mk, self.sc, self.gla = dn, dn.mk, dn.sc, gla
        mk, S = self.mk, self.mk.S
        self.NB, self.NKT = S // 64, S // 128
        self.n_cmp = (S - 32) // 16 + 1
        self.NT = (self.n_cmp + 127) // 128
        self.cin = {n: mk.din(n, shp) for n, shp in (
            ("c_cpat", [128, 17, 128]), ("c_causneg", [128, 128]), ("c_bandneg", [128, 128]),
            ("c_E", [self.NB, self.NKT, 128]), ("c_ovl", [128, self.NT, self.NB + 1]),
            ("c_amnf", [128, 2 * self.NB]), ("c_fbna", [128, 2 * self.NB]))}

    def run(self, L, UC, UT, OT, ws, es):
        sc, mk, dn, S = self.sc, self.mk, self.dn, self.mk.S
        NB, NKT, NT, n_cmp = self.NB, self.NKT, self.NT, self.n_cmp
        ident = dn.ident
        PS = dn.ps
        sc.ns = 'nsa'
        T = {}
        T['cpat'] = mk.sb(es, "ns_cpat", [128, 17 * 128], BF16)
        T['caus'] = mk.sb(es, "ns_caus", [128, 128], BF16)
        T['band'] = mk.sb(es, "ns_band", [128, 128], BF16)
        T['Eb'] = mk.sb(es, "ns_E", [NB, NKT * 128], BF16)
        T['ovl'] = mk.sb(es, "ns_ovl", [128, NT * (NB + 1)], BF16)
        T['amnf'] = mk.sb(es, "ns_amnf", [128, 2 * NB], F32)
        T['fbna'] = mk.sb(es, "ns_fbna", [128, 2 * NB], F32)
        T['ksT'] = mk.sb(es, "ns_ksT", [64, S], BF16)
        T['kwT'] = mk.sb(es, "ns_kwT", [64, S], BF16)
        T['vs'] = mk.sb(es, "ns_vs", [128, NKT, 128], BF16)
        T['vw'] = mk.sb(es, "ns_vw", [128, NKT, 128], BF16)
        T['kcmpT'] = mk.sb(es, "ns_kcmpT", [64, NT * 128], BF16)
        T['vcmp'] = mk.sb(es, "ns_vcmp", [128, NT, 128], BF16)
        T['qf'] = mk.sb(es, "ns_qf", [64, 4, 128], F32)
        T['q16'] = mk.sb(es, "ns_q16", [64, 512], BF16)
        T['gf'] = mk.sb(es, "ns_gf", [64, 12, 128], F32)
        T['Pc'] = [mk.sb(es, "ns_Pc%d" % i, [128, 512], BF16) for i in range(NT)]
        T["Pr"] = [mk.sb(es, "ns_Pr%d" % i, [128, 512], BF16) for i in range(4)]
        T['ox'] = [mk.sb(es, "ns_ox%d" % i, [64, 512], F32) for i in range(3)]
        T['zx'] = [mk.sb(es, "ns_zx%d" % i, [64, 512], F32) for i in range(3)]
        T['zr'] = mk.sb(es, "ns_zr", [128, 8], F32)
        T['imp'] = mk.sb(es, "ns_imp", [128, NB], F32)
        T['sc1'] = mk.sb(es, "ns_sc1", [128, NB], F32)
        T['sc2'] = mk.sb(es, "ns_sc2", [128, NB], F32)
        T['m8'] = mk.sb(es, "ns_m8", [128, 16], F32)
        T['selm'] = mk.sb(es, "ns_selm", [128, 128], BF16)
        T['selT'] = mk.sb(es, "ns_selT", [128, 128], BF16)
        T['acc'] = T['ox'][0]
        T['ob'] = mk.sb(es, "ns_ob", [64, 512], BF16)
        with ExitStack() as es2:
            stage = [mk.sb(es2, "ns_st%d" % i, [128, 2048], F32) for i in range(2)]
            sti = [0]

            def load_cast(dst_ap, src_ap, rows, cols, key):
                for c0 in range(0, cols, 2048):
                    c1 = min(cols, c0 + 2048)
                    si = sti[0] % 2
                    sti[0] += 1
                    sc.dma('sp', stage[si][0:rows, 0:c1 - c0], src_ap[:, c0:c1], [], [('nst', si)])
                    eng = ('dve', 'pool')[sti[0] % 2]
                    sc.op(eng, [('nst', si)], [key], lambda e, si=si, c0=c0, c1=c1: e.tensor_copy(
                        out=dst_ap[:, c0:c1], in_=stage[si][0:rows, 0:c1 - c0]))

            load_cast(T['cpat'][:], self.cin["c_cpat"].rearrange("p k t -> p (k t)"), 128, 17 * 128, 'cpat')
            load_cast(T['caus'][:], self.cin["c_causneg"], 128, 128, 'caus')
            load_cast(T['band'][:], self.cin["c_bandneg"], 128, 128, 'band')
            load_cast(T['Eb'][:], self.cin["c_E"].rearrange("p k t -> p (k t)"), NB, NKT * 128, 'E')
            load_cast(T['ovl'][:], self.cin["c_ovl"].rearrange("p k t -> p (k t)"), 128, NT * (NB + 1), 'ovl')
            sc.dma('sp', T['amnf'][:], self.cin["c_amnf"][:, :], [], ['amnf'])
            sc.dma('sp', T['fbna'][:], self.cin["c_fbna"][:, :], [], ['fbna'])
            load_cast(T['ksT'][:], UC[GIDX['nsa_ks'], 0:64, :], 64, S, 'ksT')
            load_cast(T['kwT'][:], UC[GIDX['nsa_kw'], 0:64, :], 64, S, 'kwT')
            sc.op('pool', [], ['vs'], lambda e: e.memset(T['vs'][:], 1.0))
            sc.op('pool', [], ['vw'], lambda e: e.memset(T['vw'][:], 1.0))
            sc.op('pool', [], ['vcmp'], lambda e: e.memset(T['vcmp'][:], 1.0))
            for n0 in range(0, NKT, 8):
                n1 = min(NKT, n0 + 8)
                si = sti[0] % 2
                sti[0] += 1
                stv = stage[si][:, 0:(n1 - n0) * 128].rearrange("p (n c) -> p n c", c=128)
                sc.dma('sp', stv, UT[n0 * 128:n1 * 128, 0:128].rearrange("(n p) c -> p n c", p=128), [], [('nst', si)])
                sc.op('dve', [('nst', si)], ['vs'], lambda e, n0=n0, n1=n1, stv=stv: e.tensor_copy(
                    out=T['vs'][:, n0:n1, 0:64], in_=stv[:, :, 0:64]))
                sc.op('pool', [('nst', si)], ['vw'], lambda e, n0=n0, n1=n1, stv=stv: e.tensor_copy(
                    out=T['vw'][:, n0:n1, 0:64], in_=stv[:, :, 64:128]))
            kcmpT, vcmp = T['kcmpT'], T['vcmp']
            sc.op('pool', [], ['kcmpT'], lambda e: e.memset(kcmpT[:], 0.0))
            kc16 = mk.sb(es2, "ns_kc16", [64, S], BF16)
            W1 = mk.sb(es2, "ns_W1", [64, 32 * 128], BF16)
            W2f = mk.sb(es2, "ns_W2f", [128, 64], F32)
            W2 = mk.sb(es2, "ns_W2", [128, 64], BF16)
            posf = mk.sb(es2, "ns_posf", [64, 32], F32)
            pos16 = mk.sb(es2, "ns_pos16", [64, 32], BF16)
            cb = mk.sb(es2, "ns_cb", [128, 1], F32)
            hid = mk.sb(es2, "ns_hid", [128, NT * 128], BF16)
            for which in range(2):
                gname = 'nsa_kc' if which == 0 else 'nsa_vc'
                load_cast(kc16[:], UC[GIDX[gname], 0:64, :], 64, S, 'kc16')
                load_cast(W1[:], ws['w1r'][which], 64, 32 * 128, 'W1')
                sc.dma('sp', W2f[:], ws['w2'][which], [], ['W2f'])
                sc.op('dve', ['W2f'], ['W2'], lambda e: e.tensor_copy(out=W2[:], in_=W2f[:]))
                sc.dma('sp', posf[:], ws['posT'][which], [], ['posf'])
                sc.op('dve', ['posf'], ['pos16'], lambda e: e.tensor_copy(out=pos16[:], in_=posf[:]))
                pc = 0
                for j in range(32):
                    sc.op('pes', ['W1', 'pos16'], [('ps', pc)], lambda e, j=j: e.matmul(
                        PS[pc][:, 0:1], lhsT=W1[:, j * 128:(j + 1) * 128], rhs=pos16[:, j:j + 1],
                        start=(j == 0), stop=(j == 31)))
                sc.op('dve', [('ps', pc)], ['cb'], lambda e: e.tensor_copy(out=cb[:], in_=PS[pc][:, 0:1]))
                ph = 1
                for j in range(32):
                    sc.op('pes', ['W1', 'kc16'], [('ps', ph)], lambda e, j=j: e.matmul(
                        PS[ph][:, 0:n_cmp], lhsT=W1[:, j * 128:(j + 1) * 128],
                        rhs=kc16[:, j:j + 16 * (n_cmp - 1) + 1:16], start=(j == 0), stop=(j == 31)))
                sc.op('pool', [], ['hid'], lambda e: e.memset(hid[:], 0.0))
                sc.op('act', [('ps', ph), 'cb'], ['hid'], lambda e: e.activation(
                    out=hid[:, 0:n_cmp], in_=PS[ph][:, 0:n_cmp], func=AF.Silu, bias=cb[:, 0:1], scale=1.0))
                if which == 0:
                    po = 2
                    sc.op('pe', ['W2', 'hid'], [('ps', po)], lambda e: e.matmul(
                        PS[po][0:64, 0:n_cmp], lhsT=W2[:], rhs=hid[:, 0:n_cmp], start=True, stop=True))
                    sc.op('act', [('ps', po)], ['kcmpT'], lambda e: e.activation(
                        out=kcmpT[:, 0:n_cmp], in_=PS[po][0:64, 0:n_cmp], func=AF.Copy))
                else:
                    for jn in range(NT):
                        po = 2 + jn % 2
                        sc.op('pe', ['W2', 'hid'], [('ps', po)], lambda e, jn=jn, po=po: e.matmul(
                            PS[po][:, 0:64], lhsT=hid[:, jn * 128:(jn + 1) * 128], rhs=W2[:], start=True, stop=True))
                        sc.op('act', [('ps', po)], ['vcmp'], lambda e, jn=jn, po=po: e.activation(
                            out=vcmp[:, jn, 0:64], in_=PS[po][:, 0:64], func=AF.Copy))
            sc.ns = None
            sc.barrier()
        return self.tiles(UC, OT, T)

    def tiles(self, UC, OT, T):
        sc, mk, dn, S = self.sc, self.mk, self.dn, self.mk.S
        NB, NKT, NT, n_cmp = self.NB, self.NKT, self.NT, self.n_cmp
        ident = dn.ident
        PS = dn.ps
        cpat, caus, band, Eb, ovl, amnf, fbna = (T[k] for k in ('cpat', 'caus', 'band', 'Eb', 'ovl', 'amnf', 'fbna'))
        ksT, kwT, vs, vw, kcmpT, vcmp = (T[k] for k in ('ksT', 'kwT', 'vs', 'vw', 'kcmpT', 'vcmp'))
        qf, q16, gf, Pc, Pr, ox, zx, zr, imp, sc1, sc2, m8, selm, selT, acc, ob = (T[k] for k in (
            'qf', 'q16', 'gf', 'Pc', 'Pr', 'ox', 'zx', 'zr', 'imp', 'sc1', 'sc2', 'm8', 'selm', 'selT', 'acc', 'ob'))
        sc.op('pool', [], ['selm'], lambda e: e.memset(selm[:], 0.0))
        pbk = dn.pb[1]
        SB_ = [0, 1, 3, 4]
        IB_ = [0, 1]
        OZ = 2
        DEPTH_P = 3
        pr_i = [0]
        cnt = [0, 0]

        def bc4(ap):
            return ap.unsqueeze(1).broadcast_to((ap.shape[0], 4, 128))

        def issue(kT_ap, kkey, addmask):
            sbk = SB_[cnt[0] % len(SB_)]
            cnt[0] += 1
            nmm = 1 + len(addmask)
            sc.op('pe', [kkey, 'q16'], [('ps', sbk)], lambda e: e.matmul(
                PS[sbk][:, :], lhsT=kT_ap, rhs=q16[:], start=True, stop=(nmm == 1)))
            for mi, (ml, mr, mkeys) in enumerate(addmask):
                sc.op('pe', mkeys, [('ps', sbk)], lambda e, ml=ml, mr=mr, mi=mi: e.matmul(
                    PS[sbk][:, :], lhsT=ml, rhs=mr, start=False, stop=(mi == nmm - 2)))
            pt = Pr[pr_i[0] % len(Pr)]
            pkey = ('Pr', pr_i[0] % len(Pr))
            pr_i[0] += 1
            sc.op('act', [('ps', sbk)], [pkey], lambda e: e.activation(out=pt[:], in_=PS[sbk][:, :], func=AF.Exp))
            return pt, pkey

        def consume(pt, pkey, vlhs, vkey, first, last):
            sc.op('pe', [pkey, vkey], [('ps', OZ)], lambda e: e.matmul(
                PS[OZ][:, :], lhsT=vlhs, rhs=pt[:], start=first, stop=last))

        def branch(pairs):
            pend = []
            n = len(pairs)
            done = 0
            for idx, (kT_ap, kkey, addmask, vlhs, vkey) in enumerate(pairs):
                pt, pkey = issue(kT_ap, kkey, addmask)
                pend.append((pt, pkey, vlhs, vkey))
                if len(pend) > DEPTH_P:
                    a = pend.pop(0)
                    consume(a[0], a[1], a[2], a[3], done == 0, done == n - 1)
                    done += 1
                yield
            while pend:
                a = pend.pop(0)
                consume(a[0], a[1], a[2], a[3], done == 0, done == n - 1)
                done += 1
            yield

        def finish(x):
            sc.op('act', [('ps', OZ)], [('ox', x)], lambda e: e.activation(out=ox[x][:], in_=PS[OZ][0:64, :], func=AF.Copy))
            sc.op('dve', [('ps', OZ)], [('zx', x)], lambda e: e.tensor_scalar(
                out=zx[x][:], in0=PS[OZ][64:128, :], scalar1=1e-30, scalar2=None, op0=ALU.max))
            sc.op('dve', [('zx', x)], [('zx', x)], lambda e: e.reciprocal(out=zx[x][:], in_=zx[x][:]))

        for i in range(NKT):
            t0 = i * 128
            for h in range(4):
                sc.dma('sp', qf[:, h, :], UC[GIDX['nsa_q%d' % h], 0:64, t0:t0 + 128], [], ['qf'])
            for gi in range(12):
                sc.dma('sp', gf[:, gi, :], UC[GIDX['nsa_g%d' % gi], 0:64, t0:t0 + 128], [], ['gf'])
            sc.op('act', ['qf'], ['q16'], lambda e: e.activation(
                out=q16[:], in_=qf[:].rearrange("p h t -> p (h t)"), func=AF.Copy, scale=0.125))
            sc.op('act', ['gf'], ['gf'], lambda e: e.activation(out=gf[:], in_=gf[:], func=AF.Sigmoid))
            yield
            jmax = min(NT - 1, (8 * i + 6) // 128)
            for jn in range(jmax + 1):
                sbk = SB_[cnt[0] % len(SB_)]
                cnt[0] += 1
                k = i - 16 * jn
                need_mask = k <= 16
                sc.op('pe', ['kcmpT', 'q16'], [('ps', sbk)], lambda e, jn=jn, sbk=sbk, need_mask=need_mask: e.matmul(
                    PS[sbk][:, :], lhsT=kcmpT[:, jn * 128:(jn + 1) * 128], rhs=q16[:], start=True, stop=(not need_mask)))
                if need_mask:
                    sc.op('pe', ['cpat', 'ident'], [('ps', sbk)], lambda e, k=k, sbk=sbk: e.matmul(
                        PS[sbk][:, :], lhsT=ident[:], rhs=bc4(cpat[:, k * 128:(k + 1) * 128]), start=False, stop=True))
                sc.op('act', [('ps', sbk)], [('Pc', jn)], lambda e, jn=jn, sbk=sbk: e.activation(
                    out=Pc[jn][:], in_=PS[sbk][:, :], func=AF.Exp))
                yield
            for jn in range(jmax + 1):
                sc.op('pe', [('Pc', jn), 'vcmp'], [('ps', OZ)], lambda e, jn=jn: e.matmul(
                    PS[OZ][:, :], lhsT=vcmp[:, jn, :], rhs=Pc[jn][:], start=(jn == 0), stop=(jn == jmax)))
            finish(0)
            for h in range(4):
                ib = IB_[h // 2]
                c0 = (h % 2) * (NB + 1)
                for jn in range(jmax + 1):
                    sc.op('pe', [('Pc', jn), 'ovl'], [('ps', ib)], lambda e, jn=jn, h=h, ib=ib, c0=c0: e.matmul(
                        PS[ib][:, c0:c0 + NB + 1], lhsT=Pc[jn][:, h * 128:(h + 1) * 128],
                        rhs=ovl[:, jn * (NB + 1):(jn + 1) * (NB + 1)], start=(jn == 0), stop=(jn == jmax)))
            for h in range(4):
                ib = IB_[h // 2]
                c0 = (h % 2) * (NB + 1)
                sc.op('dve', [('ps', ib)], ['zr'], lambda e, h=h, ib=ib, c0=c0: e.tensor_scalar(
                    out=zr[:, h:h + 1], in0=PS[ib][:, c0 + NB:c0 + NB + 1], scalar1=1e-30, scalar2=None, op0=ALU.max))
            sc.op('dve', ['zr'], ['zr'], lambda e: e.reciprocal(out=zr[:, 4:8], in_=zr[:, 0:4]))
            for h in range(4):
                ib = IB_[h // 2]
                c0 = (h % 2) * (NB + 1)
                if h == 0:
                    sc.op('dve', [('ps', ib), 'zr'], ['imp'], lambda e, ib=ib, c0=c0: e.tensor_scalar(
                        out=imp[:], in0=PS[ib][:, c0:c0 + NB], scalar1=zr[:, 4:5], scalar2=None, op0=ALU.mult))
                else:
                    sc.op('dve', [('ps', ib), 'zr', 'imp'], ['imp'], lambda e, h=h, ib=ib, c0=c0: e.scalar_tensor_tensor(
                        out=imp[:], in0=PS[ib][:, c0:c0 + NB], scalar=zr[:, 4 + h:5 + h], in1=imp[:],
                        op0=ALU.mult, op1=ALU.add))
            yield
            jsl = slice(NB - 2 * i, 2 * NB - 2 * i)
            sc.op('dve', ['imp', 'amnf'], ['sc1'], lambda e, jsl=jsl: e.tensor_tensor(
                out=sc1[:], in0=imp[:], in1=amnf[:, jsl], op=ALU.mult))
            sc.op('dve', ['sc1', 'fbna'], ['sc1'], lambda e, jsl=jsl: e.tensor_tensor(
                out=sc1[:], in0=sc1[:], in1=fbna[:, jsl], op=ALU.add))
            sc.op('dve', ['sc1'], ['sc1'], lambda e: e.memset(sc1[:, 0:1], BIGV))
            sc.op('dve', ['sc1'], ['m8'], lambda e: e.max(out=m8[:, 0:8], in_=sc1[:]))
            sc.op('dve', ['sc1', 'm8'], ['sc2'], lambda e: e.match_replace(
                out=sc2[:], in_to_replace=m8[:, 0:8], in_values=sc1[:], imm_value=-2.0))
            sc.op('dve', ['sc2'], ['m8'], lambda e: e.max(out=m8[:, 8:16], in_=sc2[:]))
            sc.op('dve', ['sc1', 'm8'], ['sc2'], lambda e: e.tensor_scalar(
                out=sc2[:], in0=sc1[:], scalar1=m8[:, 15:16], scalar2=None, op0=ALU.is_ge))
            sc.op('dve', ['sc2'], ['selm'], lambda e: e.tensor_scalar(
                out=selm[:, 0:NB], in0=sc2[:], scalar1=-1.0, scalar2=-NEG, op0=ALU.add, op1=ALU.mult))
            yield
            j0 = max(0, i - 4)
            pairs = []
            for j in range(j0, i + 1):
                am = []
                if j == i:
                    am.append((ident[:], bc4(caus[:]), ['ident', 'caus']))
                elif j == i - 4:
                    am.append((ident[:], bc4(band[:]), ['ident', 'band']))
                pairs.append((kwT[:, j * 128:(j + 1) * 128], 'kwT', am, vw[:, j, :], 'vw'))
            yield from branch(pairs)
            finish(2)
            sc.op('pe', ['selm', 'ident'], ['pb1'], lambda e: e.transpose(
                out=pbk[:, 0:128], in_=selm[:], identity=ident[:]))
            sc.op('act', ['pb1'], ['selT'], lambda e: e.activation(out=selT[:], in_=pbk[:, 0:128], func=AF.Copy))
            pairs = []
            for j in range(i + 1):
                am = [(Eb[0:NB, j * 128:(j + 1) * 128], bc4(selT[0:NB, :]), ['E', 'selT'])]
                if j == i:
                    am.append((ident[:], bc4(caus[:]), ['ident', 'caus']))
                pairs.append((ksT[:, j * 128:(j + 1) * 128], 'ksT', am, vs[:, j, :], 'vs'))
            yield from branch(pairs)
            finish(1)
            for x in range(3):
                sc.op('dve', [('zx', x), 'gf'], [('zx', x)], lambda e, x=x: e.tensor_tensor(
                    out=zx[x][:], in0=zx[x][:], in1=gf[:, 4 * x:4 * x + 4, :].rearrange("p h t -> p (h t)"), op=ALU.mult))
                if x == 0:
                    sc.op('pool', [('zx', x), ('ox', x)], [('ox', 0)], lambda e, x=x: e.tensor_tensor(
                        out=acc[:], in0=ox[x][:], in1=zx[x][:], op=ALU.mult))
                else:
                    sc.op('pool', [('zx', x), ('ox', x)], [('ox', x)], lambda e, x=x: e.tensor_tensor(
                        out=ox[x][:], in0=ox[x][:], in1=zx[x][:], op=ALU.mult))
                    if x == 1:
                        sc.op('dve', [('ox', 0), ('ox', x)], [('ox', 0)], lambda e, x=x: e.tensor_tensor(
                            out=acc[:], in0=acc[:], in1=ox[x][:], op=ALU.add))
                    else:
                        sc.op('dve', [('ox', 0), ('ox', x)], ['ob'], lambda e, x=x: e.tensor_tensor(
                            out=ob[:], in0=acc[:], in1=ox[x][:], op=ALU.add))
            sc.dma(STORE_Q, OT[0].rearrange("c (hl e) t -> e (c hl) t", e=64)[:, :, t0:t0 + 128],
                   ob[:].rearrange("p (h t) -> p h t", h=4), ['ob'], [])
            yield


_CACHE = {}


def kernel(**inputs):
    S = 8192
    ncores = 8
    if 'mk' not in _CACHE:
        _CACHE['mk'] = build_program(S)
    mk = _CACHE['mk']
    inp = {k: np.asarray(v) for k, v in inputs.items()}
    shared = host_inputs_shared(inp, S=S)
    shared = {k: np.ascontiguousarray(v, dtype=np.float32) for k, v in shared.items() if k in mk.inputs}
    in_maps = []
    for c in range(ncores):
        d = dict(shared)
        d['x'] = np.ascontiguousarray(inp['x'][c], dtype=np.float32)
        d['p'] = np.ascontiguousarray(inp['p'][:, c], dtype=np.float32)
        in_maps.append(d)
    res = run_bass_kernel_spmd(mk.nc, in_maps, core_ids=list(range(ncores)))
    return np.stack([np.asarray(r['out'], dtype=np.float32) for r in res.results], axis=0)
```

```python
import numpy as np
from contextlib import ExitStack
import concourse.bass as bass
import concourse.mybir as mybir
from concourse.bass_utils import run_bass_kernel_spmd

F32 = mybir.dt.float32
BF16 = mybir.dt.bfloat16
AF = mybir.ActivationFunctionType
ALU = mybir.AluOpType
AX = mybir.AxisListType

D = 1024
DEPTH = 2
DFF = 2816
NFC = DFF // 128
NSA_BASE, HG_BASE, RET_BASE, RW_BASE = 0, 652, 1676, 2700
EPS = 1e-6
STORE_Q = 'pool'


class _Keep:
    def __init__(self, es):
        self.es = es

    def __enter__(self):
        return self.es

    def __exit__(self, *a):
        return False


class Sched:
    EPOCH = 8000
    NDMA = 24

    def __init__(self, nc, es):
        self.nc = nc
        self.es = es
        self.E = {'pe': nc.tensor, 'dve': nc.vector, 'act': nc.scalar, 'pool': nc.gpsimd, 'sp': nc.sync}
        self.sems = []
        self.prog = {}
        for e in self.E:
            self.prog[e] = [self.new_sem(), 0]
        self.waited = {e: {} for e in self.E}
        self.lastw = {}
        self.readers = {}
        self.dma_pool = [self.new_sem() for _ in range(self.NDMA)]
        self.dma_val = {s: 0 for s in self.dma_pool}
        self.dma_next = 0
        self.n_inst = 0
        self.ns = None
        self.pe_sync = False

    def new_sem(self):
        h = self.es.enter_context(self.nc.semaphore("s%d" % len(self.sems)))
        self.sems.append(h)
        return len(self.sems) - 1

    def _wait(self, eng, deps):
        w = self.waited[eng]
        for s, v in deps.items():
            if w.get(s, 0) >= v:
                continue
            self.E[eng].wait_ge(self.sems[s], v)
            w[s] = v

    def _deps(self, reads, writes):
        deps = {}
        for k in reads:
            lw = self.lastw.get(k)
            if lw and deps.get(lw[0], 0) < lw[1]:
                deps[lw[0]] = lw[1]
        for k in writes:
            lw = self.lastw.get(k)
            if lw and deps.get(lw[0], 0) < lw[1]:
                deps[lw[0]] = lw[1]
            rd = self.readers.get(k)
            if rd:
                for s, v in rd.items():
                    if deps.get(s, 0) < v:
                        deps[s] = v
        return deps

    def _record(self, reads, writes, s, v):
        for k in writes:
            self.lastw[k] = (s, v)
            self.readers[k] = {}
        for k in reads:
            self.readers.setdefault(k, {})[s] = v

    def op(self, eng, r, w, fn):
        r = [self.nk(k) for k in r]
        w = [self.nk(k) for k in w]
        pk = [k for k in r if (isinstance(k, tuple) and k[0] == 'ps') or k == 'pb0']
        if pk:
            r = [k for k in r if k not in pk]
            w = list(w) + pk
        sync = False
        if eng == 'pes':
            eng, sync = 'pe', True
        deps = self._deps(r, w)
        pr = self.prog[eng]
        if eng == 'pe' and not sync:
            deps.pop(pr[0], None)
        self._wait(eng, deps)
        inst = fn(self.E[eng])
        pr[1] += 1
        inst.then_inc(self.sems[pr[0]], 1)
        self._record(r, w, pr[0], pr[1])
        self.n_inst += 1
        ret = (pr[0], pr[1])
        if pr[1] >= self.EPOCH:
            self.prog[eng] = [self.new_sem(), 0]
        return ret

    GLOBAL_KEYS = {'pb0', 'ident', 'identf', 'onesbd', 'gmask', 'rmask', 'rw_mask4', 'rw_lmask', 'rw_rmask'}

    def nk(self, k):
        if k == 'pb1':
            return 'pb0'
        if self.ns is None or k in self.GLOBAL_KEYS or (isinstance(k, tuple) and k[0] == 'ps'):
            return k
        return (self.ns, k)

    def run_streams(self, streams):
        live = list(streams)
        while live:
            for item in list(live):
                self.ns = item[0]
                try:
                    next(item[1])
                except StopIteration:
                    live.remove(item)
        self.ns = None

    def dma(self, eng, out, in_, r, w):
        r = [self.nk(k) for k in r]
        w = [self.nk(k) for k in w]
        s = self.dma_pool[self.dma_next % self.NDMA]
        self.dma_next += 1
        deps = self._deps(r, w)
        pv = self.dma_val[s]
        if pv and deps.get(s, 0) < pv:
            deps[s] = pv
        self._wait(eng, deps)
        self.E[eng].dma_start(out=out, in_=in_).then_inc(self.sems[s], 16)
        self.dma_val[s] = pv + 16
        self._record(r, w, s, pv + 16)
        self.n_inst += 1

    def barrier(self):
        allv = {}
        for e, (s, v) in self.prog.items():
            if v:
                allv[s] = v
        for s, v in self.dma_val.items():
            if v:
                allv[s] = v
        for e in self.E:
            d = dict(allv)
            self._wait(e, d)
        self.lastw = {}
        self.readers = {}


class MK:
    def __init__(self, S, last_layer_final=True):
        self.S = S
        self.nc = bass.Bass("TRN2", target_bir_lowering=False)
        self.inputs = {}

    def din(self, name, shape, dt=F32):
        t = self.nc.dram_tensor(name, list(shape), dt, kind="ExternalInput").ap()
        self.inputs[name] = t
        return t

    def dscr(self, name, shape, dt=F32):
        return self.nc.dram_tensor(name, list(shape), dt, kind="Internal").ap()

    def sb(self, es, name, shape, dt):
        self.uid = getattr(self, 'uid', 0) + 1
        return es.enter_context(self.nc.sbuf_tensor("%s_u%d" % (name, self.uid), list(shape), dt))


def _mm(sc, ps_key, ps_ap, pairs, rkeys):
    n = len(pairs)
    for i, (l, r) in enumerate(pairs):
        sc.op('pe', rkeys, [ps_key],
              lambda e, l=l, r=r, i=i: e.matmul(ps_ap, lhsT=l, rhs=r, start=(i == 0), stop=(i == n - 1)))


def p1_plan():
    cols = []
    groups = []

    def add(name, idx):
        groups.append((name, len(cols), len(idx)))
        cols.extend(idx)

    b = NSA_BASE
    for h in range(4):
        add('nsa_q%d' % h, list(range(b + 64 * h, b + 64 * h + 64)))
    add('nsa_kc', list(range(b + 256, b + 320)))
    add('nsa_vc', list(range(b + 320, b + 384)))
    add('nsa_ks', list(range(b + 384, b + 448)))
    add('nsa_kw', list(range(b + 512, b + 576)))
    for gi in range(12):
        add('nsa_g%d' % gi, [b + 640 + gi] * 64)
    b = HG_BASE
    for nm, off in (('hg_q', 0), ('hg_f', 256), ('hg_og', 768)):
        for c in range(2):
            add('%s%d' % (nm, c), list(range(b + off + 128 * c, b + off + 128 * c + 128)))
    b = RET_BASE

    def rot(base):
        out = []
        for h in range(4):
            for d in range(64):
                out.append(base + 64 * h + (d + 32) % 64)
        return out
    for nm, idx in (('ret_q', list(range(b, b + 256))), ('ret_qr', rot(b)),
                    ('ret_k', list(range(b + 256, b + 512))), ('ret_kr', rot(b + 256)),
                    ('ret_g', list(range(b + 768, b + 1024)))):
        for c in range(2):
            add('%s%d' % (nm, c), idx[128 * c:128 * c + 128])
    b = RW_BASE
    for c in range(8):
        add('rw%d' % c, list(range(b + 128 * c, b + 128 * c + 128)))
    nch = len(cols)
    tok = (list(range(NSA_BASE + 448, NSA_BASE + 512)) + list(range(NSA_BASE + 576, NSA_BASE + 640))
           + list(range(HG_BASE + 512, HG_BASE + 768)) + list(range(RET_BASE + 512, RET_BASE + 768)))
    cols.extend(tok)
    return np.array(cols, dtype=np.int64), groups, nch, len(tok)


P1_COLS, P1_GROUPS, P1_NCH, P1_NTOK = p1_plan()
P1_NC = len(P1_COLS)
GIDX = {g[0]: i for i, g in enumerate(P1_GROUPS)}
NG = len(P1_GROUPS)


class Dense:
    def __init__(self, mk, sc, es):
        self.mk, self.sc, self.nc = mk, sc, mk.nc
        nc = self.nc
        self.ps = [es.enter_context(nc.psum_tensor("ps%d" % i, [128, 512], F32)) for i in range(7)]
        pb = es.enter_context(nc.psum_tensor("pb0", [128, 1024], BF16))
        self.pb = [pb, pb]
        self.ps_pool = list(range(7))
        self.ps_i = 0
        self.ident = mk.sb(es, "ident", [128, 128], BF16)
        self.identf = mk.sb(es, "identf", [128, 128], F32)
        idin = mk.din("c_ident", [128, 128])
        sc.dma('sp', self.identf[:], idin[:, :], [], ['identf'])
        sc.op('dve', ['identf'], ['ident'], lambda e: e.tensor_copy(out=self.ident[:], in_=self.identf[:]))
        self.ev = 0

    def next_ps(self):
        i = self.ps_pool[self.ps_i % len(self.ps_pool)]
        self.ps_i += 1
        return i

    def evac_eng(self):
        self.ev += 1
        return 'act' if self.ev % 2 else 'dve'

    def copy(self, eng, out, in_, r, w):
        if eng == 'act':
            self.sc.op('act', r, w, lambda e: e.activation(out=out, in_=in_, func=AF.Copy))
        else:
            self.sc.op(eng, r, w, lambda e: e.tensor_copy(out=out, in_=in_))

    def load_w(self, es, name, src, K, N, stage):
        sc = self.sc
        kc = K // 128
        dst = self.mk.sb(es, name, [128, kc, N], BF16)
        engs = ['pool', 'dve', 'act']
        for c in range(kc):
            for n0 in range(0, N, 2048):
                n1 = min(N, n0 + 2048)
                si = self.ev % 2
                self.ev += 1
                st = stage[si]
                sc.dma('sp', st[:, 0:n1 - n0], src[c * 128:(c + 1) * 128, n0:n1], [], [('wst', si)])
                self.copy(engs[self.ev % 3], dst[:, c, n0:n1], st[:, 0:n1 - n0], [('wst', si)], [name])
        return dst

    def front(self, bufs, h_src, t0, nsub, gain_key):
        sc = self.sc
        hk, xnT, xn, junk, ss, gain = bufs['h'], bufs['xnT'], bufs['xn'], bufs['junk'], bufs['ss'], bufs[gain_key]
        pb = self.pb[0]
        for sub in range(nsub):
            r0 = t0 + sub * 128
            sc.dma('sp', hk[:, sub, :], h_src[r0:r0 + 128, :], [], [('h', sub)])
            self.norm_T(bufs, hk[:, sub, :], ('h', sub), gain, gain_key, sub)

    def rstd(self, h_key, h_ap, junk, ss):
        sc = self.sc
        sc.op('act', [h_key], ['junk', 'ss'], lambda e: e.activation(
            out=junk[:], in_=h_ap, func=AF.Square, scale=1.0 / 32.0, accum_out=ss[:, 0:1]))
        sc.op('dve', ['ss'], ['ss'], lambda e: e.tensor_scalar(
            out=ss[:, 1:2], in0=ss[:, 0:1], scalar1=EPS, scalar2=None, op0=ALU.add))
        sc.op('act', ['ss'], ['ss'], lambda e: e.activation(out=ss[:, 3:4], in_=ss[:, 1:2], func=AF.Sqrt))
        sc.op('dve', ['ss'], ['ss'], lambda e: e.reciprocal(out=ss[:, 2:3], in_=ss[:, 3:4]))

    def norm_T(self, bufs, h_ap, h_key, gain, gain_key, sub):
        sc = self.sc
        xnT, xn, junk, ss = bufs['xnT'], bufs['xn'], bufs['junk'], bufs['ss']
        pb = self.pb[0]
        self.rstd(h_key, h_ap, junk, ss)
        sc.op('dve', [h_key, 'ss', gain_key], ['xn'], lambda e: e.scalar_tensor_tensor(
            out=xn[:], in0=h_ap, scalar=ss[:, 2:3], in1=gain[:], op0=ALU.mult, op1=ALU.mult))
        for k in range(8):
            sc.op('pe', ['xn', 'ident'], ['pb0'], lambda e, k=k: e.transpose(
                out=pb[:, k * 128:(k + 1) * 128], in_=xn[:, k * 128:(k + 1) * 128], identity=self.ident[:]))
        sc.op('act', ['pb0'], ['xnT'], lambda e: e.activation(
            out=xnT[:, :, sub * 128:(sub + 1) * 128], in_=pb[:].rearrange("p (k t) -> p k t", k=8), func=AF.Copy))

    def front_bufs(self, es, nsub, gains):
        mk = self.mk
        b = {'h': mk.sb(es, "f_h", [128, nsub, D], F32), 'xnT': mk.sb(es, "f_xnT", [128, 8, nsub * 128], BF16),
             'xn': mk.sb(es, "f_xn", [128, D], BF16), 'junk': mk.sb(es, "f_junk", [128, D], BF16),
             'ss': mk.sb(es, "f_ss", [128, 4], F32)}
        for key, src in gains.items():
            b[key] = mk.sb(es, "f_" + key, [128, D], F32)
            self.sc.dma('sp', b[key][:], src, [], [key])
        return b

    def pass_p1(self, L, h_src, w1_src, gain_src, UC, UT):
        sc, mk, S = self.sc, self.mk, self.mk.S
        with ExitStack() as es:
            stage = [mk.sb(es, "wst%d" % i, [128, 2048], F32) for i in range(2)]
            W1 = self.load_w(es, "W1", w1_src, D, P1_NC, stage)
            fb = self.front_bufs(es, 4, {'gain': gain_src})
            ost = [mk.sb(es, "ost%d" % i, [128, 640], F32) for i in range(4)]
            oi = 0
            for t0 in range(0, S, 512):
                self.front(fb, h_src, t0, 4, 'gain')
                xnT = fb['xnT']
                for gi, (name, off, M) in enumerate(P1_GROUPS):
                    pi = self.next_ps()
                    ps = self.ps[pi]
                    _mm(sc, ('ps', pi), ps[0:M, :],
                        [(W1[:, k, off:off + M], xnT[:, k, :]) for k in range(8)], ['W1', 'xnT'])
                    o = oi % 4
                    oi += 1
                    self.copy(self.evac_eng(), ost[o][0:M, 0:512], ps[0:M, :], [('ps', pi)], [('ost', o)])
                    sc.dma(STORE_Q, UC[gi, 0:M, t0:t0 + 512], ost[o][0:M, 0:512], [('ost', o)], [])
                for sub in range(4):
                    o = oi % 4
                    oi += 1
                    for (c0, c1) in ((0, 512), (512, P1_NTOK)):
                        pi = self.next_ps()
                        ps = self.ps[pi]
                        _mm(sc, ('ps', pi), ps[:, 0:c1 - c0],
                            [(xnT[:, k, sub * 128:(sub + 1) * 128], W1[:, k, P1_NCH + c0:P1_NCH + c1])
                             for k in range(8)], ['W1', 'xnT'])
                        self.copy(self.evac_eng(), ost[o][:, c0:c1], ps[:, 0:c1 - c0], [('ps', pi)], [('ost', o)])
                    r0 = t0 + sub * 128
                    sc.dma(STORE_Q, UT[r0:r0 + 128, :], ost[o][:, 0:P1_NTOK], [('ost', o)], [])
            sc.barrier()

    def pass_merge(self, L, h_src, h_dst, OT, wg_src, wb_src, wo_src, bg_src, gain_src):
        sc, mk, S = self.sc, self.mk, self.mk.S
        with ExitStack() as es:
            stage = [mk.sb(es, "wst%d" % i, [128, 2048], F32) for i in range(2)]
            Wg = [self.load_w(es, "Wg%d" % m, wg_src[m], D, D, stage) for m in range(4)]
            Wb = [self.load_w(es, "Wb%d" % m, wb_src[m], 256, D, stage) for m in range(4)]
            Wo = self.load_w(es, "Wo", wo_src, D, D, stage)
            bg = mk.sb(es, "bg", [128, 32], F32)
            sc.dma('sp', bg[:], bg_src, [], ['bg'])
            NS = 4
            TT = NS * 128
            fb = self.front_bufs(es, NS, {'gain': gain_src})
            ot = mk.sb(es, "m_ot", [128, 8, TT], BF16)
            gsb = [mk.sb(es, "m_g%d" % i, [128, TT], F32) for i in range(2)]
            acc = mk.sb(es, "m_acc", [128, TT], F32)
            tmp = mk.sb(es, "m_tmp", [128, TT], F32)
            mT = mk.sb(es, "m_mT", [128, 8, TT], BF16)
            hn = mk.sb(es, "m_hn", [128, D], F32)
            gi_ = 0
            for t0 in range(0, S, TT):
                self.front(fb, h_src, t0, NS, 'gain')
                xnT = fb['xnT']
                for m in range(4):
                    for c in range(2):
                        sc.dma('sp', ot[:, m * 2 + c, :], OT[m, c, :, t0:t0 + TT], [], ['ot'])
                for j in range(8):
                    for m in range(4):
                        pg = self.next_ps()
                        _mm(sc, ('ps', pg), self.ps[pg][:, 0:TT],
                            [(Wg[m][:, k, j * 128:(j + 1) * 128], xnT[:, k, :]) for k in range(8)],
                            ['Wg%d' % m, 'xnT'])
                        pbr = self.next_ps()
                        _mm(sc, ('ps', pbr), self.ps[pbr][:, 0:TT],
                            [(Wb[m][:, c, j * 128:(j + 1) * 128], ot[:, m * 2 + c, :]) for c in range(2)],
                            ['Wb%d' % m, 'ot'])
                        g = gi_ % 2
                        gi_ += 1
                        sc.op('act', [('ps', pg), 'bg'], [('gsb', g)], lambda e, g=g, pg=pg, m=m, j=j: e.activation(
                            out=gsb[g][:], in_=self.ps[pg][:, 0:TT], func=AF.Sigmoid,
                            bias=bg[:, m * 8 + j:m * 8 + j + 1], scale=1.0))
                        if m == 0:
                            sc.op('dve', [('gsb', g), ('ps', pbr)], ['acc'], lambda e, g=g, pbr=pbr: e.tensor_tensor(
                                out=acc[:], in0=gsb[g][:], in1=self.ps[pbr][:, 0:TT], op=ALU.mult))
                        else:
                            sc.op('dve', [('gsb', g), ('ps', pbr)], ['tmp'], lambda e, g=g, pbr=pbr: e.tensor_tensor(
                                out=tmp[:], in0=gsb[g][:], in1=self.ps[pbr][:, 0:TT], op=ALU.mult))
                            if m < 3:
                                sc.op('pool', ['tmp', 'acc'], ['acc'], lambda e: e.tensor_tensor(
                                    out=acc[:], in0=acc[:], in1=tmp[:], op=ALU.add))
                            else:
                                sc.op('pool', ['tmp', 'acc'], ['mT'], lambda e, j=j: e.tensor_tensor(
                                    out=mT[:, j, :], in0=acc[:], in1=tmp[:], op=ALU.add))
                for sub in range(NS):
                    for half in range(2):
                        po = self.next_ps()
                        _mm(sc, ('ps', po), self.ps[po][:, :],
                            [(mT[:, k, sub * 128:(sub + 1) * 128], Wo[:, k, half * 512:(half + 1) * 512])
                             for k in range(8)], ['Wo', 'mT'])
                        sc.op('dve', [('ps', po), ('h', sub)], ['hn'], lambda e, po=po, sub=sub, half=half: e.tensor_tensor(
                            out=hn[:, half * 512:(half + 1) * 512], in0=fb['h'][:, sub, half * 512:(half + 1) * 512],
                            in1=self.ps[po][:, :], op=ALU.add))
                    r0 = t0 + sub * 128
                    sc.dma(STORE_Q, h_dst[r0:r0 + 128, :], hn[:], ['hn'], [])
            sc.barrier()

    def pass_ffn(self, L, h_src, h_dst, wfg_src, wfu_src, wfd_src, gain_src):
        sc, mk, S = self.sc, self.mk, self.mk.S
        with ExitStack() as es:
            stage = [mk.sb(es, "wst%d" % i, [128, 2048], F32) for i in range(2)]
            Wfg = self.load_w(es, "Wfg", wfg_src, D, DFF, stage)
            Wfu = self.load_w(es, "Wfu", wfu_src, D, DFF, stage)
            Wfd = self.load_w(es, "Wfd", wfd_src, DFF, D, stage)
            NS = 2
            TT = NS * 128
            fb = self.front_bufs(es, NS, {'gain': gain_src})
            hid = mk.sb(es, "f_hid", [128, NFC, TT], BF16)
            gsb = [mk.sb(es, "f_g%d" % i, [128, TT], F32) for i in range(2)]
            hn = mk.sb(es, "f_hn", [128, D], F32)
            gi_ = 0
            for t0 in range(0, S, TT):
                self.front(fb, h_src, t0, NS, 'gain')
                xnT = fb['xnT']
                for f in range(NFC):
                    pg = self.next_ps()
                    _mm(sc, ('ps', pg), self.ps[pg][:, 0:TT],
                        [(Wfg[:, k, f * 128:(f + 1) * 128], xnT[:, k, :]) for k in range(8)], ['Wfg', 'xnT'])
                    pu = self.next_ps()
                    _mm(sc, ('ps', pu), self.ps[pu][:, 0:TT],
                        [(Wfu[:, k, f * 128:(f + 1) * 128], xnT[:, k, :]) for k in range(8)], ['Wfu', 'xnT'])
                    g = gi_ % 2
                    gi_ += 1
                    sc.op('act', [('ps', pg)], [('gsb', g)], lambda e, g=g, pg=pg: e.activation(
                        out=gsb[g][:], in_=self.ps[pg][:, 0:TT], func=AF.Silu))
                    sc.op('dve', [('gsb', g), ('ps', pu)], ['hid'], lambda e, g=g, pu=pu, f=f: e.tensor_tensor(
                        out=hid[:, f, :], in0=gsb[g][:], in1=self.ps[pu][:, 0:TT], op=ALU.mult))
                for sub in range(NS):
                    for half in range(2):
                        po = self.next_ps()
                        _mm(sc, ('ps', po), self.ps[po][:, :],
                            [(hid[:, f, sub * 128:(sub + 1) * 128], Wfd[:, f, half * 512:(half + 1) * 512])
                             for f in range(NFC)], ['Wfd', 'hid'])
                        sc.op('dve', [('ps', po), ('h', sub)], ['hn'], lambda e, po=po, sub=sub, half=half: e.tensor_tensor(
                            out=hn[:, half * 512:(half + 1) * 512], in0=fb['h'][:, sub, half * 512:(half + 1) * 512],
                            in1=self.ps[po][:, :], op=ALU.add))
                    r0 = t0 + sub * 128
                    sc.dma(STORE_Q, h_dst[r0:r0 + 128, :], hn[:], ['hn'], [])
            sc.barrier()

    def pass_ple(self, L, h_src, h_dst, p_src, wpg_src, wpp_src, gain_src, final_gain_src):
        sc, mk, S = self.sc, self.mk, self.mk.S
        with ExitStack() as es:
            stage = [mk.sb(es, "wst%d" % i, [128, 2048], F32) for i in range(2)]
            Wpg = self.load_w(es, "Wpg", wpg_src, D, D, stage)
            Wpp = self.load_w(es, "Wpp", wpp_src, 256, D, stage)
            NS = 4
            gains = {'gain': gain_src}
            if final_gain_src is not None:
                gains['fgain'] = final_gain_src
            fb = self.front_bufs(es, NS, gains)
            pt = mk.sb(es, "p_pt", [128, 256], F32)
            ptb = mk.sb(es, "p_ptb", [128, 256], BF16)
            pT = mk.sb(es, "p_pT", [128, 2, 128], BF16)
            gsb = mk.sb(es, "p_g", [128, 512], F32)
            hn = mk.sb(es, "p_hn", [128, D], F32)
            ho = mk.sb(es, "p_ho", [128, D], F32)
            pbk = self.pb[1]
            for t0 in range(0, S, NS * 128):
                self.front(fb, h_src, t0, NS, 'gain')
                xnT = fb['xnT']
                for sub in range(NS):
                    r0 = t0 + sub * 128
                    sc.dma('sp', pt[:], p_src[r0:r0 + 128, :], [], ['pt'])
                    sc.op('pool', ['pt'], ['ptb'], lambda e: e.tensor_copy(out=ptb[:], in_=pt[:]))
                    for c in range(2):
                        sc.op('pe', ['ptb', 'ident'], ['pb1'], lambda e, c=c: e.transpose(
                            out=pbk[:, c * 128:(c + 1) * 128], in_=ptb[:, c * 128:(c + 1) * 128], identity=self.ident[:]))
                    sc.op('dve', ['pb1'], ['pT'], lambda e: e.tensor_copy(
                        out=pT[:], in_=pbk[:, 0:256].rearrange("p (c t) -> p c t", c=2)))
                    for half in range(2):
                        pg = self.next_ps()
                        _mm(sc, ('ps', pg), self.ps[pg][:, :],
                            [(xnT[:, k, sub * 128:(sub + 1) * 128], Wpg[:, k, half * 512:(half + 1) * 512])
                             for k in range(8)], ['Wpg', 'xnT'])
                        pp = self.next_ps()
                        _mm(sc, ('ps', pp), self.ps[pp][:, :],
                            [(pT[:, c, :], Wpp[:, c, half * 512:(half + 1) * 512]) for c in range(2)], ['Wpp', 'pT'])
                        sc.op('act', [('ps', pg)], ['gsb'], lambda e, pg=pg: e.activation(
                            out=gsb[:], in_=self.ps[pg][:, :], func=AF.Sigmoid))
                        sc.op('dve', ['gsb', ('ps', pp)], ['gsb'], lambda e, pp=pp: e.tensor_tensor(
                            out=gsb[:], in0=gsb[:], in1=self.ps[pp][:, :], op=ALU.mult))
                        sc.op('pool', ['gsb', ('h', sub)], ['hn'], lambda e, sub=sub, half=half: e.tensor_tensor(
                            out=hn[:, half * 512:(half + 1) * 512], in0=fb['h'][:, sub, half * 512:(half + 1) * 512],
                            in1=gsb[:], op=ALU.add))
                    if final_gain_src is None:
                        sc.dma(STORE_Q, h_dst[r0:r0 + 128, :], hn[:], ['hn'], [])
                    else:
                        ss, junk = fb['ss'], fb['junk']
                        self.rstd('hn', hn[:], junk, ss)
                        sc.op('dve', ['hn', 'ss', 'fgain'], ['ho'], lambda e: e.scalar_tensor_tensor(
                            out=ho[:], in0=hn[:], scalar=ss[:, 2:3], in1=fb['fgain'][:], op0=ALU.mult, op1=ALU.mult))
                        sc.dma(STORE_Q, h_dst[r0:r0 + 128, :], ho[:], ['ho'], [])
            sc.barrier()


def rep128(v):
    return np.ascontiguousarray(np.broadcast_to(np.asarray(v, np.float32)[None, :], (128, v.shape[-1])))


def build_program(S, debug=False, ext_ot=False, layers=DEPTH, skip_mixers=False, mixers=('nsa', 'hg', 'ret', 'rw'), dense=True):
    mk = MK(S)
    nc = mk.nc
    if debug:
        mk.dscr = lambda name, shape, dt=F32: nc.dram_tensor(name, list(shape), dt, kind="ExternalOutput").ap()
    x = mk.din("x", [S, D])
    p = mk.din("p", [DEPTH, S, 256])
    out = nc.dram_tensor("out", [S, D], F32, kind="ExternalOutput").ap()
    w = {}
    for L in range(layers):
        w[L] = dict(
            w1=mk.din("w1_%d" % L, [D, P1_NC]), g_mix=mk.din("g_mix_%d" % L, [128, D]),
            wg=mk.din("wg_%d" % L, [4, D, D]), wb=mk.din("wb_%d" % L, [4, 256, D]), wo=mk.din("wo_%d" % L, [D, D]),
            bg=mk.din("bg_%d" % L, [128, 32]),
            g_ffn=mk.din("g_ffn_%d" % L, [128, D]), wfg=mk.din("wfg_%d" % L, [D, DFF]),
            wfu=mk.din("wfu_%d" % L, [D, DFF]), wfd=mk.din("wfd_%d" % L, [DFF, D]),
            g_ple=mk.din("g_ple_%d" % L, [128, D]), wpg=mk.din("wpg_%d" % L, [D, D]), wpp=mk.din("wpp_%d" % L, [256, D]),
        )
    g_final = mk.din("g_final", [128, D])
    lbz = mk.din("hg_lbz", [128, DEPTH * 2])
    for L in range(layers):
        w[L].update(hgn=mk.din("hg_gn_%d" % L, [128, 2]))
        w[L].update(retgb=mk.din("ret_gb_%d" % L, [128, 4]))
        w[L].update(ns_w1k=mk.din("ns_w1k_%d" % L, [64, 32 * 128]), ns_w1v=mk.din("ns_w1v_%d" % L, [64, 32 * 128]),
                    ns_w2k=mk.din("ns_w2k_%d" % L, [128, 64]), ns_w2v=mk.din("ns_w2v_%d" % L, [128, 64]),
                    ns_pk=mk.din("ns_pk_%d" % L, [64, 32]), ns_pv=mk.din("ns_pv_%d" % L, [64, 32]))
        w[L].update(rw_wup=mk.din("rw_wup_%d" % L, [64, 256]), rw_aup=mk.din("rw_aup_%d" % L, [64, 256]),
                    rw_gup=mk.din("rw_gup_%d" % L, [128, 256]), rw_pvec=mk.din("rw_pvec_%d" % L, [128, 24]))
    UC = mk.dscr("UC", [NG, 128, S])
    UT = mk.dscr("UT", [S, P1_NTOK])
    if ext_ot:
        OT = mk.din("OT", [4, 2, 128, S], BF16)
    else:
        OT = mk.dscr("OT", [4, 2, 128, S], BF16)
    hA = mk.dscr("hA", [S, D])
    hB = mk.dscr("hB", [S, D])
    hC = mk.dscr("hC", [S, D])
    with ExitStack() as es:
        sc = Sched(nc, es)
        dn = Dense(mk, sc, es)
        gla = GLA(dn, es)
        rwk = RWKV(dn, gla, es)
        nsa = NSA(dn, gla, es)
        sc.barrier()
        h_in = x
        for L in range(layers):
            wl = w[L]
            dn.pass_p1(L, h_in, wl['w1'], wl['g_mix'], UC, UT)
            if not skip_mixers:
                with ExitStack() as esx:
                    streams = []
                    if 'nsa' in mixers:
                        streams.append(('nsa', nsa.run(L, UC, UT, OT, {
                            'w1r': [wl['ns_w1k'], wl['ns_w1v']], 'w2': [wl['ns_w2k'][:, :], wl['ns_w2v'][:, :]],
                            'posT': [wl['ns_pk'][:, :], wl['ns_pv'][:, :]]}, esx)))
                    if 'rw' in mixers:
                        streams.append(('rw', rwk.run(L, UC, OT, {'w_up': wl['rw_wup'][:, :], 'a_up': wl['rw_aup'][:, :],
                                                                  'g_up': wl['rw_gup'][:, :], 'pvec': wl['rw_pvec'][:, :]}, esx)))
                    sc.run_streams(streams)
                    sc.barrier()
                with ExitStack() as esy:
                    streams = []
                    if 'hg' in mixers:
                        streams.append(('hg', gla.run_hgrn(L, UC, UT, OT, lbz[:, :], wl['hgn'][:, :], esy)))
                    if 'ret' in mixers:
                        streams.append(('ret', gla.run_ret(L, UC, UT, OT, wl['retgb'][:, :], esy)))
                    sc.run_streams(streams)
                    sc.barrier()
            if not dense:
                continue
            dn.pass_merge(L, h_in, hA, OT, wl['wg'], wl['wb'], wl['wo'], wl['bg'], wl['g_mix'])
            dn.pass_ffn(L, hA, hB, wl['wfg'], wl['wfu'], wl['wfd'], wl['g_ffn'])
            last = (L == layers - 1)
            dn.pass_ple(L, hB, out if last else hC, p[L], wl['wpg'], wl['wpp'], wl['g_ple'], g_final if last else None)
            h_in = hC
        sc.barrier()
    mk.n_inst = sc.n_inst
    return mk


def host_inputs_shared(inp, layers=DEPTH, S=8192):
    d = {"c_ident": np.eye(128, dtype=np.float32)}
    for L in range(layers):
        d["w1_%d" % L] = np.ascontiguousarray(inp['w_in'][L][:, P1_COLS])
        d["g_mix_%d" % L] = rep128(inp['norm_mix'][L])
        d["wg_%d" % L] = np.ascontiguousarray(inp['w_gate'][L])
        d["wb_%d" % L] = np.ascontiguousarray(inp['w_branch'][L])
        d["wo_%d" % L] = np.ascontiguousarray(inp['w_out'][L])
        d["bg_%d" % L] = np.ascontiguousarray(inp['b_gate'][L].reshape(4, 8, 128).transpose(2, 0, 1).reshape(128, 32))
        d["g_ffn_%d" % L] = rep128(inp['norm_ffn'][L])
        d["wfg_%d" % L] = np.ascontiguousarray(inp['w_ffn_gate'][L])
        d["wfu_%d" % L] = np.ascontiguousarray(inp['w_ffn_up'][L])
        d["wfd_%d" % L] = np.ascontiguousarray(inp['w_ffn_down'][L])
        d["g_ple_%d" % L] = rep128(inp['norm_ple'][L])
        d["wpg_%d" % L] = np.ascontiguousarray(inp['w_ple_gate'][L])
        d["wpp_%d" % L] = np.ascontiguousarray(inp['w_ple_proj'][L])
    d["g_final"] = rep128(inp['norm_final'])
    d.update(gla_consts())
    d["c_cos"], d["c_sin"] = rope_tables(S)
    d.update(rwkv_consts())
    d.update(nsa_consts(S))
    for L in range(layers):
        r1 = lambda w: np.ascontiguousarray(w.reshape(32, 64, 128).transpose(1, 0, 2).reshape(64, 32 * 128))
        d["ns_w1k_%d" % L] = r1(inp['nsa_cmp_k1'][L])
        d["ns_w1v_%d" % L] = r1(inp['nsa_cmp_v1'][L])
        d["ns_w2k_%d" % L] = np.ascontiguousarray(inp['nsa_cmp_k2'][L])
        d["ns_w2v_%d" % L] = np.ascontiguousarray(inp['nsa_cmp_v2'][L])
        d["ns_pk_%d" % L] = np.ascontiguousarray(inp['nsa_pos_k'][L].T)
        d["ns_pv_%d" % L] = np.ascontiguousarray(inp['nsa_pos_v'][L].T)
    for L in range(layers):
        c2 = lambda v: v.reshape(2, 128).T
        pvec = np.zeros((128, 24), np.float32)
        pvec[:, 0:8] = inp['rwkv_mu'][L].reshape(8, 128).T
        for j, nm in enumerate(['rwkv_w0', 'rwkv_a0', 'rwkv_k_k', 'rwkv_k_a', 'rwkv_r_k', 'rwkv_norm_g', 'rwkv_norm_b']):
            pvec[:, 8 + 2 * j:10 + 2 * j] = c2(inp[nm][L])
        d["rw_pvec_%d" % L] = pvec
        d["rw_wup_%d" % L] = np.ascontiguousarray(inp['rwkv_w_up'][L])
        d["rw_aup_%d" % L] = np.ascontiguousarray(inp['rwkv_a_up'][L])
        d["rw_gup_%d" % L] = np.ascontiguousarray(inp['rwkv_g_up'][L])
    d["hg_lbz"] = np.ascontiguousarray(inp['hgrn_lb_logits'].reshape(DEPTH, 2, 128).transpose(2, 0, 1).reshape(128, DEPTH * 2))
    for L in range(layers):
        d["hg_gn_%d" % L] = np.ascontiguousarray(inp['hgrn_norm'][L].reshape(2, 128).T)
        d["ret_gb_%d" % L] = np.ascontiguousarray(np.concatenate(
            [inp['ret_norm_g'][L].reshape(2, 128).T, inp['ret_norm_b'][L].reshape(2, 128).T], axis=1))
    return d


GLA_C = 32
RET_LOGG = [float(np.log(1.0 - 2.0 ** (-5.0 - h))) for h in range(4)]


def gla_consts():
    s = np.arange(128)
    m = ((s[:, None] // 32 == s[None, :] // 32) & (s[:, None] <= s[None, :])).astype(np.float32)
    d = {"c_gmask": np.ascontiguousarray(np.tile(m, (1, 4)))}
    rm = np.ones((128, 128), np.float32)
    rm[:, ::32] = 0.0
    d["c_rmask"] = rm
    bd = np.zeros((128, 128), np.float32)
    bd[:64, :64] = 1.0
    bd[64:, 64:] = 1.0
    d["c_onesbd"] = bd
    i = (np.arange(128) % 32).astype(np.float64)
    ret = np.zeros((2, 3, 128, 128), np.float32)
    for hp in range(2):
        for hl in range(2):
            lg = RET_LOGG[hp * 2 + hl]
            ret[hp, 0, hl * 64:(hl + 1) * 64, :] = np.exp((i + 1) * lg)[None, :]
            ret[hp, 1, hl * 64:(hl + 1) * 64, :] = 0.125 * np.exp(-(i + 1) * lg)[None, :]
            ret[hp, 2, hl * 64:(hl + 1) * 64, :] = 0.125 * np.exp((31 - i) * lg)[None, :]
    d["c_retdec"] = ret
    g32 = np.zeros((128, 2), np.float32)
    for hp in range(2):
        for hl in range(2):
            g32[hl * 64:(hl + 1) * 64, hp] = np.exp(32 * RET_LOGG[hp * 2 + hl])
    d["c_retg32"] = g32
    return d


def rope_tables(S):
    pos = np.arange(S, dtype=np.float32)
    inv = (10000.0 ** (-np.arange(0, 64, 2, dtype=np.float32) / 64)).astype(np.float32)
    ang = pos[None, :] * inv[:, None]
    c = np.cos(ang).astype(np.float32)
    s_ = np.sin(ang).astype(np.float32)
    cos64 = np.concatenate([c, c], 0)
    sin64 = np.concatenate([-s_, s_], 0)
    return (np.ascontiguousarray(np.concatenate([cos64, cos64], 0)),
            np.ascontiguousarray(np.concatenate([sin64, sin64], 0)))


class GLA:
    def __init__(self, dn, es):
        self.dn, self.mk, self.sc, self.nc = dn, dn.mk, dn.sc, dn.nc
        mk, sc = self.mk, self.sc
        self.gmask = mk.sb(es, "gmask", [128, 512], F32)
        self.rmask = mk.sb(es, "rmask", [128, 128], F32)
        self.onesbd = mk.sb(es, "onesbd", [128, 128], BF16)
        tmpf = mk.sb(es, "g_tmpf", [128, 128], F32)
        sc.dma('sp', self.gmask[:], mk.din("c_gmask", [128, 512])[:, :], [], ['gmask'])
        sc.dma('sp', self.rmask[:], mk.din("c_rmask", [128, 128])[:, :], [], ['rmask'])
        sc.dma('sp', tmpf[:], mk.din("c_onesbd", [128, 128])[:, :], [], ['g_tmpf'])
        sc.op('dve', ['g_tmpf'], ['onesbd'], lambda e: e.tensor_copy(out=self.onesbd[:], in_=tmpf[:]))
        self.c_retdec = mk.din("c_retdec", [2, 3, 128, 128])
        self.c_retg32 = mk.din("c_retg32", [128, 2])
        self.c_cos = mk.din("c_cos", [128, mk.S])
        self.c_sin = mk.din("c_sin", [128, mk.S])

    def alloc_core(self, es):
        mk = self.mk
        b = {}
        b['PT'] = mk.sb(es, "gl_PT", [128, 512], BF16)
        b['stf'] = [mk.sb(es, "gl_stf%d" % hp, [128, 128], F32) for hp in range(2)]
        b['snap'] = [[mk.sb(es, "gl_snap%d_%d" % (par, hp), [128, 5, 128], BF16) for hp in range(2)] for par in range(2)]
        b['osb'] = mk.sb(es, "gl_osb", [128, 256], F32)
        for hp in range(2):
            self.sc.op('dve', [], [('stf', hp)], lambda e, hp=hp: e.memset(b['stf'][hp][:], 0.0))
            self.sc.op('pool', [], [('snap', 1, hp, 4)], lambda e, hp=hp: e.memset(b['snap'][1][hp][:, 4, :], 0.0))
        return b

    def core(self, b, ti, qs, ks, kd, kd3, v, dec, keys):
        sc, dn = self.sc, self.dn
        par = ti % 2
        pA = dn.next_ps()
        A = dn.ps[pA]
        for h in range(4):
            hp, hl = h // 2, h % 2
            sl = slice(hl * 64, (hl + 1) * 64)
            sc.op('pes', [keys['qs'][hp], keys['ks'][hp]], [('ps', pA)], lambda e, h=h, hp=hp, sl=sl: e.matmul(
                A[:, h * 128:(h + 1) * 128], lhsT=ks[hp][sl, :], rhs=qs[hp][sl, :], start=True, stop=True))
        sc.op('dve', [('ps', pA), 'gmask'], ['gl_PT'], lambda e: e.tensor_tensor(
            out=b['PT'][:], in0=A[:, :], in1=self.gmask[:], op=ALU.mult))
        pK = [dn.next_ps(), dn.next_ps()]
        for hp in range(2):
            for c in range(4):
                K = dn.ps[pK[hp]]
                rsl = slice(c * 32, (c + 1) * 32) if c < 3 else slice(64, 128)
                kk_ = kd if c < 3 else kd3
                sc.op('pes', [keys['kd'], keys['v']], [('ps', pK[hp])], lambda e, hp=hp, c=c, K=K, rsl=rsl, kk_=kk_: e.matmul(
                    K[:, c * 128:(c + 1) * 128], lhsT=kk_[rsl, hp * 128:(hp + 1) * 128],
                    rhs=v[rsl, hp * 128:(hp + 1) * 128], start=True, stop=True))
        for c in range(4):
            for hp in range(2):
                K = dn.ps[pK[hp]]
                sc.op('dve', [('ps', pK[hp]), ('stf', hp), keys['dec'][hp]], [('stf', hp)],
                      lambda e, hp=hp, c=c, K=K: e.scalar_tensor_tensor(
                          out=b['stf'][hp][:], in0=b['stf'][hp][:], scalar=dec(hp, c),
                          in1=K[:, c * 128:(c + 1) * 128], op0=ALU.mult, op1=ALU.add))
                sc.op('act', [('stf', hp)], [('snap', par, hp, c + 1)], lambda e, hp=hp, c=c: e.activation(
                    out=b['snap'][par][hp][:, c + 1, :], in_=b['stf'][hp][:], func=AF.Copy))
        pB = dn.next_ps()
        B = dn.ps[pB]
        for h in range(4):
            hp, hl = h // 2, h % 2
            sl = slice(hl * 64, (hl + 1) * 64)
            sc.op('pes', ['gl_PT', keys['v']], [('ps', pB)], lambda e, h=h, hp=hp, sl=sl: e.matmul(
                B[sl, hp * 128:(hp + 1) * 128], lhsT=v[:, h * 64:(h + 1) * 64], rhs=b['PT'][:, h * 128:(h + 1) * 128],
                start=True, stop=False))
            for c in range(4):
                if c == 0:
                    st = b['snap'][1 - par][hp][sl, 4, hl * 64:(hl + 1) * 64]
                    skey = ('snap', 1 - par, hp, 4)
                else:
                    st = b['snap'][par][hp][sl, c, hl * 64:(hl + 1) * 64]
                    skey = ('snap', par, hp, c)
                sc.op('pes', [skey, keys['qs'][hp]], [('ps', pB)], lambda e, hp=hp, sl=sl, c=c, st=st: e.matmul(
                    B[sl, hp * 128 + c * 32:hp * 128 + (c + 1) * 32], lhsT=st, rhs=qs[hp][sl, c * 32:(c + 1) * 32],
                    start=False, stop=(c == 3)))
        sc.op('act', [('ps', pB)], ['gl_osb'], lambda e: e.activation(out=b['osb'][:], in_=B[:, 0:256], func=AF.Copy))

    def run_hgrn(self, L, UC, UT, OT, lbz_src, hgn_src, es_ext):
        sc, mk, dn, S = self.sc, self.mk, self.dn, self.mk.S
        with _Keep(es_ext) as es:
            b = self.alloc_core(es)
            lbz = mk.sb(es, "hg_lbz", [128, DEPTH * 2], F32)
            lbe = mk.sb(es, "hg_lbe", [128, DEPTH * 2], F32)
            lbs = mk.sb(es, "hg_lbs", [128, 8], F32)
            gn = mk.sb(es, "hg_gn", [128, 2], F32)
            sc.dma('sp', lbz[:], lbz_src, [], ['lbz'])
            sc.dma('sp', gn[:], hgn_src, [], ['hg_gn'])
            sc.op('act', ['lbz'], ['lbe'], lambda e: e.activation(out=lbe[:], in_=lbz[:], func=AF.Exp))
            sc.op('dve', ['lbe'], ['lbs'], lambda e: e.tensor_tensor(
                out=lbs[:, 0:2], in0=lbe[:, 0:2], in1=lbe[:, 2:4], op=ALU.add))
            for l in range(2, DEPTH):
                sc.op('dve', ['lbe', 'lbs'], ['lbs'], lambda e, l=l: e.tensor_tensor(
                    out=lbs[:, 0:2], in0=lbs[:, 0:2], in1=lbe[:, 2 * l:2 * l + 2], op=ALU.add))
            sc.op('dve', ['lbs'], ['lbs'], lambda e: e.reciprocal(out=lbs[:, 2:4], in_=lbs[:, 0:2]))
            sc.op('dve', [], ['lbs'], lambda e: e.memset(lbs[:, 4:6], 0.0))
            for l in range(1, L + 1):
                sc.op('dve', ['lbs', 'lbe'], ['lbs'], lambda e, l=l: e.tensor_tensor(
                    out=lbs[:, 6:8], in0=lbe[:, 2 * l:2 * l + 2], in1=lbs[:, 2:4], op=ALU.mult))
                sc.op('dve', ['lbs'], ['lbs'], lambda e: e.tensor_tensor(
                    out=lbs[:, 4:6], in0=lbs[:, 4:6], in1=lbs[:, 6:8], op=ALU.add))
            sc.op('dve', ['lbs'], ['lbs'], lambda e: e.tensor_scalar(
                out=lbs[:, 6:8], in0=lbs[:, 4:6], scalar1=-1.0, scalar2=1.0, op0=ALU.mult, op1=ALU.add))
            TT = 512
            inq = [mk.sb(es, "hg_q%d" % hp, [128, TT], F32) for hp in range(2)]
            inz = [mk.sb(es, "hg_z%d" % hp, [128, TT], F32) for hp in range(2)]
            ing = [mk.sb(es, "hg_g%d" % hp, [128, TT], F32) for hp in range(2)]
            vin = mk.sb(es, "hg_vin", [128, 4, 256], F32)
            vb = mk.sb(es, "hg_vb", [128, 256], BF16)
            fT = mk.sb(es, "hg_fT", [128, 128], F32)
            kT = mk.sb(es, "hg_kT", [128, 128], F32)
            lf = mk.sb(es, "hg_lf", [128, 128], F32)
            bT = mk.sb(es, "hg_bT", [128, 128], F32)
            eb = [mk.sb(es, "hg_eb%d" % hp, [128, 128], F32) for hp in range(2)]
            enb = mk.sb(es, "hg_enb", [128, 128], F32)
            sq = mk.sb(es, "hg_sq", [128, 128], F32)
            ksf = mk.sb(es, "hg_ksf", [128, 128], F32)
            qs = [mk.sb(es, "hg_qs%d" % hp, [128, 128], BF16) for hp in range(2)]
            ks = [mk.sb(es, "hg_ks%d" % hp, [128, 128], BF16) for hp in range(2)]
            kdT = mk.sb(es, "hg_kdT", [128, 128], BF16)
            kd = mk.sb(es, "hg_kd", [128, 256], BF16)
            kd3 = mk.sb(es, "hg_kd3", [128, 256], BF16)
            sc.op('pool', [], ['kd'], lambda e: e.memset(kd3[:], 0.0))
            osq = mk.sb(es, "hg_osq", [128, 256], BF16)
            rs = mk.sb(es, "hg_rs", [128, 256], F32)
            sg = mk.sb(es, "hg_sg", [128, 256], F32)
            ob = mk.sb(es, "hg_ob", [128, 256], BF16)
            pbk = dn.pb[1]
            for t0 in range(0, S, TT):
                for hp in range(2):
                    sc.dma('sp', inq[hp][:], UC[GIDX['hg_q%d' % hp], :, t0:t0 + TT], [], [('inq', hp)])
                    sc.dma('sp', inz[hp][:], UC[GIDX['hg_f%d' % hp], :, t0:t0 + TT], [], [('inz', hp)])
                    sc.dma('sp', ing[hp][:], UC[GIDX['hg_og%d' % hp], :, t0:t0 + TT], [], [('ing', hp)])
                sc.dma('sp', vin[:], UT[t0:t0 + TT, 128:384].rearrange("(n p) c -> p n c", p=128), [], ['vin'])
                for tl in range(TT // 128):
                    ti = t0 // 128 + tl
                    ts = slice(tl * 128, (tl + 1) * 128)
                    sc.op('pool', ['vin'], ['vb'], lambda e, tl=tl: e.tensor_copy(out=vb[:], in_=vin[:, tl, :]))
                    for hp in range(2):
                        sc.op('act', [('inz', hp)], ['fT'], lambda e, hp=hp, ts=ts: e.activation(
                            out=fT[:], in_=inz[hp][:, ts], func=AF.Sigmoid))
                        sc.op('dve', ['fT', 'lbs'], ['fT'], lambda e, hp=hp: e.tensor_scalar(
                            out=fT[:], in0=fT[:], scalar1=lbs[:, 6 + hp:7 + hp], scalar2=lbs[:, 4 + hp:5 + hp],
                            op0=ALU.mult, op1=ALU.add))
                        sc.op('pool', ['fT'], ['kT'], lambda e: e.tensor_scalar(
                            out=kT[:], in0=fT[:], scalar1=-1.0, scalar2=1.0, op0=ALU.mult, op1=ALU.add))
                        sc.op('dve', ['fT'], ['lf'], lambda e: e.tensor_scalar(
                            out=lf[:], in0=fT[:], scalar1=1e-20, scalar2=None, op0=ALU.max))
                        sc.op('act', ['lf'], ['lf'], lambda e: e.activation(out=lf[:], in_=lf[:], func=AF.Ln))
                        sc.op('dve', ['lf', 'rmask'], ['bT'], lambda e: e.tensor_tensor_scan(
                            out=bT[:], data0=self.rmask[:], data1=lf[:], initial=0.0, op0=ALU.mult, op1=ALU.add))
                        sc.op('act', ['bT'], [('eb', hp)], lambda e, hp=hp: e.activation(out=eb[hp][:], in_=bT[:], func=AF.Exp))
                        sc.op('act', ['bT'], ['enb'], lambda e: e.activation(out=enb[:], in_=bT[:], func=AF.Exp, scale=-1.0))
                        sc.op('act', [('inq', hp)], ['sq'], lambda e, hp=hp, ts=ts: e.activation(
                            out=sq[:], in_=inq[hp][:, ts], func=AF.Silu))
                        sc.op('dve', ['sq', ('eb', hp)], [('qs', hp)], lambda e, hp=hp: e.tensor_tensor(
                            out=qs[hp][:], in0=sq[:], in1=eb[hp][:], op=ALU.mult))
                        sc.op('dve', ['kT', 'enb'], ['ksf'], lambda e: e.tensor_tensor(
                            out=ksf[:], in0=kT[:], in1=enb[:], op=ALU.mult))
                        sc.op('pool', ['ksf'], [('ks', hp)], lambda e, hp=hp: e.tensor_copy(out=ks[hp][:], in_=ksf[:]))
                        sc.op('dve', ['ksf', ('eb', hp)], ['kdT'], lambda e, hp=hp: e.tensor_tensor(
                            out=kdT[:].rearrange("p (c i) -> p c i", i=32),
                            in0=ksf[:].rearrange("p (c i) -> p c i", i=32),
                            in1=eb[hp][:].rearrange("p (c i) -> p c i", i=32)[:, :, 31:32].broadcast_to((128, 4, 32)),
                            op=ALU.mult))
                        sc.op('pe', ['kdT', 'ident'], ['pb1'], lambda e: e.transpose(
                            out=pbk[:, 0:128], in_=kdT[:], identity=dn.ident[:]))
                        sc.op('act', ['pb1'], ['kd'], lambda e, hp=hp: e.activation(
                            out=kd[:, hp * 128:(hp + 1) * 128], in_=pbk[:, 0:128], func=AF.Copy))
                        sc.op('dve', ['pb1'], ['kd'], lambda e, hp=hp: e.tensor_copy(
                            out=kd3[96:128, hp * 128:(hp + 1) * 128], in_=pbk[96:128, 0:128]))
                    yield
                    self.core(b, ti, [q[:] for q in qs], [k[:] for k in ks], kd[:], kd3[:], vb[:],
                              lambda hp, c: eb[hp][:, c * 32 + 31:c * 32 + 32],
                              {'qs': [('qs', 0), ('qs', 1)], 'ks': [('ks', 0), ('ks', 1)], 'kd': 'kd', 'v': 'vb', 'dec': [('eb', 0), ('eb', 1)]})
                    yield
                    osb = b['osb']
                    sc.op('act', ['gl_osb'], ['osq'], lambda e: e.activation(out=osq[:], in_=osb[:], func=AF.Square))
                    pS = dn.next_ps()
                    sc.op('pe', ['osq', 'onesbd'], [('ps', pS)], lambda e, pS=pS: e.matmul(
                        dn.ps[pS][:, 0:256], lhsT=self.onesbd[:], rhs=osq[:], start=True, stop=True))
                    sc.op('dve', [('ps', pS)], ['rs'], lambda e, pS=pS: e.tensor_scalar(
                        out=rs[:], in0=dn.ps[pS][:, 0:256], scalar1=1.0 / 64, scalar2=EPS, op0=ALU.mult, op1=ALU.add))
                    sc.op('act', ['rs'], ['rs'], lambda e: e.activation(out=rs[:], in_=rs[:], func=AF.Sqrt))
                    sc.op('dve', ['rs'], ['rs'], lambda e: e.reciprocal(out=rs[:], in_=rs[:]))
                    for hp in range(2):
                        sc.op('act', [('ing', hp)], ['sg'], lambda e, hp=hp, ts=ts: e.activation(
                            out=sg[:, hp * 128:(hp + 1) * 128], in_=ing[hp][:, ts], func=AF.Silu))
                        sc.op('dve', ['rs', 'gl_osb', 'hg_gn'], ['rs'], lambda e, hp=hp: e.scalar_tensor_tensor(
                            out=rs[:, hp * 128:(hp + 1) * 128], in0=rs[:, hp * 128:(hp + 1) * 128],
                            scalar=gn[:, hp:hp + 1], in1=osb[:, hp * 128:(hp + 1) * 128], op0=ALU.mult, op1=ALU.mult))
                    sc.op('dve', ['rs', 'sg'], ['ob'], lambda e: e.tensor_tensor(out=ob[:], in0=rs[:], in1=sg[:], op=ALU.mult))
                    tg = t0 + tl * 128
                    sc.dma(STORE_Q, OT[1, :, :, tg:tg + 128].rearrange("c p t -> p c t"),
                           ob[:].rearrange("p (c t) -> p c t", c=2), ['ob'], [])
                    yield


def _run_ret(self, L, UC, UT, OT, gb_src, es_ext):
    sc, mk, dn, S = self.sc, self.mk, self.dn, self.mk.S
    with _Keep(es_ext) as es:
        b = self.alloc_core(es)
        gb = mk.sb(es, "rt_gb", [128, 4], F32)
        g32 = mk.sb(es, "rt_g32", [128, 2], F32)
        cdec = mk.sb(es, "rt_cdec", [128, 6, 128], F32)
        sc.dma('sp', gb[:], gb_src, [], ['rt_gb'])
        sc.dma('sp', g32[:], self.c_retg32[:, :], [], ['rt_g32'])
        sc.dma('sp', cdec[:], self.c_retdec.rearrange("a b p t -> p (a b) t"), [], ['rt_cdec'])
        TT = 512
        names = ['q', 'qr', 'k', 'kr', 'g']
        inb = {n: [mk.sb(es, "rt_%s%d" % (n, hp), [128, TT], F32) for hp in range(2)] for n in names}
        cs = mk.sb(es, "rt_cos", [128, TT], F32)
        sn = mk.sb(es, "rt_sin", [128, TT], F32)
        vin = mk.sb(es, "rt_vin", [128, 4, 256], F32)
        vb = mk.sb(es, "rt_vb", [128, 256], BF16)
        t1 = mk.sb(es, "rt_t1", [128, 128], F32)
        t2 = mk.sb(es, "rt_t2", [128, 128], F32)
        qro = mk.sb(es, "rt_qro", [128, 128], F32)
        kro = mk.sb(es, "rt_kro", [128, 128], F32)
        qs = [mk.sb(es, "rt_qs%d" % hp, [128, 128], BF16) for hp in range(2)]
        ks = [mk.sb(es, "rt_ks%d" % hp, [128, 128], BF16) for hp in range(2)]
        kdT = mk.sb(es, "rt_kdT", [128, 128], BF16)
        kd = mk.sb(es, "rt_kd", [128, 256], BF16)
        kd3 = mk.sb(es, "rt_kd3", [128, 256], BF16)
        sc.op('pool', [], ['kd'], lambda e: e.memset(kd3[:], 0.0))
        o16 = mk.sb(es, "rt_o16", [128, 256], BF16)
        cen = mk.sb(es, "rt_cen", [128, 256], F32)
        sq = mk.sb(es, "rt_sq", [128, 256], BF16)
        rs = mk.sb(es, "rt_rs", [128, 256], F32)
        sg = mk.sb(es, "rt_sg", [128, 256], F32)
        ob = mk.sb(es, "rt_ob", [128, 256], BF16)
        pbk = dn.pb[1]
        for t0 in range(0, S, TT):
            for hp in range(2):
                for n in names:
                    sc.dma('sp', inb[n][hp][:], UC[GIDX['ret_%s%d' % (n, hp)], :, t0:t0 + TT], [], [('rin', n, hp)])
            sc.dma('sp', cs[:], self.c_cos[:, t0:t0 + TT], [], ['rt_cos'])
            sc.dma('sp', sn[:], self.c_sin[:, t0:t0 + TT], [], ['rt_sin'])
            sc.dma('sp', vin[:], UT[t0:t0 + TT, 384:640].rearrange("(n p) c -> p n c", p=128), [], ['vin'])
            for tl in range(TT // 128):
                ti = t0 // 128 + tl
                ts = slice(tl * 128, (tl + 1) * 128)
                sc.op('pool', ['vin'], ['vb'], lambda e, tl=tl: e.tensor_copy(out=vb[:], in_=vin[:, tl, :]))
                for hp in range(2):
                    for (a, ar, dst, dkey) in (('q', 'qr', qro, 'qro'), ('k', 'kr', kro, 'kro')):
                        sc.op('dve', [('rin', a, hp), 'rt_cos'], ['t1'], lambda e, a=a, hp=hp, ts=ts: e.tensor_tensor(
                            out=t1[:], in0=inb[a][hp][:, ts], in1=cs[:, ts], op=ALU.mult))
                        sc.op('pool', [('rin', ar, hp), 'rt_sin'], ['t2'], lambda e, ar=ar, hp=hp, ts=ts: e.tensor_tensor(
                            out=t2[:], in0=inb[ar][hp][:, ts], in1=sn[:, ts], op=ALU.mult))
                        sc.op('dve', ['t1', 't2'], [dkey], lambda e, dst=dst: e.tensor_tensor(
                            out=dst[:], in0=t1[:], in1=t2[:], op=ALU.add))
                    sc.op('dve', ['qro', 'rt_cdec'], [('qs', hp)], lambda e, hp=hp: e.tensor_tensor(
                        out=qs[hp][:], in0=qro[:], in1=cdec[:, hp * 3 + 0, :], op=ALU.mult))
                    sc.op('pool', ['kro', 'rt_cdec'], [('ks', hp)], lambda e, hp=hp: e.tensor_tensor(
                        out=ks[hp][:], in0=kro[:], in1=cdec[:, hp * 3 + 1, :], op=ALU.mult))
                    sc.op('dve', ['kro', 'rt_cdec'], ['kdT'], lambda e, hp=hp: e.tensor_tensor(
                        out=kdT[:], in0=kro[:], in1=cdec[:, hp * 3 + 2, :], op=ALU.mult))
                    sc.op('pe', ['kdT', 'ident'], ['pb1'], lambda e: e.transpose(
                        out=pbk[:, 0:128], in_=kdT[:], identity=dn.ident[:]))
                    sc.op('act', ['pb1'], ['kd'], lambda e, hp=hp: e.activation(
                        out=kd[:, hp * 128:(hp + 1) * 128], in_=pbk[:, 0:128], func=AF.Copy))
                    sc.op('dve', ['pb1'], ['kd'], lambda e, hp=hp: e.tensor_copy(
                        out=kd3[96:128, hp * 128:(hp + 1) * 128], in_=pbk[96:128, 0:128]))
                yield
                self.core(b, ti, [q[:] for q in qs], [k[:] for k in ks], kd[:], kd3[:], vb[:],
                          lambda hp, c: g32[:, hp:hp + 1],
                          {'qs': [('qs', 0), ('qs', 1)], 'ks': [('ks', 0), ('ks', 1)], 'kd': 'kd', 'v': 'vb',
                           'dec': ['rt_g32', 'rt_g32']})
                yield
                osb = b['osb']
                sc.op('pool', ['gl_osb'], ['o16'], lambda e: e.tensor_copy(out=o16[:], in_=osb[:]))
                pM = dn.next_ps()
                sc.op('pe', ['o16', 'onesbd'], [('ps', pM)], lambda e, pM=pM: e.matmul(
                    dn.ps[pM][:, 0:256], lhsT=self.onesbd[:], rhs=o16[:], start=True, stop=True))
                sc.op('dve', [('ps', pM), 'gl_osb'], ['cen'], lambda e, pM=pM: e.scalar_tensor_tensor(
                    out=cen[:], in0=dn.ps[pM][:, 0:256], scalar=-1.0 / 64, in1=osb[:], op0=ALU.mult, op1=ALU.add))
                sc.op('act', ['cen'], ['sq'], lambda e: e.activation(out=sq[:], in_=cen[:], func=AF.Square))
                pV = dn.next_ps()
                sc.op('pe', ['sq', 'onesbd'], [('ps', pV)], lambda e, pV=pV: e.matmul(
                    dn.ps[pV][:, 0:256], lhsT=self.onesbd[:], rhs=sq[:], start=True, stop=True))
                sc.op('dve', [('ps', pV)], ['rs'], lambda e, pV=pV: e.tensor_scalar(
                    out=rs[:], in0=dn.ps[pV][:, 0:256], scalar1=1.0 / 64, scalar2=1e-5, op0=ALU.mult, op1=ALU.add))
                sc.op('act', ['rs'], ['rs'], lambda e: e.activation(out=rs[:], in_=rs[:], func=AF.Sqrt))
                sc.op('dve', ['rs'], ['rs'], lambda e: e.reciprocal(out=rs[:], in_=rs[:]))
                sc.op('dve', ['rs', 'cen'], ['cen'], lambda e: e.tensor_tensor(out=cen[:], in0=cen[:], in1=rs[:], op=ALU.mult))
                for hp in range(2):
                    sc.op('act', [('rin', 'g', hp)], ['sg'], lambda e, hp=hp, ts=ts: e.activation(
                        out=sg[:, hp * 128:(hp + 1) * 128], in_=inb['g'][hp][:, ts], func=AF.Silu))
                    sc.op('dve', ['cen', 'rt_gb'], ['cen'], lambda e, hp=hp: e.tensor_scalar(
                        out=cen[:, hp * 128:(hp + 1) * 128], in0=cen[:, hp * 128:(hp + 1) * 128],
                        scalar1=gb[:, hp:hp + 1], scalar2=gb[:, 2 + hp:3 + hp], op0=ALU.mult, op1=ALU.add))
                sc.op('dve', ['cen', 'sg'], ['ob'], lambda e: e.tensor_tensor(out=ob[:], in0=cen[:], in1=sg[:], op=ALU.mult))
                tg = t0 + tl * 128
                sc.dma(STORE_Q, OT[2, :, :, tg:tg + 128].rearrange("c p t -> p c t"),
                       ob[:].rearrange("p (c t) -> p c t", c=2), ['ob'], [])
                yield


GLA.run_ret = _run_ret


RW_C = 64


def rwkv_consts():
    i = np.arange(128)
    same = (i[:, None] // 64) == (i[None, :] // 64)
    su = (same & (i[:, None] % 64 < i[None, :] % 64)).astype(np.float32)
    iu = (same & (i[:, None] % 64 <= i[None, :] % 64)).astype(np.float32)
    d = {"c_rwmask4": np.ascontiguousarray(np.concatenate([su, su, iu, iu], axis=1)),
         "c_rwlmask": np.ascontiguousarray(su.T)}
    rm = np.ones((128, 256), np.float32)
    rm[:, ::64] = 0.0
    d["c_rmask64"] = rm
    return d


class RWKV:
    def __init__(self, dn, gla, es):
        self.dn, self.mk, self.sc, self.gla = dn, dn.mk, dn.sc, gla
        mk, sc = self.mk, self.sc
        self.mask4 = mk.sb(es, "rw_mask4", [128, 512], F32)
        self.lmask = mk.sb(es, "rw_lmask", [128, 128], F32)
        self.rmask = mk.sb(es, "rw_rmask", [128, 256], F32)
        sc.dma('sp', self.mask4[:], mk.din("c_rwmask4", [128, 512])[:, :], [], ['rw_mask4'])
        sc.dma('sp', self.lmask[:], mk.din("c_rwlmask", [128, 128])[:, :], [], ['rw_lmask'])
        sc.dma('sp', self.rmask[:], mk.din("c_rmask64", [128, 256])[:, :], [], ['rw_rmask'])

    def run(self, L, UC, OT, ws, es_ext, banks=(5, 6)):
        sc, mk, dn, S = self.sc, self.mk, self.dn, self.mk.S
        ident = dn.ident
        onesbd = self.gla.onesbd
        bi = [0]

        def nps():
            b_ = banks[bi[0] % len(banks)]
            bi[0] += 1
            return b_
        with _Keep(es_ext) as es:
            TT = min(256, S)
            NCH = TT // 64
            wa_f = mk.sb(es, "rw_waf", [128, 256], F32)
            WA = mk.sb(es, "rw_WA", [128, 256], BF16)
            gu_f = mk.sb(es, "rw_guf", [128, 256], F32)
            GU = mk.sb(es, "rw_GU", [128, 256], BF16)
            sc.dma('sp', wa_f[0:64, :], ws['w_up'], [], ['rw_waf'])
            sc.dma('sp', wa_f[64:128, :], ws['a_up'], [], ['rw_waf'])
            sc.dma('sp', gu_f[:], ws['g_up'], [], ['rw_guf'])
            sc.op('dve', ['rw_waf'], ['rw_WA'], lambda e: e.tensor_copy(out=WA[:], in_=wa_f[:]))
            sc.op('dve', ['rw_guf'], ['rw_GU'], lambda e: e.tensor_copy(out=GU[:], in_=gu_f[:]))
            pv = mk.sb(es, "rw_pv", [128, 24], F32)
            sc.dma('sp', pv[:], ws['pvec'], [], ['rw_pv'])
            sc.op('dve', ['rw_pv'], ['rw_pv'], lambda e: e.tensor_scalar(
                out=pv[:, 22:24], in0=pv[:, 14:16], scalar1=-1.0, scalar2=1.0, op0=ALU.mult, op1=ALU.add))
            raw = [mk.sb(es, "rw_raw%d" % g, [128, TT + 1], F32) for g in range(8)]
            sh = [mk.sb(es, "rw_sh%d" % g, [128, TT], F32) for g in range(8)]
            dtmp = mk.sb(es, "rw_dtmp", [128, TT], F32)
            twa = mk.sb(es, "rw_twa", [128, TT], BF16)
            sgl = mk.sb(es, "rw_sgl", [128, TT], BF16)
            F = lambda n: mk.sb(es, "rw_" + n, [128, TT], F32)
            lw, av, kk, rn, kkn, kp, lG, enG, eGp, t1 = (F(n) for n in
                ('lw', 'av', 'kk', 'rn', 'kkn', 'kp', 'lG', 'enG', 'eGp', 't1'))
            As_f, Ks_f = F('Asf'), F('Ksf')
            sq16 = mk.sb(es, "rw_sq16", [128, TT], BF16)
            y16 = mk.sb(es, "rw_y16", [128, TT], BF16)
            cen = mk.sb(es, "rw_cen", [128, TT], F32)
            rs = mk.sb(es, "rw_rs", [128, TT], F32)
            ob = mk.sb(es, "rw_ob", [128, TT], BF16)
            H = []
            for hp in range(2):
                hb = {}
                for n in ('As', 'Ks', 'Bt', 'Rt', 'Ah', 'Kh'):
                    hb['bd_' + n] = mk.sb(es, "rw_bd_%s%d" % (n, hp), [128, NCH, 128], BF16)
                    sc.op('pool', [], [('bd', n, hp)], lambda e, tl=hb['bd_' + n]: e.memset(tl[:], 0.0))
                for n in ('Bt', 'Rt', 'As', 'v'):
                    hb['r2_' + n] = mk.sb(es, "rw_r2_%s%d" % (n, hp), [128, NCH, 2, 64], BF16)
                hb['eG'] = mk.sb(es, "rw_eG%d" % hp, [128, TT], F32)
                hb['gT'] = mk.sb(es, "rw_gT%d" % hp, [128, TT], F32)
                hb['rkk16'] = mk.sb(es, "rw_rkk%d" % hp, [128, TT], BF16)
                hb['Tf'] = mk.sb(es, "rw_Tf%d" % hp, [128, 128], F32)
                hb['Tb'] = mk.sb(es, "rw_Tb%d" % hp, [128, 128], BF16)
                hb['PQ'] = [mk.sb(es, "rw_PQ%d_%d" % (hp, i), [128, 256], BF16) for i in range(2)]
                hb['NM'] = [mk.sb(es, "rw_NM%d_%d" % (hp, i), [128, 384], BF16) for i in range(2)]
                hb['Z'] = [[mk.sb(es, "rw_Z%d_%d_%d" % (hp, i, j), [128, 128], BF16) for j in range(2)] for i in range(2)]
                hb['Vtok'] = [mk.sb(es, "rw_Vtok%d_%d" % (hp, i), [128, 128], BF16) for i in range(2)]
                hb['AKtok'] = [mk.sb(es, "rw_AKtok%d_%d" % (hp, i), [128, 256], BF16) for i in range(2)]
                hb['Wsb'] = mk.sb(es, "rw_Wsb%d" % hp, [128, 128], BF16)
                hb['Usb'] = mk.sb(es, "rw_Usb%d" % hp, [128, 128], BF16)
                hb['yT'] = mk.sb(es, "rw_yT%d" % hp, [128, TT], F32)
                sc.op('dve', [], [('Tf', hp)], lambda e, hb=hb: e.memset(hb['Tf'][:], 0.0))
                sc.op('pool', [], [('Tb', hp)], lambda e, hb=hb: e.memset(hb['Tb'][:], 0.0))
                H.append(hb)
            pbk = dn.pb[1]
            c3 = lambda ap: ap.rearrange("p (c t) -> p c t", t=64)
            r4 = lambda ap: ap.rearrange("p (c t) -> p c t", t=64).unsqueeze(2).broadcast_to((128, NCH, 2, 64))

            def prep_hp(hp):
                hb = H[hp]
                hs = slice(hp * 128, (hp + 1) * 128)
                shr, shk, shv = sh[hp], sh[2 + hp], sh[4 + hp]
                kr, kk_, kv = ('sh', hp), ('sh', 2 + hp), ('sh', 4 + hp)
                eG, gT = hb['eG'], hb['gT']
                p1 = nps()
                sc.op('pes', ['rw_WA', 'twa'], [('ps', p1)], lambda e: e.matmul(
                    dn.ps[p1][:, 0:TT], lhsT=WA[0:64, hs], rhs=twa[0:64, :], start=True, stop=True))
                sc.op('act', [('ps', p1), 'rw_pv'], ['lw'], lambda e: e.activation(
                    out=lw[:], in_=dn.ps[p1][:, 0:TT], func=AF.Sigmoid, bias=pv[:, 8 + hp:9 + hp], scale=1.0))
                sc.op('pool', ['lw'], ['lw'], lambda e: e.tensor_scalar(
                    out=lw[:], in0=lw[:], scalar1=-0.6065306597126334, scalar2=None, op0=ALU.mult))
                p2 = nps()
                sc.op('pes', ['rw_WA', 'twa'], [('ps', p2)], lambda e: e.matmul(
                    dn.ps[p2][:, 0:TT], lhsT=WA[64:128, hs], rhs=twa[64:128, :], start=True, stop=True))
                sc.op('act', [('ps', p2), 'rw_pv'], ['av'], lambda e: e.activation(
                    out=av[:], in_=dn.ps[p2][:, 0:TT], func=AF.Sigmoid, bias=pv[:, 10 + hp:11 + hp], scale=1.0))
                p3 = nps()
                sc.op('pe', ['rw_GU', 'sgl'], [('ps', p3)], lambda e: e.matmul(
                    dn.ps[p3][:, 0:TT], lhsT=GU[:, hs], rhs=sgl[:], start=True, stop=True))
                sc.op('act', [('ps', p3)], [('gT', hp)], lambda e: e.activation(
                    out=gT[:], in_=dn.ps[p3][:, 0:TT], func=AF.Copy))
                sc.op('dve', [kk_, 'rw_pv'], ['kk'], lambda e: e.tensor_scalar(
                    out=kk[:], in0=shk[:], scalar1=pv[:, 12 + hp:13 + hp], scalar2=None, op0=ALU.mult))
                sc.op('act', ['kk'], ['sq16'], lambda e: e.activation(out=sq16[:], in_=kk[:], func=AF.Square))
                p4 = nps()
                sc.op('pe', ['sq16', 'onesbd'], [('ps', p4)], lambda e: e.matmul(
                    dn.ps[p4][:, 0:TT], lhsT=onesbd[:], rhs=sq16[:], start=True, stop=True))
                sc.op('dve', [('ps', p4)], ['rn'], lambda e: e.tensor_scalar(
                    out=rn[:], in0=dn.ps[p4][:, 0:TT], scalar1=1e-24, scalar2=None, op0=ALU.max))
                sc.op('act', ['rn'], ['rn'], lambda e: e.activation(out=rn[:], in_=rn[:], func=AF.Sqrt))
                sc.op('dve', ['rn'], ['rn'], lambda e: e.reciprocal(out=rn[:], in_=rn[:]))
                sc.op('dve', ['kk', 'rn'], ['kkn'], lambda e: e.tensor_tensor(out=kkn[:], in0=kk[:], in1=rn[:], op=ALU.mult))
                sc.op('pool', ['av', 'rw_pv'], ['t1'], lambda e: e.tensor_scalar(
                    out=t1[:], in0=av[:], scalar1=pv[:, 14 + hp:15 + hp], scalar2=pv[:, 22 + hp:23 + hp],
                    op0=ALU.mult, op1=ALU.add))
                sc.op('dve', ['t1', kk_], ['kp'], lambda e: e.tensor_tensor(out=kp[:], in0=shk[:], in1=t1[:], op=ALU.mult))
                sc.op('dve', ['lw', 'rw_rmask'], ['lG'], lambda e: e.tensor_tensor_scan(
                    out=lG[:], data0=self.rmask[:, 0:TT], data1=lw[:], initial=0.0, op0=ALU.mult, op1=ALU.add))
                sc.op('act', ['lG'], [('eG', hp)], lambda e: e.activation(out=eG[:], in_=lG[:], func=AF.Exp))
                sc.op('act', ['lG'], ['enG'], lambda e: e.activation(out=enG[:], in_=lG[:], func=AF.Exp, scale=-1.0))
                sc.op('pool', ['lG', 'lw'], ['t1'], lambda e: e.tensor_tensor(out=t1[:], in0=lG[:], in1=lw[:], op=ALU.subtract))
                sc.op('act', ['t1'], ['eGp'], lambda e: e.activation(out=eGp[:], in_=t1[:], func=AF.Exp))
                sc.op('dve', ['kkn', 'av'], ['t1'], lambda e: e.tensor_tensor(out=t1[:], in0=kkn[:], in1=av[:], op=ALU.mult))
                sc.op('dve', ['t1', 'enG'], ['Asf'], lambda e: e.tensor_tensor(out=As_f[:], in0=t1[:], in1=enG[:], op=ALU.mult))
                sc.op('pool', ['kp', 'enG'], ['Ksf'], lambda e: e.tensor_tensor(out=Ks_f[:], in0=kp[:], in1=enG[:], op=ALU.mult))
                sc.op('dve', ['kkn', 'eGp'], ['eGp'], lambda e: e.scalar_tensor_tensor(
                    out=eGp[:], in0=kkn[:], scalar=-1.0, in1=eGp[:], op0=ALU.mult, op1=ALU.mult))
                sc.op('pool', [kr, ('eG', hp)], ['t1'], lambda e: e.tensor_tensor(out=t1[:], in0=shr[:], in1=eG[:], op=ALU.mult))
                for half in range(2):
                    psl = slice(half * 64, (half + 1) * 64)
                    csl = slice(half * 64, (half + 1) * 64)
                    eng = 'dve' if half == 0 else 'pool'
                    for (nm, src, skey) in (('As', As_f, 'Asf'), ('Ks', Ks_f, 'Ksf'), ('Bt', eGp, 'eGp'), ('Rt', t1, 't1')):
                        sc.op(eng, [skey], [('bd', nm, hp)], lambda e, nm=nm, src=src, psl=psl, csl=csl: e.tensor_copy(
                            out=hb['bd_' + nm][psl, :, csl], in_=c3(src[psl, :])))
                    for (nm, src, skey) in (('Ah', As_f, 'Asf'), ('Kh', Ks_f, 'Ksf')):
                        sc.op('dve', [skey, ('eG', hp)], [('bd', nm, hp)], lambda e, nm=nm, src=src, psl=psl, csl=csl: e.tensor_tensor(
                            out=hb['bd_' + nm][psl, :, csl], in0=c3(src[psl, :]),
                            in1=c3(eG[psl, :])[:, :, 63:64].broadcast_to((64, NCH, 64)), op=ALU.mult))
                sc.op('pool', ['eGp'], [('r2', 'Bt', hp)], lambda e: e.tensor_copy(out=hb['r2_Bt'][:], in_=r4(eGp[:])))
                sc.op('pool', ['t1'], [('r2', 'Rt', hp)], lambda e: e.tensor_copy(out=hb['r2_Rt'][:], in_=r4(t1[:])))
                sc.op('dve', ['Asf'], [('r2', 'As', hp)], lambda e: e.tensor_copy(out=hb['r2_As'][:], in_=r4(As_f[:])))
                sc.op('pool', [kv], [('r2', 'v', hp)], lambda e: e.tensor_copy(out=hb['r2_v'][:], in_=r4(shv[:])))
                sc.op('dve', [kr, 'kp', 'rw_pv'], [('rkk16', hp)], lambda e: e.scalar_tensor_tensor(
                    out=hb['rkk16'][:], in0=shr[:], scalar=pv[:, 16 + hp:17 + hp], in1=kp[:], op0=ALU.mult, op1=ALU.mult))

            def chunk(hp, c, par):
                hb = H[hp]
                PQ, NM, Z, Vtok, AKtok, Wsb, Usb, Tf, Tb, yT, eG = (hb[k] for k in
                    ('PQ', 'NM', 'Z', 'Vtok', 'AKtok', 'Wsb', 'Usb', 'Tf', 'Tb', 'yT', 'eG'))
                f2 = lambda t: t[:, c, :, :].rearrange("p a t -> p (a t)")
                K_ = lambda *a: a + (hp,)
                pX = nps()
                X = dn.ps[pX]
                for j, (lt, rt) in enumerate((('As', 'Bt'), ('Ks', 'Bt'), ('As', 'Rt'), ('Ks', 'Rt'))):
                    sc.op('pe', [('bd', lt, hp), ('r2', rt, hp)], [('ps', pX)], lambda e, j=j, lt=lt, rt=rt: e.matmul(
                        X[:, j * 128:(j + 1) * 128], lhsT=hb['bd_' + lt][:, c, :], rhs=f2(hb['r2_' + rt]), start=True, stop=True))
                pQ = nps()
                sc.op('pe', [('bd', 'Bt', hp), ('r2', 'As', hp)], [('ps', pQ)], lambda e: e.matmul(
                    dn.ps[pQ][:, 0:128], lhsT=hb['bd_Bt'][:, c, :], rhs=f2(hb['r2_As']), start=True, stop=True))
                sc.op('dve', [('ps', pX), 'rw_mask4'], [K_('PQ', 0)], lambda e: e.tensor_tensor(
                    out=PQ[0][:, 0:128], in0=X[:, 0:128], in1=self.mask4[:, 0:128], op=ALU.mult))
                sc.op('dve', [('ps', pX), 'rw_mask4'], [K_('NM', par)], lambda e: e.tensor_tensor(
                    out=NM[par][:], in0=X[:, 128:512], in1=self.mask4[:, 128:512], op=ALU.mult))
                sc.op('dve', [('ps', pQ), 'rw_lmask'], [K_('PQ', 0)], lambda e: e.tensor_tensor(
                    out=PQ[0][:, 128:256], in0=dn.ps[pQ][:, 0:128], in1=self.lmask[:], op=ALU.mult))
                zi = 0
                sc.op('pool', [K_('PQ', 0), 'ident'], [K_('Z', par, zi)], lambda e: e.tensor_tensor(
                    out=Z[par][0][:], in0=PQ[0][:, 0:128], in1=ident[:], op=ALU.add))
                yield
                cur = 0
                for lvl in range(5):
                    last = (lvl == 4)
                    nxt = 1 - cur
                    pP = nps()
                    Pp = dn.ps[pP]
                    if not last:
                        sc.op('pe', [K_('PQ', cur)], [('ps', pP)], lambda e, cur=cur, Pp=Pp: e.matmul(
                            Pp[:, 0:128], lhsT=PQ[cur][:, 128:256], rhs=PQ[cur][:, 0:128], start=True, stop=True))
                    sc.op('pe', [K_('PQ', cur)], [('ps', pP)], lambda e, cur=cur, Pp=Pp: e.matmul(
                        Pp[:, 128:256], lhsT=PQ[cur][:, 0:128], rhs=PQ[cur][:, 128:256], start=True, stop=True))
                    if not last:
                        sc.op('act', [('ps', pP)], [K_('PQ', nxt)], lambda e, nxt=nxt, Pp=Pp: e.activation(
                            out=PQ[nxt][:], in_=Pp[:, 0:256], func=AF.Copy))
                    else:
                        sc.op('act', [('ps', pP)], [K_('PQ', nxt)], lambda e, nxt=nxt, Pp=Pp: e.activation(
                            out=PQ[nxt][:, 128:256], in_=Pp[:, 128:256], func=AF.Copy))
                    yield
                    pZ = nps()
                    sc.op('pe', [K_('PQ', nxt), K_('Z', par, zi)], [('ps', pZ)], lambda e, nxt=nxt, pZ=pZ, zi=zi: e.matmul(
                        dn.ps[pZ][:, 0:128], lhsT=PQ[nxt][:, 128:256], rhs=Z[par][zi][:], start=True, stop=True))
                    sc.op('dve', [('ps', pZ), K_('Z', par, zi)], [K_('Z', par, 1 - zi)], lambda e, pZ=pZ, zi=zi: e.tensor_tensor(
                        out=Z[par][1 - zi][:], in0=dn.ps[pZ][:, 0:128], in1=Z[par][zi][:], op=ALU.add))
                    yield
                    zi = 1 - zi
                    cur = nxt
                Zf = Z[par][zi]
                zkey = K_('Z', par, zi)
                pV = nps()
                sc.op('pe', [('r2', 'v', hp), 'ident'], [('ps', pV)], lambda e: e.matmul(
                    dn.ps[pV][:, 0:128], lhsT=f2(hb['r2_v']), rhs=ident[:], start=True, stop=True))
                sc.op('act', [('ps', pV)], [K_('Vtok', par)], lambda e: e.activation(
                    out=Vtok[par][:], in_=dn.ps[pV][:, 0:128], func=AF.Copy))
                sc.op('pe', [('bd', 'Ah', hp), 'ident'], ['pb1'], lambda e: e.transpose(
                    out=pbk[:, 0:128], in_=hb['bd_Ah'][:, c, :], identity=ident[:]))
                sc.op('pe', [('bd', 'Kh', hp), 'ident'], ['pb1'], lambda e: e.transpose(
                    out=pbk[:, 128:256], in_=hb['bd_Kh'][:, c, :], identity=ident[:]))
                sc.op('dve', ['pb1'], [K_('AKtok', par)], lambda e: e.tensor_copy(out=AKtok[par][:], in_=pbk[:, 0:256]))
                yield
                pW = nps()
                sc.op('pe', [('bd', 'Bt', hp), ('Tb', hp)], [('ps', pW)], lambda e: e.matmul(
                    dn.ps[pW][:, 0:128], lhsT=hb['bd_Bt'][:, c, :], rhs=Tb[:], start=True, stop=False))
                sc.op('pe', [K_('NM', par), K_('Vtok', par)], [('ps', pW)], lambda e: e.matmul(
                    dn.ps[pW][:, 0:128], lhsT=NM[par][:, 0:128], rhs=Vtok[par][:], start=False, stop=True))
                sc.op('act', [('ps', pW)], [K_('Wsb')], lambda e: e.activation(out=Wsb[:], in_=dn.ps[pW][:, 0:128], func=AF.Copy))
                yield
                pU = nps()
                sc.op('pe', [zkey, K_('Wsb')], [('ps', pU)], lambda e: e.matmul(
                    dn.ps[pU][:, 0:128], lhsT=Zf[:], rhs=Wsb[:], start=True, stop=True))
                sc.op('dve', [('ps', pU)], [K_('Usb')], lambda e: e.tensor_copy(out=Usb[:], in_=dn.ps[pU][:, 0:128]))
                yield
                pT = nps()
                sc.op('pe', [K_('AKtok', par), K_('Usb')], [('ps', pT)], lambda e: e.matmul(
                    dn.ps[pT][:, 0:128], lhsT=AKtok[par][:, 0:128], rhs=Usb[:], start=True, stop=False))
                sc.op('pe', [K_('AKtok', par), K_('Vtok', par)], [('ps', pT)], lambda e: e.matmul(
                    dn.ps[pT][:, 0:128], lhsT=AKtok[par][:, 128:256], rhs=Vtok[par][:], start=False, stop=True))
                pY = nps()
                sc.op('pe', [('Tb', hp), ('bd', 'Rt', hp)], [('ps', pY)], lambda e: e.matmul(
                    dn.ps[pY][:, 0:128], lhsT=Tb[:], rhs=hb['bd_Rt'][:, c, :], start=True, stop=False))
                sc.op('pe', [K_('Usb'), K_('NM', par)], [('ps', pY)], lambda e: e.matmul(
                    dn.ps[pY][:, 0:128], lhsT=Usb[:], rhs=NM[par][:, 128:256], start=False, stop=False))
                sc.op('pe', [K_('Vtok', par), K_('NM', par)], [('ps', pY)], lambda e: e.matmul(
                    dn.ps[pY][:, 0:128], lhsT=Vtok[par][:], rhs=NM[par][:, 256:384], start=False, stop=True))
                sc.op('dve', [('ps', pT), ('Tf', hp), ('eG', hp)], [('Tf', hp)], lambda e: e.scalar_tensor_tensor(
                    out=Tf[:], in0=Tf[:], scalar=eG[:, c * 64 + 63:c * 64 + 64], in1=dn.ps[pT][:, 0:128],
                    op0=ALU.mult, op1=ALU.add))
                sc.op('act', [('Tf', hp)], [('Tb', hp)], lambda e: e.activation(out=Tb[:], in_=Tf[:], func=AF.Copy))
                sc.op('act', [('ps', pY)], [('yT', hp)], lambda e: e.activation(
                    out=yT[0:64, c * 64:(c + 1) * 64], in_=dn.ps[pY][0:64, 0:64], func=AF.Copy))
                sc.op('act', [('ps', pY)], [('yT', hp)], lambda e: e.activation(
                    out=yT[64:128, c * 64:(c + 1) * 64], in_=dn.ps[pY][64:128, 64:128], func=AF.Copy))
                yield

            def epilogue(hp, t0):
                hb = H[hp]
                yT, gT, shv, kv = hb['yT'], hb['gT'], sh[4 + hp], ('sh', 4 + hp)
                sc.op('pool', [('yT', hp)], ['y16'], lambda e: e.tensor_copy(out=y16[:], in_=yT[:]))
                pM = nps()
                sc.op('pe', ['y16', 'onesbd'], [('ps', pM)], lambda e: e.matmul(
                    dn.ps[pM][:, 0:TT], lhsT=onesbd[:], rhs=y16[:], start=True, stop=True))
                sc.op('dve', [('ps', pM), ('yT', hp)], ['cen'], lambda e: e.scalar_tensor_tensor(
                    out=cen[:], in0=dn.ps[pM][:, 0:TT], scalar=-1.0 / 64, in1=yT[:], op0=ALU.mult, op1=ALU.add))
                sc.op('act', ['cen'], ['y16'], lambda e: e.activation(out=y16[:], in_=cen[:], func=AF.Square))
                pV2 = nps()
                sc.op('pe', ['y16', 'onesbd'], [('ps', pV2)], lambda e: e.matmul(
                    dn.ps[pV2][:, 0:TT], lhsT=onesbd[:], rhs=y16[:], start=True, stop=True))
                sc.op('dve', [('ps', pV2)], ['rs'], lambda e: e.tensor_scalar(
                    out=rs[:], in0=dn.ps[pV2][:, 0:TT], scalar1=1.0 / 64, scalar2=64e-5, op0=ALU.mult, op1=ALU.add))
                sc.op('act', ['rs'], ['rs'], lambda e: e.activation(out=rs[:], in_=rs[:], func=AF.Sqrt))
                sc.op('dve', ['rs'], ['rs'], lambda e: e.reciprocal(out=rs[:], in_=rs[:]))
                sc.op('dve', ['rs', 'cen'], ['cen'], lambda e: e.tensor_tensor(out=cen[:], in0=cen[:], in1=rs[:], op=ALU.mult))
                sc.op('dve', ['cen', 'rw_pv'], ['cen'], lambda e: e.tensor_scalar(
                    out=cen[:], in0=cen[:], scalar1=pv[:, 18 + hp:19 + hp], scalar2=pv[:, 20 + hp:21 + hp],
                    op0=ALU.mult, op1=ALU.add))
                pBn = nps()
                sc.op('pe', [('rkk16', hp), 'onesbd'], [('ps', pBn)], lambda e: e.matmul(
                    dn.ps[pBn][:, 0:TT], lhsT=onesbd[:], rhs=hb['rkk16'][:], start=True, stop=True))
                sc.op('dve', [('ps', pBn), kv], ['rs'], lambda e: e.tensor_tensor(
                    out=rs[:], in0=dn.ps[pBn][:, 0:TT], in1=shv[:], op=ALU.mult))
                sc.op('pool', ['rs', 'cen'], ['cen'], lambda e: e.tensor_tensor(out=cen[:], in0=cen[:], in1=rs[:], op=ALU.add))
                sc.op('dve', ['cen', ('gT', hp)], ['ob'], lambda e: e.tensor_tensor(out=ob[:], in0=cen[:], in1=gT[:], op=ALU.mult))
                sc.dma(STORE_Q, OT[3, hp, :, t0:t0 + TT], ob[:], ['ob'], [])

            ci = 0
            for t0 in range(0, S, TT):
                for g in range(8):
                    gi = GIDX['rw%d' % g]
                    if t0 == 0:
                        sc.op('pool', [], [('raw', g)], lambda e, g=g: e.memset(raw[g][:, 0:1], 0.0))
                        sc.dma('sp', raw[g][:, 1:TT + 1], UC[gi, :, 0:TT], [], [('raw', g)])
                    else:
                        sc.dma('sp', raw[g][:, :], UC[gi, :, t0 - 1:t0 + TT], [], [('raw', g)])
                    sc.op('pool', [('raw', g)], ['dtmp'], lambda e, g=g: e.tensor_tensor(
                        out=dtmp[:], in0=raw[g][:, 0:TT], in1=raw[g][:, 1:TT + 1], op=ALU.subtract))
                    sc.op('dve', ['dtmp', ('raw', g), 'rw_pv'], [('sh', g)], lambda e, g=g: e.scalar_tensor_tensor(
                        out=sh[g][:], in0=dtmp[:], scalar=pv[:, g:g + 1], in1=raw[g][:, 1:TT + 1],
                        op0=ALU.mult, op1=ALU.add))
                    if g % 2:
                        yield
                sc.op('act', [('sh', 6)], ['twa'], lambda e: e.activation(out=twa[0:64, :], in_=sh[6][0:64, :], func=AF.Tanh))
                sc.op('pool', [('sh', 6)], ['twa'], lambda e: e.tensor_copy(out=twa[64:128, :], in_=sh[6][64:128, :]))
                sc.op('act', [('sh', 7)], ['sgl'], lambda e: e.activation(out=sgl[:], in_=sh[7][:], func=AF.Sigmoid))
                for hp in range(2):
                    prep_hp(hp)
                    yield
                for c in range(NCH):
                    par = ci % 2
                    ci += 1
                    gens = [chunk(hp, c, par) for hp in range(2)]
                    while gens:
                        for g_ in list(gens):
                            try:
                                next(g_)
                            except StopIteration:
                                gens.remove(g_)
                        yield
                for hp in range(2):
                    epilogue(hp, t0)
                    yield


NEG = -30000.0
BIGV = 1e30


def nsa_consts(S):
    NB, NKT = S // 64, S // 128
    n_cmp = (S - 32) // 16 + 1
    NT = (n_cmp + 127) // 128
    n = np.arange(128)
    t = np.arange(128)
    d = {}
    cp = np.zeros((128, 17, 128), np.float32)
    for k in range(17):
        cp[:, k, :] = np.where(16 * n[:, None] + 31 <= 128 * k + t[None, :], 0.0, NEG)
    d["c_cpat"] = cp
    d["c_causneg"] = np.where(n[:, None] > t[None, :], NEG, 0.0).astype(np.float32)
    d["c_bandneg"] = np.where(n[:, None] <= t[None, :], NEG, 0.0).astype(np.float32)
    E = np.zeros((128, NKT, 128), np.float32)
    for j in range(NKT):
        for sl in range(128):
            bb = 2 * j + sl // 64
            E[bb, j, sl] = 1.0
    d["c_E"] = E[:max(NB, 1)] if NB <= 128 else E
    ov = np.zeros((128, NT, NB + 1), np.float32)
    for jn in range(NT):
        nn = jn * 128 + n
        valid = nn < n_cmp
        cs = 16 * nn
        for b in range(NB):
            ov[:, jn, b] = ((cs < 64 * (b + 1)) & (cs + 32 > 64 * b) & valid).astype(np.float32)
        ov[:, jn, NB] = valid.astype(np.float32)
    d["c_ovl"] = ov
    jj = np.arange(2 * NB)
    j = jj - NB
    hi = (t >= 64).astype(np.int64)
    allowed = j[None, :] <= hi[:, None]
    forced = (j[None, :] == hi[:, None]) | (j[None, :] == hi[:, None] - 1)
    d["c_amnf"] = (allowed & ~forced).astype(np.float32)
    d["c_fbna"] = np.where(forced, BIGV, np.where(allowed, 0.0, -1.0)).astype(np.float32)
    return d


class NSA:
    def __init__(self, dn, gla, es):
        self.dn, self.mk, self.sc, self.gla = dn, dn.mk, dn.sc, gla
        mk, S = self.mk, self.mk.S
        self.NB, self.NKT = S // 64, S // 128
        self.n_cmp = (S - 32) // 16 + 1
        self.NT = (self.n_cmp + 127) // 128
        self.cin = {n: mk.din(n, shp) for n, shp in (
            ("c_cpat", [128, 17, 128]), ("c_causneg", [128, 128]), ("c_bandneg", [128, 128]),
            ("c_E", [self.NB, self.NKT, 128]), ("c_ovl", [128, self.NT, self.NB + 1]),
            ("c_amnf", [128, 2 * self.NB]), ("c_fbna", [128, 2 * self.NB]))}

    def run(self, L, UC, UT, OT, ws, es):
        sc, mk, dn, S = self.sc, self.mk, self.dn, self.mk.S
        NB, NKT, NT, n_cmp = self.NB, self.NKT, self.NT, self.n_cmp
        ident = dn.ident
        PS = dn.ps
        sc.ns = 'nsa'
        T = {}
        T['cpat'] = mk.sb(es, "ns_cpat", [128, 17 * 128], BF16)
        T['caus'] = mk.sb(es, "ns_caus", [128, 128], BF16)
        T['band'] = mk.sb(es, "ns_band", [128, 128], BF16)
        T['Eb'] = mk.sb(es, "ns_E", [NB, NKT * 128], BF16)
        T['ovl'] = mk.sb(es, "ns_ovl", [128, NT * (NB + 1)], BF16)
        T['amnf'] = mk.sb(es, "ns_amnf", [128, 2 * NB], F32)
        T['fbna'] = mk.sb(es, "ns_fbna", [128, 2 * NB], F32)
        T['ksT'] = mk.sb(es, "ns_ksT", [64, S], BF16)
        T['kwT'] = mk.sb(es, "ns_kwT", [64, S], BF16)
        T['vs'] = mk.sb(es, "ns_vs", [128, NKT, 128], BF16)
        T['vw'] = mk.sb(es, "ns_vw", [128, NKT, 128], BF16)
        T['kcmpT'] = mk.sb(es, "ns_kcmpT", [64, NT * 128], BF16)
        T['vcmp'] = mk.sb(es, "ns_vcmp", [128, NT, 128], BF16)
        T['qf'] = mk.sb(es, "ns_qf", [64, 4, 128], F32)
        T['q16'] = mk.sb(es, "ns_q16", [64, 512], BF16)
        T['gf'] = mk.sb(es, "ns_gf", [64, 12, 128], F32)
        T['Pc'] = [mk.sb(es, "ns_Pc%d" % i, [128, 512], BF16) for i in range(NT)]
        T["Pr"] = [mk.sb(es, "ns_Pr%d" % i, [128, 512], BF16) for i in range(4)]
        T['ox'] = [mk.sb(es, "ns_ox%d" % i, [64, 512], F32) for i in range(3)]
        T['zx'] = [mk.sb(es, "ns_zx%d" % i, [64, 512], F32) for i in range(3)]
        T['zr'] = mk.sb(es, "ns_zr", [128, 8], F32)
        T['imp'] = mk.sb(es, "ns_imp", [128, NB], F32)
        T['sc1'] = mk.sb(es, "ns_sc1", [128, NB], F32)
        T['sc2'] = mk.sb(es, "ns_sc2", [128, NB], F32)
        T['m8'] = mk.sb(es, "ns_m8", [128, 16], F32)
        T['selm'] = mk.sb(es, "ns_selm", [128, 128], BF16)
        T['selT'] = mk.sb(es, "ns_selT", [128, 128], BF16)
        T['acc'] = T['ox'][0]
        T['ob'] = mk.sb(es, "ns_ob", [64, 512], BF16)
        with ExitStack() as es2:
            stage = [mk.sb(es2, "ns_st%d" % i, [128, 2048], F32) for i in range(2)]
            sti = [0]

            def load_cast(dst_ap, src_ap, rows, cols, key):
                for c0 in range(0, cols, 2048):
                    c1 = min(cols, c0 + 2048)
                    si = sti[0] % 2
                    sti[0] += 1
                    sc.dma('sp', stage[si][0:rows, 0:c1 - c0], src_ap[:, c0:c1], [], [('nst', si)])
                    eng = ('dve', 'pool')[sti[0] % 2]
                    sc.op(eng, [('nst', si)], [key], lambda e, si=si, c0=c0, c1=c1: e.tensor_copy(
                        out=dst_ap[:, c0:c1], in_=stage[si][0:rows, 0:c1 - c0]))

            load_cast(T['cpat'][:], self.cin["c_cpat"].rearrange("p k t -> p (k t)"), 128, 17 * 128, 'cpat')
            load_cast(T['caus'][:], self.cin["c_causneg"], 128, 128, 'caus')
            load_cast(T['band'][:], self.cin["c_bandneg"], 128, 128, 'band')
            load_cast(T['Eb'][:], self.cin["c_E"].rearrange("p k t -> p (k t)"), NB, NKT * 128, 'E')
            load_cast(T['ovl'][:], self.cin["c_ovl"].rearrange("p k t -> p (k t)"), 128, NT * (NB + 1), 'ovl')
            sc.dma('sp', T['amnf'][:], self.cin["c_amnf"][:, :], [], ['amnf'])
            sc.dma('sp', T['fbna'][:], self.cin["c_fbna"][:, :], [], ['fbna'])
            load_cast(T['ksT'][:], UC[GIDX['nsa_ks'], 0:64, :], 64, S, 'ksT')
            load_cast(T['kwT'][:], UC[GIDX['nsa_kw'], 0:64, :], 64, S, 'kwT')
            sc.op('pool', [], ['vs'], lambda e: e.memset(T['vs'][:], 1.0))
            sc.op('pool', [], ['vw'], lambda e: e.memset(T['vw'][:], 1.0))
            sc.op('pool', [], ['vcmp'], lambda e: e.memset(T['vcmp'][:], 1.0))
            for n0 in range(0, NKT, 8):
                n1 = min(NKT, n0 + 8)
                si = sti[0] % 2
                sti[0] += 1
                stv = stage[si][:, 0:(n1 - n0) * 128].rearrange("p (n c) -> p n c", c=128)
                sc.dma('sp', stv, UT[n0 * 128:n1 * 128, 0:128].rearrange("(n p) c -> p n c", p=128), [], [('nst', si)])
                sc.op('dve', [('nst', si)], ['vs'], lambda e, n0=n0, n1=n1, stv=stv: e.tensor_copy(
                    out=T['vs'][:, n0:n1, 0:64], in_=stv[:, :, 0:64]))
                sc.op('pool', [('nst', si)], ['vw'], lambda e, n0=n0, n1=n1, stv=stv: e.tensor_copy(
                    out=T['vw'][:, n0:n1, 0:64], in_=stv[:, :, 64:128]))
            kcmpT, vcmp = T['kcmpT'], T['vcmp']
            sc.op('pool', [], ['kcmpT'], lambda e: e.memset(kcmpT[:], 0.0))
            kc16 = mk.sb(es2, "ns_kc16", [64, S], BF16)
            W1 = mk.sb(es2, "ns_W1", [64, 32 * 128], BF16)
            W2f = mk.sb(es2, "ns_W2f", [128, 64], F32)
            W2 = mk.sb(es2, "ns_W2", [128, 64], BF16)
            posf = mk.sb(es2, "ns_posf", [64, 32], F32)
            pos16 = mk.sb(es2, "ns_pos16", [64, 32], BF16)
            cb = mk.sb(es2, "ns_cb", [128, 1], F32)
            hid = mk.sb(es2, "ns_hid", [128, NT * 128], BF16)
            for which in range(2):
                gname = 'nsa_kc' if which == 0 else 'nsa_vc'
                load_cast(kc16[:], UC[GIDX[gname], 0:64, :], 64, S, 'kc16')
                load_cast(W1[:], ws['w1r'][which], 64, 32 * 128, 'W1')
                sc.dma('sp', W2f[:], ws['w2'][which], [], ['W2f'])
                sc.op('dve', ['W2f'], ['W2'], lambda e: e.tensor_copy(out=W2[:], in_=W2f[:]))
                sc.dma('sp', posf[:], ws['posT'][which], [], ['posf'])
                sc.op('dve', ['posf'], ['pos16'], lambda e: e.tensor_copy(out=pos16[:], in_=posf[:]))
                pc = 0
                for j in range(32):
                    sc.op('pes', ['W1', 'pos16'], [('ps', pc)], lambda e, j=j: e.matmul(
                        PS[pc][:, 0:1], lhsT=W1[:, j * 128:(j + 1) * 128], rhs=pos16[:, j:j + 1],
                        start=(j == 0), stop=(j == 31)))
                sc.op('dve', [('ps', pc)], ['cb'], lambda e: e.tensor_copy(out=cb[:], in_=PS[pc][:, 0:1]))
                ph = 1
                for j in range(32):
                    sc.op('pes', ['W1', 'kc16'], [('ps', ph)], lambda e, j=j: e.matmul(
                        PS[ph][:, 0:n_cmp], lhsT=W1[:, j * 128:(j + 1) * 128],
                        rhs=kc16[:, j:j + 16 * (n_cmp - 1) + 1:16], start=(j == 0), stop=(j == 31)))
                sc.op('pool', [], ['hid'], lambda e: e.memset(hid[:], 0.0))
                sc.op('act', [('ps', ph), 'cb'], ['hid'], lambda e: e.activation(
                    out=hid[:, 0:n_cmp], in_=PS[ph][:, 0:n_cmp], func=AF.Silu, bias=cb[:, 0:1], scale=1.0))
                if which == 0:
                    po = 2
                    sc.op('pe', ['W2', 'hid'], [('ps', po)], lambda e: e.matmul(
                        PS[po][0:64, 0:n_cmp], lhsT=W2[:], rhs=hid[:, 0:n_cmp], start=True, stop=True))
                    sc.op('act', [('ps', po)], ['kcmpT'], lambda e: e.activation(
                        out=kcmpT[:, 0:n_cmp], in_=PS[po][0:64, 0:n_cmp], func=AF.Copy))
                else:
                    for jn in range(NT):
                        po = 2 + jn % 2
                        sc.op('pe', ['W2', 'hid'], [('ps', po)], lambda e, jn=jn, po=po: e.matmul(
                            PS[po][:, 0:64], lhsT=hid[:, jn * 128:(jn + 1) * 128], rhs=W2[:], start=True, stop=True))
                        sc.op('act', [('ps', po)], ['vcmp'], lambda e, jn=jn, po=po: e.activation(
                            out=vcmp[:, jn, 0:64], in_=PS[po][:, 0:64], func=AF.Copy))
            sc.ns = None
            sc.barrier()
        return self.tiles(UC, OT, T)

    def tiles(self, UC, OT, T):
        sc, mk, dn, S = self.sc, self.mk, self.dn, self.mk.S
        NB, NKT, NT, n_cmp = self.NB, self.NKT, self.NT, self.n_cmp
        ident = dn.ident
        PS = dn.ps
        cpat, caus, band, Eb, ovl, amnf, fbna = (T[k] for k in ('cpat', 'caus', 'band', 'Eb', 'ovl', 'amnf', 'fbna'))
        ksT, kwT, vs, vw, kcmpT, vcmp = (T[k] for k in ('ksT', 'kwT', 'vs', 'vw', 'kcmpT', 'vcmp'))
        qf, q16, gf, Pc, Pr, ox, zx, zr, imp, sc1, sc2, m8, selm, selT, acc, ob = (T[k] for k in (
            'qf', 'q16', 'gf', 'Pc', 'Pr', 'ox', 'zx', 'zr', 'imp', 'sc1', 'sc2', 'm8', 'selm', 'selT', 'acc', 'ob'))
        sc.op('pool', [], ['selm'], lambda e: e.memset(selm[:], 0.0))
        pbk = dn.pb[1]
        SB_ = [0, 1, 3, 4]
        IB_ = [0, 1]
        OZ = 2
        DEPTH_P = 3
        pr_i = [0]
        cnt = [0, 0]

        def bc4(ap):
            return ap.unsqueeze(1).broadcast_to((ap.shape[0], 4, 128))

        def issue(kT_ap, kkey, addmask):
            sbk = SB_[cnt[0] % len(SB_)]
            cnt[0] += 1
            nmm = 1 + len(addmask)
            sc.op('pe', [kkey, 'q16'], [('ps', sbk)], lambda e: e.matmul(
                PS[sbk][:, :], lhsT=kT_ap, rhs=q16[:], start=True, stop=(nmm == 1)))
            for mi, (ml, mr, mkeys) in enumerate(addmask):
                sc.op('pe', mkeys, [('ps', sbk)], lambda e, ml=ml, mr=mr, mi=mi: e.matmul(
                    PS[sbk][:, :], lhsT=ml, rhs=mr, start=False, stop=(mi == nmm - 2)))
            pt = Pr[pr_i[0] % len(Pr)]
            pkey = ('Pr', pr_i[0] % len(Pr))
            pr_i[0] += 1
            sc.op('act', [('ps', sbk)], [pkey], lambda e: e.activation(out=pt[:], in_=PS[sbk][:, :], func=AF.Exp))
            return pt, pkey

        def consume(pt, pkey, vlhs, vkey, first, last):
            sc.op('pe', [pkey, vkey], [('ps', OZ)], lambda e: e.matmul(
                PS[OZ][:, :], lhsT=vlhs, rhs=pt[:], start=first, stop=last))

        def branch(pairs):
            pend = []
            n = len(pairs)
            done = 0
            for idx, (kT_ap, kkey, addmask, vlhs, vkey) in enumerate(pairs):
                pt, pkey = issue(kT_ap, kkey, addmask)
                pend.append((pt, pkey, vlhs, vkey))
                if len(pend) > DEPTH_P:
                    a = pend.pop(0)
                    consume(a[0], a[1], a[2], a[3], done == 0, done == n - 1)
                    done += 1
                yield
            while pend:
                a = pend.pop(0)
                consume(a[0], a[1], a[2], a[3], done == 0, done == n - 1)
                done += 1
            yield

        def finish(x):
            sc.op('act', [('ps', OZ)], [('ox', x)], lambda e: e.activation(out=ox[x][:], in_=PS[OZ][0:64, :], func=AF.Copy))
            sc.op('dve', [('ps', OZ)], [('zx', x)], lambda e: e.tensor_scalar(
                out=zx[x][:], in0=PS[OZ][64:128, :], scalar1=1e-30, scalar2=None, op0=ALU.max))
            sc.op('dve', [('zx', x)], [('zx', x)], lambda e: e.reciprocal(out=zx[x][:], in_=zx[x][:]))

        for i in range(NKT):
            t0 = i * 128
            for h in range(4):
                sc.dma('sp', qf[:, h, :], UC[GIDX['nsa_q%d' % h], 0:64, t0:t0 + 128], [], ['qf'])
            for gi in range(12):
                sc.dma('sp', gf[:, gi, :], UC[GIDX['nsa_g%d' % gi], 0:64, t0:t0 + 128], [], ['gf'])
            sc.op('act', ['qf'], ['q16'], lambda e: e.activation(
                out=q16[:], in_=qf[:].rearrange("p h t -> p (h t)"), func=AF.Copy, scale=0.125))
            sc.op('act', ['gf'], ['gf'], lambda e: e.activation(out=gf[:], in_=gf[:], func=AF.Sigmoid))
            yield
            jmax = min(NT - 1, (8 * i + 6) // 128)
            for jn in range(jmax + 1):
                sbk = SB_[cnt[0] % len(SB_)]
                cnt[0] += 1
                k = i - 16 * jn
                need_mask = k <= 16
                sc.op('pe', ['kcmpT', 'q16'], [('ps', sbk)], lambda e, jn=jn, sbk=sbk, need_mask=need_mask: e.matmul(
                    PS[sbk][:, :], lhsT=kcmpT[:, jn * 128:(jn + 1) * 128], rhs=q16[:], start=True, stop=(not need_mask)))
                if need_mask:
                    sc.op('pe', ['cpat', 'ident'], [('ps', sbk)], lambda e, k=k, sbk=sbk: e.matmul(
                        PS[sbk][:, :], lhsT=ident[:], rhs=bc4(cpat[:, k * 128:(k + 1) * 128]), start=False, stop=True))
                sc.op('act', [('ps', sbk)], [('Pc', jn)], lambda e, jn=jn, sbk=sbk: e.activation(
                    out=Pc[jn][:], in_=PS[sbk][:, :], func=AF.Exp))
                yield
            for jn in range(jmax + 1):
                sc.op('pe', [('Pc', jn), 'vcmp'], [('ps', OZ)], lambda e, jn=jn: e.matmul(
                    PS[OZ][:, :], lhsT=vcmp[:, jn, :], rhs=Pc[jn][:], start=(jn == 0), stop=(jn == jmax)))
            finish(0)
            for h in range(4):
                ib = IB_[h // 2]
                c0 = (h % 2) * (NB + 1)
                for jn in range(jmax + 1):
                    sc.op('pe', [('Pc', jn), 'ovl'], [('ps', ib)], lambda e, jn=jn, h=h, ib=ib, c0=c0: e.matmul(
                        PS[ib][:, c0:c0 + NB + 1], lhsT=Pc[jn][:, h * 128:(h + 1) * 128],
                        rhs=ovl[:, jn * (NB + 1):(jn + 1) * (NB + 1)], start=(jn == 0), stop=(jn == jmax)))
            for h in range(4):
                ib = IB_[h // 2]
                c0 = (h % 2) * (NB + 1)
                sc.op('dve', [('ps', ib)], ['zr'], lambda e, h=h, ib=ib, c0=c0: e.tensor_scalar(
                    out=zr[:, h:h + 1], in0=PS[ib][:, c0 + NB:c0 + NB + 1], scalar1=1e-30, scalar2=None, op0=ALU.max))
            sc.op('dve', ['zr'], ['zr'], lambda e: e.reciprocal(out=zr[:, 4:8], in_=zr[:, 0:4]))
            for h in range(4):
                ib = IB_[h // 2]
                c0 = (h % 2) * (NB + 1)
                if h == 0:
                    sc.op('dve', [('ps', ib), 'zr'], ['imp'], lambda e, ib=ib, c0=c0: e.tensor_scalar(
                        out=imp[:], in0=PS[ib][:, c0:c0 + NB], scalar1=zr[:, 4:5], scalar2=None, op0=ALU.mult))
                else:
                    sc.op('dve', [('ps', ib), 'zr', 'imp'], ['imp'], lambda e, h=h, ib=ib, c0=c0: e.scalar_tensor_tensor(
                        out=imp[:], in0=PS[ib][:, c0:c0 + NB], scalar=zr[:, 4 + h:5 + h], in1=imp[:],
                        op0=ALU.mult, op1=ALU.add))
            yield
            jsl = slice(NB - 2 * i, 2 * NB - 2 * i)
            sc.op('dve', ['imp', 'amnf'], ['sc1'], lambda e, jsl=jsl: e.tensor_tensor(
                out=sc1[:], in0=imp[:], in1=amnf[:, jsl], op=ALU.mult))
            sc.op('dve', ['sc1', 'fbna'], ['sc1'], lambda e, jsl=jsl: e.tensor_tensor(
                out=sc1[:], in0=sc1[:], in1=fbna[:, jsl], op=ALU.add))
            sc.op('dve', ['sc1'], ['sc1'], lambda e: e.memset(sc1[:, 0:1], BIGV))
            sc.op('dve', ['sc1'], ['m8'], lambda e: e.max(out=m8[:, 0:8], in_=sc1[:]))
            sc.op('dve', ['sc1', 'm8'], ['sc2'], lambda e: e.match_replace(
                out=sc2[:], in_to_replace=m8[:, 0:8], in_values=sc1[:], imm_value=-2.0))
            sc.op('dve', ['sc2'], ['m8'], lambda e: e.max(out=m8[:, 8:16], in_=sc2[:]))
            sc.op('dve', ['sc1', 'm8'], ['sc2'], lambda e: e.tensor_scalar(
                out=sc2[:], in0=sc1[:], scalar1=m8[:, 15:16], scalar2=None, op0=ALU.is_ge))
            sc.op('dve', ['sc2'], ['selm'], lambda e: e.tensor_scalar(
                out=selm[:, 0:NB], in0=sc2[:], scalar1=-1.0, scalar2=-NEG, op0=ALU.add, op1=ALU.mult))
            yield
            j0 = max(0, i - 4)
            pairs = []
            for j in range(j0, i + 1):
                am = []
                if j == i:
                    am.append((ident[:], bc4(caus[:]), ['ident', 'caus']))
                elif j == i - 4:
                    am.append((ident[:], bc4(band[:]), ['ident', 'band']))
                pairs.append((kwT[:, j * 128:(j + 1) * 128], 'kwT', am, vw[:, j, :], 'vw'))
            yield from branch(pairs)
            finish(2)
            sc.op('pe', ['selm', 'ident'], ['pb1'], lambda e: e.transpose(
                out=pbk[:, 0:128], in_=selm[:], identity=ident[:]))
            sc.op('act', ['pb1'], ['selT'], lambda e: e.activation(out=selT[:], in_=pbk[:, 0:128], func=AF.Copy))
            pairs = []
            for j in range(i + 1):
                am = [(Eb[0:NB, j * 128:(j + 1) * 128], bc4(selT[0:NB, :]), ['E', 'selT'])]
                if j == i:
                    am.append((ident[:], bc4(caus[:]), ['ident', 'caus']))
                pairs.append((ksT[:, j * 128:(j + 1) * 128], 'ksT', am, vs[:, j, :], 'vs'))
            yield from branch(pairs)
            finish(1)
            for x in range(3):
                sc.op('dve', [('zx', x), 'gf'], [('zx', x)], lambda e, x=x: e.tensor_tensor(
                    out=zx[x][:], in0=zx[x][:], in1=gf[:, 4 * x:4 * x + 4, :].rearrange("p h t -> p (h t)"), op=ALU.mult))
                if x == 0:
                    sc.op('pool', [('zx', x), ('ox', x)], [('ox', 0)], lambda e, x=x: e.tensor_tensor(
                        out=acc[:], in0=ox[x][:], in1=zx[x][:], op=ALU.mult))
                else:
                    sc.op('pool', [('zx', x), ('ox', x)], [('ox', x)], lambda e, x=x: e.tensor_tensor(
                        out=ox[x][:], in0=ox[x][:], in1=zx[x][:], op=ALU.mult))
                    if x == 1:
                        sc.op('dve', [('ox', 0), ('ox', x)], [('ox', 0)], lambda e, x=x: e.tensor_tensor(
                            out=acc[:], in0=acc[:], in1=ox[x][:], op=ALU.add))
                    else:
                        sc.op('dve', [('ox', 0), ('ox', x)], ['ob'], lambda e, x=x: e.tensor_tensor(
                            out=ob[:], in0=acc[:], in1=ox[x][:], op=ALU.add))
            sc.dma(STORE_Q, OT[0].rearrange("c (hl e) t -> e (c hl) t", e=64)[:, :, t0:t0 + 128],
                   ob[:].rearrange("p (h t) -> p h t", h=4), ['ob'], [])
            yield


_CACHE = {}


def kernel(**inputs):
    S = 8192
    ncores = 8
    if 'mk' not in _CACHE:
        _CACHE['mk'] = build_program(S)
    mk = _CACHE['mk']
    inp = {k: np.asarray(v) for k, v in inputs.items()}
    shared = host_inputs_shared(inp, S=S)
    shared = {k: np.ascontiguousarray(v, dtype=np.float32) for k, v in shared.items() if k in mk.inputs}
    in_maps = []
    for c in range(ncores):
        d = dict(shared)
        d['x'] = np.ascontiguousarray(inp['x'][c], dtype=np.float32)
        d['p'] = np.ascontiguousarray(inp['p'][:, c], dtype=np.float32)
        in_maps.append(d)
    res = run_bass_kernel_spmd(mk.nc, in_maps, core_ids=list(range(ncores)))
    return np.stack([np.asarray(r['out'], dtype=np.float32) for r in res.results], axis=0)
```
